# Optimizing a Trainium2 kernel written in Bass

```python
import math
import jax, jax.numpy as jnp
from jax import lax
import numpy as np

D_MODEL = 1024
BATCH = 8
SEQ = 2048
DEPTH = 4

SSD_D_INNER = D_MODEL
SSD_HEADDIM = 64
SSD_HEADS = SSD_D_INNER // SSD_HEADDIM
SSD_GROUPS = 2
SSD_HPG = SSD_HEADS // SSD_GROUPS
SSD_STATE = 128
SSD_CONV = 4
SSD_CHUNK = 128
SSD_CONV_DIM = SSD_D_INNER + 2 * SSD_GROUPS * SSD_STATE
RET_HEADS = 4
RET_QK_DIM = 128
RET_HEAD_V = D_MODEL // RET_HEADS
RET_CHUNK = 128
ROPE_BASE = 10000.0
ATT_Q_HEADS = 16
ATT_KV_HEADS = 2
ATT_GROUP = ATT_Q_HEADS // ATT_KV_HEADS
ATT_HEAD_DIM = D_MODEL // ATT_Q_HEADS
WINDOW = 128
ATT_BLOCK = 128
REL_BUCKETS = 32
REL_MAX_EXACT = REL_BUCKETS // 2
REL_MAX_DIST = 128
N_BRANCH = 3
BRANCH_WIDTH = D_MODEL
D_FF = 2816
EPS = 1e-6
NEG = -1e30

IN_SPLIT_SIZES = (
    SSD_D_INNER,
    SSD_CONV_DIM,
    SSD_HEADS,
    RET_HEADS * RET_QK_DIM,
    RET_HEADS * RET_QK_DIM,
    RET_HEADS * RET_HEAD_V,
    RET_HEADS * RET_HEAD_V,
    ATT_Q_HEADS * ATT_HEAD_DIM,
    ATT_KV_HEADS * ATT_HEAD_DIM,
    ATT_KV_HEADS * ATT_HEAD_DIM,
    N_BRANCH * D_MODEL,
)
IN_WIDTH = sum(IN_SPLIT_SIZES)

kernel_name = "hybrid_ssd_retention_swa_macaron"


def rmsnorm(x, g):
    xf = x.astype(jnp.float32)
    y = xf * lax.rsqrt(jnp.mean(xf * xf, axis=-1, keepdims=True) + EPS)
    return (y * g.astype(jnp.float32)).astype(x.dtype)


def swiglu(x, w_in, w_out):
    a, b = jnp.split(x @ w_in, 2, axis=-1)
    return (jax.nn.silu(a) * b) @ w_out


def causal_dwconv(x, w, b):
    c = x.shape[-1]
    y = lax.conv_general_dilated(x, w[:, None, :].astype(x.dtype), (1,), [(SSD_CONV - 1, 0)],
                                 dimension_numbers=('NWC', 'WIO', 'NWC'), feature_group_count=c)
    return y + b


def ssd_mixer(z, xbc, dt_raw, conv_w, conv_b, dt_bias, a_log, d_skip, norm_g):
    bsz, L, _ = z.shape
    nc = L // SSD_CHUNK
    xbc = jax.nn.silu(causal_dwconv(xbc, conv_w, conv_b))
    xs, bm, cm = jnp.split(xbc, [SSD_D_INNER, SSD_D_INNER + SSD_GROUPS * SSD_STATE], axis=-1)
    xs = xs.reshape(bsz, nc, SSD_CHUNK, SSD_GROUPS, SSD_HPG, SSD_HEADDIM)
    bm = bm.reshape(bsz, nc, SSD_CHUNK, SSD_GROUPS, SSD_STATE)
    cm = cm.reshape(bsz, nc, SSD_CHUNK, SSD_GROUPS, SSD_STATE)
    dt = jax.nn.softplus(dt_raw.astype(jnp.float32) + dt_bias.astype(jnp.float32))
    dt = dt.reshape(bsz, nc, SSD_CHUNK, SSD_GROUPS, SSD_HPG)
    a = -jnp.exp(a_log.astype(jnp.float32)).reshape(SSD_GROUPS, SSD_HPG)
    da = jnp.moveaxis(dt * a, 2, -1)
    a_cs = jnp.cumsum(da, axis=-1)
    causal = jnp.tril(jnp.ones((SSD_CHUNK, SSD_CHUNK), dtype=bool))
    seg = a_cs[..., :, None] - a_cs[..., None, :]
    lmat = jnp.exp(jnp.where(causal, seg, -jnp.inf))
    xdt = xs * dt[..., None]
    cb = jnp.einsum('bclgn,bcsgn->bcgls', cm, bm)
    y_diag = jnp.einsum('bcgjls,bcsgjp->bclgjp', cb[:, :, :, None] * lmat, xdt)
    decay_st = jnp.moveaxis(jnp.exp(a_cs[..., -1:] - a_cs), -1, 2)
    states = jnp.einsum('bclgn,bclgjp->bcgjpn', bm, xdt * decay_st[..., None])
    chunk_decay = jnp.exp(a_cs[..., -1])

    def step(h, inp):
        s, dcy = inp
        return (h * dcy[..., None, None] + s).astype(h.dtype), h

    h0 = jnp.zeros_like(states[:, 0])
    _, prev = lax.scan(step, h0, (jnp.moveaxis(states, 1, 0), jnp.moveaxis(chunk_decay, 1, 0)))
    prev = jnp.moveaxis(prev, 0, 1)
    decay_out = jnp.moveaxis(jnp.exp(a_cs), -1, 2)
    y_off = jnp.einsum('bclgn,bcgjpn->bclgjp', cm, prev) * decay_out[..., None]
    y = y_diag + y_off + xs * d_skip.reshape(SSD_GROUPS, SSD_HPG, 1)
    y = y.reshape(bsz, L, SSD_D_INNER).astype(z.dtype)
    return rmsnorm(y * jax.nn.silu(z), norm_g)


def rope(x, cos, sin):
    x1, x2 = jnp.split(x, 2, axis=-1)
    c = cos[:, None, :]
    s = sin[:, None, :]
    return jnp.concatenate([x1 * c - x2 * s, x2 * c + x1 * s], axis=-1)


def retention_mixer(q, k, v, g, gn_g, cos, sin):
    bsz, L, _ = q.shape
    nc = L // RET_CHUNK
    q = rope(q.reshape(bsz, L, RET_HEADS, RET_QK_DIM), cos, sin)
    k = rope(k.reshape(bsz, L, RET_HEADS, RET_QK_DIM), cos, sin) * (RET_QK_DIM ** -0.5)
    v = v.reshape(bsz, L, RET_HEADS, RET_HEAD_V)
    log_g = jnp.log(1.0 - jnp.exp2(-5.0 - jnp.arange(RET_HEADS, dtype=jnp.float32)))
    idx = jnp.arange(RET_CHUNK, dtype=jnp.float32)
    diff = idx[:, None] - idx[None, :]
    dmat = jnp.where(diff >= 0, jnp.exp(jnp.maximum(diff, 0.0)[None] * log_g[:, None, None]), 0.0)
    qc = q.reshape(bsz, nc, RET_CHUNK, RET_HEADS, RET_QK_DIM)
    kc = k.reshape(bsz, nc, RET_CHUNK, RET_HEADS, RET_QK_DIM)
    vc = v.reshape(bsz, nc, RET_CHUNK, RET_HEADS, RET_HEAD_V)
    inner = jnp.einsum('bnhij,bnjhe->bnihe', jnp.einsum('bnihd,bnjhd->bnhij', qc, kc) * dmat, vc)
    zeta = jnp.exp((RET_CHUNK - 1.0 - idx)[:, None] * log_g[None, :])
    chunk_kv = jnp.einsum('bnjhd,bnjhe->bnhde', kc * zeta[..., None], vc)
    chunk_decay = jnp.exp(RET_CHUNK * log_g)

    def step(r, s):
        return (r * chunk_decay[:, None, None] + s).astype(r.dtype), r

    r0 = jnp.zeros_like(chunk_kv[:, 0])
    _, prev = lax.scan(step, r0, jnp.moveaxis(chunk_kv, 1, 0))
    prev = jnp.moveaxis(prev, 0, 1)
    xi = jnp.exp((idx + 1.0)[:, None] * log_g[None, :])
    cross = jnp.einsum('bnihd,bnhde->bnihe', qc, prev) * xi[..., None]
    o = (inner + cross).reshape(bsz, L, RET_HEADS, RET_HEAD_V).astype(jnp.float32)
    mu = jnp.mean(o, axis=-1, keepdims=True)
    var = jnp.mean(jnp.square(o - mu), axis=-1, keepdims=True)
    o = ((o - mu) * lax.rsqrt(var + EPS)).reshape(bsz, L, RET_HEADS * RET_HEAD_V)
    o = (o * gn_g.astype(jnp.float32)).astype(g.dtype)
    return o * jax.nn.silu(g)


def t5_bucket(dist):
    is_small = dist < REL_MAX_EXACT
    d = jnp.maximum(dist, 1).astype(jnp.float32)
    large = REL_MAX_EXACT + (jnp.log(d / REL_MAX_EXACT) / math.log(REL_MAX_DIST / REL_MAX_EXACT)
                             * (REL_BUCKETS - REL_MAX_EXACT)).astype(jnp.int32)
    large = jnp.minimum(large, REL_BUCKETS - 1)
    return jnp.where(is_small, dist, large)


def swa_sink_attention(q, k, v, sinks, band_bias, band_mask):
    bsz, L, _ = q.shape
    nb = L // ATT_BLOCK
    q = q.reshape(bsz, nb, ATT_BLOCK, ATT_KV_HEADS, ATT_GROUP, ATT_HEAD_DIM)
    k = k.reshape(bsz, L, ATT_KV_HEADS, ATT_HEAD_DIM)
    v = v.reshape(bsz, L, ATT_KV_HEADS, ATT_HEAD_DIM)

    def band(t):
        tp = jnp.pad(t, ((0, 0), (ATT_BLOCK, 0), (0, 0), (0, 0)))
        tp = tp.reshape(bsz, nb + 1, ATT_BLOCK, ATT_KV_HEADS, ATT_HEAD_DIM)
        return jnp.concatenate([tp[:, :-1], tp[:, 1:]], axis=2)

    kb, vb = band(k), band(v)
    s = jnp.einsum('bnqkgd,bnskd->bnkgqs', q, kb).astype(jnp.float32) * (ATT_HEAD_DIM ** -0.5)
    s = jnp.where(band_mask[None, :, None, None], s + band_bias, NEG)
    sink = sinks.astype(jnp.float32).reshape(ATT_KV_HEADS, ATT_GROUP, 1, 1)
    m = jnp.maximum(jnp.max(s, axis=-1, keepdims=True), sink)
    e = jnp.exp(s - m)
    p = e / (jnp.sum(e, axis=-1, keepdims=True) + jnp.exp(sink - m))
    o = jnp.einsum('bnkgqs,bnskd->bnqkgd', p.astype(v.dtype), vb)
    return o.reshape(bsz, L, ATT_Q_HEADS * ATT_HEAD_DIM)


def setup_inputs(seed: int = 0) -> dict:
    key = jax.random.key(seed)
    ks = iter(jax.random.split(key, 32))
    f32 = jnp.float32

    def nrm(shape, fan_in):
        return jax.random.normal(next(ks), shape, f32) * (fan_in ** -0.5)

    def gain(shape):
        return 1.0 + 0.1 * jax.random.normal(next(ks), shape, f32)

    x = jax.random.normal(next(ks), (BATCH, SEQ, D_MODEL), f32)
    ffn1_pre_g = gain((DEPTH, D_MODEL))
    ffn1_post_g = gain((DEPTH, D_MODEL))
    w_ffn1_in = nrm((DEPTH, D_MODEL, 2 * D_FF), D_MODEL)
    w_ffn1_out = nrm((DEPTH, D_FF, D_MODEL), D_FF)
    mix_pre_g = gain((DEPTH, D_MODEL))
    mix_post_g = gain((DEPTH, D_MODEL))
    w_in = nrm((DEPTH, D_MODEL, IN_WIDTH), D_MODEL)
    conv_w = nrm((DEPTH, SSD_CONV, SSD_CONV_DIM), SSD_CONV)
    conv_b = 0.1 * jax.random.normal(next(ks), (DEPTH, SSD_CONV_DIM), f32)
    dt0 = jnp.exp(jax.random.uniform(next(ks), (DEPTH, SSD_HEADS), f32,
                                     math.log(1e-3), math.log(1e-1)))
    dt_bias = dt0 + jnp.log(-jnp.expm1(-dt0))
    a_log = jnp.log(jax.random.uniform(next(ks), (DEPTH, SSD_HEADS), f32, 1.0, 16.0))
    d_skip = gain((DEPTH, SSD_HEADS))
    ssd_norm_g = gain((DEPTH, SSD_D_INNER))
    ret_gn_g = gain((DEPTH, RET_HEADS * RET_HEAD_V))
    attn_sinks = 0.5 * jax.random.normal(next(ks), (DEPTH, ATT_Q_HEADS), f32)
    rel_bias = 0.5 * jax.random.normal(next(ks), (REL_BUCKETS, ATT_Q_HEADS), f32)
    b_gate = 0.1 * jax.random.normal(next(ks), (DEPTH, N_BRANCH * D_MODEL), f32)
    w_branch = nrm((DEPTH, N_BRANCH, BRANCH_WIDTH, D_MODEL), BRANCH_WIDTH)
    w_out = nrm((DEPTH, D_MODEL, D_MODEL), D_MODEL)
    ffn2_pre_g = gain((DEPTH, D_MODEL))
    ffn2_post_g = gain((DEPTH, D_MODEL))
    w_ffn2_in = nrm((DEPTH, D_MODEL, 2 * D_FF), D_MODEL)
    w_ffn2_out = nrm((DEPTH, D_FF, D_MODEL), D_FF)
    return {"x": x, "ffn1_pre_g": ffn1_pre_g, "ffn1_post_g": ffn1_post_g,
            "w_ffn1_in": w_ffn1_in, "w_ffn1_out": w_ffn1_out,
            "mix_pre_g": mix_pre_g, "mix_post_g": mix_post_g, "w_in": w_in,
            "conv_w": conv_w, "conv_b": conv_b, "dt_bias": dt_bias, "a_log": a_log,
            "d_skip": d_skip, "ssd_norm_g": ssd_norm_g, "ret_gn_g": ret_gn_g,
            "attn_sinks": attn_sinks, "rel_bias": rel_bias, "b_gate": b_gate,
            "w_branch": w_branch, "w_out": w_out,
            "ffn2_pre_g": ffn2_pre_g, "ffn2_post_g": ffn2_post_g,
            "w_ffn2_in": w_ffn2_in, "w_ffn2_out": w_ffn2_out}


def reference(x, ffn1_pre_g, ffn1_post_g, w_ffn1_in, w_ffn1_out, mix_pre_g, mix_post_g, w_in,
              conv_w, conv_b, dt_bias, a_log, d_skip, ssd_norm_g, ret_gn_g, attn_sinks, rel_bias,
              b_gate, w_branch, w_out, ffn2_pre_g, ffn2_post_g, w_ffn2_in, w_ffn2_out):
    bsz, L, _ = x.shape
    offsets = np.cumsum(IN_SPLIT_SIZES)[:-1].tolist()
    pos = jnp.arange(L, dtype=jnp.float32)
    inv = 1.0 / (ROPE_BASE ** (jnp.arange(0, RET_QK_DIM, 2, dtype=jnp.float32) / RET_QK_DIM))
    ang = pos[:, None] * inv[None, :]
    cos, sin = jnp.cos(ang), jnp.sin(ang)
    nb = L // ATT_BLOCK
    qi = jnp.arange(ATT_BLOCK)[:, None]
    sj = jnp.arange(2 * ATT_BLOCK)[None, :]
    dist = qi + ATT_BLOCK - sj
    blk = jnp.arange(nb)[:, None, None]
    band_mask = (dist >= 0) & (dist < WINDOW) & ((blk > 0) | (sj >= ATT_BLOCK))
    band_bias = jnp.transpose(rel_bias[t5_bucket(jnp.maximum(dist, 0))], (2, 0, 1))
    band_bias = band_bias.astype(jnp.float32).reshape(ATT_KV_HEADS, ATT_GROUP, ATT_BLOCK, 2 * ATT_BLOCK)

    for l in range(DEPTH):
        x = x + 0.5 * rmsnorm(swiglu(rmsnorm(x, ffn1_pre_g[l]), w_ffn1_in[l], w_ffn1_out[l]), ffn1_post_g[l])
        h = rmsnorm(x, mix_pre_g[l])
        (z, xbc, dt_raw, rq, rk, rv, rg, aq, ak, av, gate) = jnp.split(h @ w_in[l], offsets, axis=-1)
        y_ssd = ssd_mixer(z, xbc, dt_raw, conv_w[l], conv_b[l], dt_bias[l], a_log[l], d_skip[l], ssd_norm_g[l])
        y_ret = retention_mixer(rq, rk, rv, rg, ret_gn_g[l], cos, sin)
        y_att = swa_sink_attention(aq, ak, av, attn_sinks[l], band_bias, band_mask)
        ys = jnp.stack([y_ssd.astype(x.dtype), y_ret.astype(x.dtype), y_att.astype(x.dtype)], axis=2)
        branch = jnp.einsum('blmc,mcd->blmd', ys, w_branch[l])
        gates = jax.nn.sigmoid((gate + b_gate[l]).reshape(bsz, L, N_BRANCH, D_MODEL))
        y = jnp.sum(gates * branch, axis=2) @ w_out[l]
        x = x + rmsnorm(y, mix_post_g[l])
        x = x + 0.5 * rmsnorm(swiglu(rmsnorm(x, ffn2_pre_g[l]), w_ffn2_in[l], w_ffn2_out[l]), ffn2_post_g[l])
    return x
```

```python
import math
from contextlib import ExitStack
import numpy as np
import concourse.bass as bass
import concourse.mybir as mybir
from concourse.bass_utils import run_bass_kernel_spmd

F32 = mybir.dt.float32
BF16 = mybir.dt.bfloat16
AF = mybir.ActivationFunctionType
ALU = mybir.AluOpType

D = 1024
DFF = 2816
INW = 10000
DEPTH = 4
SEQ = 2048
T = 512
EPS = 1e-6
NV = 212
NCST = 1796
ENGS = ("pe", "act", "dve", "pool", "sp")
C_Z, C_XBC, C_DT, C_RQ, C_RK, C_RV, C_RG, C_AQ, C_AK, C_AV, C_GATE = 0, 1024, 2560, 2576, 3088, 3600, 4624, 5648, 6672, 6800, 6928
LOGG = [math.log(1.0 - 2.0 ** (-5.0 - h)) for h in range(4)]


class Op:
    __slots__ = ("eng", "fn", "reads", "writes", "dma", "waits", "signal", "semkey", "deps", "sigval", "idx", "need")

    def __init__(self, eng, fn, reads, writes, dma):
        self.eng, self.fn, self.reads, self.writes, self.dma = eng, fn, reads, writes, dma
        self.waits = {}
        self.signal = False
        self.semkey = None


class Sched:
    def __init__(self, nc):
        self.nc = nc
        self.ops = []
        self.last_writer = {}
        self.readers = {}

    def add(self, eng, fn, reads=(), writes=(), dma=None):
        op = Op(eng, fn, tuple(reads), tuple(writes), dma)
        deps = set()
        for r in op.reads:
            w = self.last_writer.get(r)
            if w is not None:
                deps.add(w)
        for r in op.writes:
            w = self.last_writer.get(r)
            if w is not None:
                deps.add(w)
            for rd in self.readers.get(r, ()):
                deps.add(rd)
        op.deps = deps
        for r in op.reads:
            self.readers.setdefault(r, []).append(op)
        for r in op.writes:
            self.last_writer[r] = op
            self.readers[r] = []
        self.ops.append(op)
        return op

    @staticmethod
    def _skip(d, op):
        return d is op or (d.dma is None and op.dma is None and d.eng == "pe" and op.eng == "pe")

    def finalize(self):
        counters = {}
        for i, op in enumerate(self.ops):
            op.idx = i
        for op in self.ops:
            best = {}
            for d in op.deps:
                if self._skip(d, op):
                    continue
                k = ("dma", d.dma) if d.dma is not None else ("eng", d.eng)
                if k not in best or d.idx > best[k].idx:
                    best[k] = d
            op.need = list(best.values())
            for d in op.need:
                d.signal = True
        for op in self.ops:
            if op.dma is not None:
                op.semkey = ("dma", op.dma)
                op.signal = True
            else:
                op.semkey = ("eng", op.eng)
            if op.signal:
                counters[op.semkey] = counters.get(op.semkey, 0) + (16 if op.dma is not None else 1)
                op.sigval = counters[op.semkey]
        seen = {e: {} for e in ENGS}
        for op in self.ops:
            for d in op.need:
                k, v = d.semkey, d.sigval
                if seen[op.eng].get(k, 0) >= v:
                    continue
                if op.waits.get(k, 0) < v:
                    op.waits[k] = v
            for k, v in op.waits.items():
                seen[op.eng][k] = v
        self.semkeys = list(counters.keys())
        self.final_counts = counters

    def emit(self):
        nc = self.nc
        self.finalize()
        with ExitStack() as st:
            sems = {}
            for i, k in enumerate(self.semkeys):
                sems[k] = st.enter_context(nc.semaphore("s%d" % i))
            block = st.enter_context(nc.Block())
            per = {e: [op for op in self.ops if op.eng == e] for e in ENGS}

            def run(e, ops):
                for op in ops:
                    for k, v in op.waits.items():
                        e.wait_ge(sems[k], v)
                    ins = op.fn(e)
                    if op.signal:
                        ins.then_inc(sems[op.semkey], 16 if op.dma is not None else 1)

            @block.tensor
            def _(e):
                run(e, per["pe"])

            @block.scalar
            def _(e):
                run(e, per["act"])

            @block.vector
            def _(e):
                run(e, per["dve"])

            @block.gpsimd
            def _(e):
                run(e, per["pool"])

            @block.sync
            def _(e):
                run(e, per["sp"])
                for k in self.semkeys:
                    e.wait_ge(sems[k], self.final_counts[k])


class Buf:
    def __init__(self, mem, off, nbytes, dtype, shape=None):
        self.off, self.nbytes = off, nbytes
        if shape is not None:
            n = 1
            for d_ in shape:
                n *= d_
            assert n * (4 if dtype == F32 else 2) == nbytes, (shape, nbytes)
        v = mem[:, off // 2:(off + nbytes) // 2]
        if dtype == F32:
            v = v.bitcast(F32)
        if shape is not None and len(shape) == 2:
            v = v.rearrange("p (a b) -> p a b", a=shape[0])
        elif shape is not None and len(shape) == 3:
            v = v.rearrange("p (a b c) -> p a b c", a=shape[0], b=shape[1])
        self.ap = v

    def pg(self, lo=0, hi=None):
        hi = self.nbytes if hi is None else hi
        return [("pg", i) for i in range((self.off + lo) // 1024, (self.off + hi - 1) // 1024 + 1)]


def build(L=DEPTH, NTOK=SEQ, dbg=False, stop=99):
    NT = NTOK // T
    nc = bass.Bass("TRN2", target_bir_lowering=False)
    dr = lambda n, s, k="ExternalInput": nc.dram_tensor(n, s, F32, kind=k).ap()
    x_d = dr("x", [NTOK, D])
    w1i_d, w1o_d = dr("w1i", [L, D, 2 * DFF]), dr("w1o", [L, DFF, D])
    w2i_d, w2o_d = dr("w2i", [L, D, 2 * DFF]), dr("w2o", [L, DFF, D])
    win_d, wbr_d, wo_d = dr("win", [L, D, INW]), dr("wbr", [L, 3, D, D]), dr("wo", [L, D, D])
    pv_d, cst_d = dr("pv", [128, L * NV]), dr("cst", [128, NCST])
    cs_d = dr("cs", [2, 128, NTOK])
    bias_d = dr("biasT", [128, 2 * 16 * 128])
    out_d = dr("out", [NTOK, D], "ExternalOutput")
    xs_d = dr("xscr", [128, 8, NTOK], "Internal")
    dbg_d = dr("dbgo", [3, 128, 8, NTOK], "ExternalOutput") if dbg else None

    with ExitStack() as st:
        TOTAL = 206 * 1024
        mem = st.enter_context(nc.sbuf_tensor("mem", [128, TOTAL // 2], BF16))
        ps = st.enter_context(nc.psum_tensor("ps", [128, 4096], F32))
        S = Sched(nc)
        cur = [0]

        def alloc(nbytes, dtype, shape=None):
            nb_ = (nbytes + 1023) // 1024 * 1024
            b = Buf(mem, cur[0], nbytes, dtype, shape)
            cur[0] += nb_
            assert cur[0] <= TOTAL, cur[0]
            return b

        cst = alloc(NCST * 4, F32)
        pv = alloc(L * NV * 4, F32)
        cbf = alloc(3 * 128 * 2, BF16)
        biasT = alloc(2 * 16 * 128 * 2, BF16, (2, 16, 128))
        xt = alloc(8 * T * 4, F32, (8, T))
        hT = alloc(8 * T * 2, BF16, (8, T))
        ytmp = alloc(8 * T * 4, F32, (8, T))
        rs = alloc(T * 4, F32)
        wsl = [alloc(8 * 512 * 2, BF16, (8, 512)) for _ in range(3)]
        ysum = alloc(8 * T * 4, F32, (8, T))
        yT = alloc(8 * T * 2, BF16, (8, T))
        etmp = [alloc(T * 4, F32) for _ in range(2)]
        sst = alloc(1024 * 4, F32)
        sstb = alloc(1024 * 2, BF16)
        rst = alloc(1024 * 4, F32)
        rstb = alloc(1024 * 2, BF16)
        hist = alloc(12 * 3 * 4, F32, (12, 3))
        kTa = alloc(2 * 640 * 2, BF16, (2, 640))
        vtok = alloc(5 * 2 * 128 * 2, BF16, (5, 2, 128))
        smal = alloc(1024, F32)
        smal2 = alloc(1024, F32)
        A0 = cur[0]

        def arena(off, nbytes, dtype, shape=None):
            assert A0 + off + nbytes <= TOTAL, (A0, off, nbytes)
            return Buf(mem, A0 + off, nbytes, dtype, shape)

        K = 1024
        sq = arena(0, 8 * K, BF16, (8, T))
        hid = arena(8 * K, 22 * K, BF16, (22, T))
        wsl.extend(arena(30 * K + i * 8 * K, 8 * K, BF16, (8, 512)) for i in range(4))
        wring = [[0, 1, 2]]
        ynb = arena(0, 2 * K, BF16)
        yf = [arena(2 * K + i * 4 * K, 4 * K, F32) for i in range(2)]
        MX = 10 * K
        sz = arena(MX, 8 * K, BF16, (4, 1024))
        xs_tok = arena(MX + 8 * K, 8 * K, BF16, (4, 1024))
        B_tok = arena(MX + 16 * K, 2 * K, BF16, (4, 256))
        bcT = arena(MX + 18 * K, 4 * K, BF16, (4, T))
        raw = [arena(MX + 22 * K + i * 3 * K, 515 * 4, F32) for i in range(2)]
        cacc = [arena(MX + 28 * K + i * 2 * K, 2 * K, F32) for i in range(2)]
        xsTt = [arena(MX + 32 * K + i * K, K, BF16) for i in range(2)]
        xdt = arena(MX + 34 * K, 2 * K, BF16)
        xdd = arena(MX + 36 * K, 2 * K, BF16)
        rhsm = arena(MX + 38 * K, 8 * K, F32)
        Eb = arena(MX + 46 * K, 4 * K, BF16, (16, 128))
        MTb = arena(MX + 50 * K, 4 * K, BF16, (16, 128))
        cbm = arena(MX + 54 * K, 512, BF16, (2, 128))
        qf = [arena(MX + i * 2 * K, 2 * K, F32) for i in range(2)]
        t12 = [arena(MX + 4 * K + i * 2 * K, 2 * K, F32) for i in range(2)]
        qTr = arena(MX + 8 * K, 4 * K, BF16, (4, T))
        kTr = arena(MX + 12 * K, 4 * K, BF16, (4, T))
        v_tok = arena(MX + 16 * K, 8 * K, BF16, (4, 1024))
        srg = arena(MX + 24 * K, 8 * K, BF16, (4, 1024))
        SDT = arena(MX + 32 * K, K, BF16, (4, 128))
        kz = arena(MX + 33 * K, K, BF16, (4, 128))
        qx = arena(MX + 34 * K, K, BF16, (4, 128))
        cst_t = arena(MX + 35 * K, 4 * K, F32, (2, T))
        qTm = arena(MX, 16 * K, BF16, (16, T))
        stmp = [arena(MX + 16 * K + i * 2 * K, 2 * K, F32) for i in range(2)]
        PT = arena(MX + 20 * K, 8 * K, BF16, (2, 16, 128))
        PTf = arena(MX + 20 * K, 8 * K, BF16, (2, 2048))

        identf = cst.ap[:, 0:128]
        triu = cst.ap[:, 128:256]
        onesf = cst.ap[:, 256:384]
        slow = cst.ap[:, 384:512]
        causT = cst.ap[:, 512:640]
        Rm = cst.ap[:, 640:768]
        dmatT = cst.ap[:, 768:1280].rearrange("p (h i) -> p h i", h=4)
        xiT = cst.ap[:, 1280:1792].rearrange("p (h i) -> p h i", h=4)
        zeta = cst.ap[:, 1792:1796]
        identb = cbf.ap[:, 0:128]
        onesb = cbf.ap[:, 128:256]
        one1b = cbf.ap[:, 256:257]

        pbank = [0]

        def nb():
            b = pbank[0] % 8
            pbank[0] += 1
            return b

        def nb2():
            if pbank[0] % 2:
                pbank[0] += 1
            b = pbank[0] % 8
            pbank[0] += 2
            return b

        def nb4():
            while pbank[0] % 4:
                pbank[0] += 1
            b = pbank[0] % 8
            pbank[0] += 4
            return b

        PB = lambda b, n=1: ps[:, b * 512:(b + n) * 512]
        PBb = lambda b: ps[:, b * 512:(b + 1) * 512].bitcast(BF16)
        PK = lambda b, n=1: [("ps", b + i) for i in range(n)]

        def mm(out, lhsT, rhs, start, stop, r, w):
            S.add("pe", lambda e: e.matmul(out, lhsT=lhsT, rhs=rhs, start=start, stop=stop), r, w)

        def tr(out, in_, ident, r, w):
            S.add("pe", lambda e: e.transpose(out, in_, ident), r, w)

        def act(out, in_, func, r, w, **kw):
            S.add("act", lambda e: e.activation(out=out, in_=in_, func=func, **kw), r, w)

        def tt(out, in0, in1, op, r, w, eng="dve"):
            S.add(eng, lambda e: e.tensor_tensor(out=out, in0=in0, in1=in1, op=op), r, w)

        def stt(out, in0, scalar, in1, op0, op1, r, w):
            S.add("dve", lambda e: e.scalar_tensor_tensor(out=out, in0=in0, scalar=scalar, in1=in1, op0=op0, op1=op1), r, w)

        def ts(out, in0, s1, s2, op0, op1, r, w):
            S.add("dve", lambda e: e.tensor_scalar(out=out, in0=in0, scalar1=s1, scalar2=s2, op0=op0, op1=op1), r, w)

        def ts1(out, in0, s1, op, r, w):
            S.add("dve", lambda e: e.tensor_single_scalar(out=out, in_=in0, scalar=s1, op=op), r, w)

        def cp(out, in_, r, w, eng="dve"):
            S.add(eng, lambda e: e.tensor_copy(out=out, in_=in_), r, w)

        def ms(ap, val, w, eng="dve"):
            S.add(eng, lambda e: e.memset(ap, val), (), w)

        def dma(eng, out, in_, r, w, key):
            S.add(eng, lambda e: e.dma_start(out=out, in_=in_), r, w, dma=key)

        wcnt = [0]

        def wslab(w2d, k0, nk, c0, ncols, dst_c0=0, slot=None):
            if slot is None:
                slot = wring[0][wcnt[0] % len(wring[0])]
                wcnt[0] += 1
            src = w2d[k0 * 128:(k0 + nk) * 128, c0:c0 + ncols].rearrange("(k p) n -> p k n", p=128)
            dst = wsl[slot].ap[:, 0:nk, dst_c0:dst_c0 + ncols]
            dma("pool", dst, src, (), wsl[slot].pg(), ("w", slot))
            return slot

        dma("sp", cst.ap, cst_d, (), cst.pg(), "cst")
        dma("sp", pv.ap, pv_d, (), pv.pg(), "pv")
        dma("pool", biasT.ap, bias_d.rearrange("p (b h q) -> p b h q", b=2, h=16), (), biasT.pg(), "bias")
        cp(identb, identf, cst.pg(), cbf.pg())
        ts1(onesb, onesf, 1.0 / 1024.0, ALU.mult, cst.pg(), cbf.pg())
        cp(cbf.ap[:, 256:384], onesf, cst.pg(), cbf.pg())

        def pvc(l, c, n=1):
            return pv.ap[:, l * NV + c:l * NV + c + n]

        def norm_stats(src_ap, src_keys):
            act(sq.ap, src_ap, AF.Square, src_keys, sq.pg())
            b = nb()
            for c in range(8):
                mm(PB(b), onesb, sq.ap[:, c, :], c == 0, c == 7, sq.pg() + cbf.pg(), PK(b))
            act(rs.ap, PB(b), AF.Ln, PK(b), rs.pg(), bias=EPS, scale=1.0)
            act(rs.ap, rs.ap, AF.Exp, rs.pg(), rs.pg(), scale=-0.5)

        def pre_norm(l, gcol):
            norm_stats(xt.ap, xt.pg())
            g_b = pvc(l, gcol, 8).unsqueeze(2).broadcast_to([128, 8, T])
            rs_b = rs.ap.unsqueeze(1).broadcast_to([128, 8, T])
            tt(ytmp.ap, xt.ap, g_b, ALU.mult, xt.pg() + pv.pg(), ytmp.pg())
            tt(hT.ap, ytmp.ap, rs_b, ALU.mult, ytmp.pg() + rs.pg(), hT.pg())

        def post_norm_add(l, gcol, half):
            norm_stats(ytmp.ap, ytmp.pg())
            g_b = pvc(l, gcol, 8).unsqueeze(2).broadcast_to([128, 8, T])
            rs_b = rs.ap.unsqueeze(1).broadcast_to([128, 8, T])
            tt(ytmp.ap, ytmp.ap, g_b, ALU.mult, ytmp.pg() + pv.pg(), ytmp.pg())
            tt(ytmp.ap, ytmp.ap, rs_b, ALU.mult, ytmp.pg() + rs.pg(), ytmp.pg())
            stt(xt.ap, ytmp.ap, half, xt.ap, ALU.mult, ALU.add, ytmp.pg() + xt.pg(), xt.pg())

        def ffn(l, wi_d, wo_d_, gpre, gpost):
            pre_norm(l, gpre)
            wring[0] = [0, 1, 2, 3, 4, 5, 6]
            wi = wi_d[l]
            wo_ = wo_d_[l]
            j = 0
            ecnt = 0
            for g0 in range(0, DFF, 512):
                gw = min(512, DFF - g0)
                sa = wslab(wi, 0, 8, g0, gw)
                sb = wslab(wi, 0, 8, DFF + g0, gw)
                for jj in range(gw // 128):
                    ba, bb = nb(), nb()
                    for k in range(8):
                        mm(PB(ba), wsl[sa].ap[:, k, jj * 128:(jj + 1) * 128], hT.ap[:, k, :], k == 0, k == 7,
                           wsl[sa].pg() + hT.pg(), PK(ba))
                    for k in range(8):
                        mm(PB(bb), wsl[sb].ap[:, k, jj * 128:(jj + 1) * 128], hT.ap[:, k, :], k == 0, k == 7,
                           wsl[sb].pg() + hT.pg(), PK(bb))
                    et = etmp[ecnt % 2]
                    ecnt += 1
                    act(et.ap, PB(ba), AF.Silu, PK(ba), et.pg())
                    tt(hid.ap[:, j, :], et.ap, PB(bb), ALU.mult, et.pg() + PK(bb), hid.pg(j * 1024, (j + 1) * 1024))
                    j += 1
            for mg in range(2):
                b0 = nb4()
                for kg in range(3):
                    k0 = kg * 8
                    nk = min(8, 22 - k0)
                    s = wslab(wo_, k0, nk, mg * 512, 512)
                    for m in range(4):
                        for k in range(nk):
                            mm(PB(b0 + m), wsl[s].ap[:, k, m * 128:(m + 1) * 128], hid.ap[:, k0 + k, :],
                               kg == 0 and k == 0, kg == 2 and k == nk - 1,
                               wsl[s].pg() + hid.pg((k0 + k) * 1024, (k0 + k + 1) * 1024), PK(b0 + m))
                for m in range(4):
                    c = mg * 4 + m
                    act(ytmp.ap[:, c, :], PB(b0 + m), AF.Copy, PK(b0 + m), ytmp.pg(c * 2048, (c + 1) * 2048))
            wring[0] = [0, 1, 2]
            post_norm_add(l, gpost, 0.5)

        def proj_fm(wl, col0, nch, consumer, dup=None):
            ci = 0
            while ci < nch:
                n = min(4, nch - ci)
                s = wslab(wl, 0, 8, col0 + ci * 128, n * 128)
                for jj in range(n):
                    b = nb()
                    for k in range(8):
                        mm(PB(b), wsl[s].ap[:, k, jj * 128:(jj + 1) * 128], hT.ap[:, k, :], k == 0, k == 7,
                           wsl[s].pg() + hT.pg(), PK(b))
                    consumer(ci + jj, b)
                ci += n

        def proj_tm(wl, col0, ncols, consumer):
            s = wslab(wl, 0, 8, col0, ncols)
            for i in range(4):
                b = nb()
                for k in range(8):
                    mm(PB(b)[:, 0:ncols], hT.ap[:, k, i * 128:(i + 1) * 128], wsl[s].ap[:, k, 0:ncols], k == 0, k == 7,
                       wsl[s].pg() + hT.pg(), PK(b))
                consumer(i, b)

        def to_featmajor(i, l, gcol):
            b = nb()
            for c in range(8):
                tr(PBb(b)[:, c * 128:(c + 1) * 128], ynb.ap[:, c * 128:(c + 1) * 128], identb, ynb.pg() + cbf.pg(), PK(b))
            src = PBb(b).rearrange("p (c t) -> p c t", c=8)
            dst = yT.ap[:, :, i * 128:(i + 1) * 128]
            if gcol is None:
                cp(dst, src, PK(b), yT.pg())
            else:
                tt(dst, src, pvc(l, gcol, 8).unsqueeze(2).broadcast_to([128, 8, 128]), ALU.mult, PK(b) + pv.pg(), yT.pg())

        def merge(l, m):
            wl = win_d[l]
            wb = wbr_d[l, m]
            ecnt = 0
            for half in range(2):
                sg = wslab(wl, 0, 8, C_GATE + m * 1024 + half * 512, 512)
                sbm = wslab(wb, 0, 8, half * 512, 512)
                for jj in range(4):
                    c = half * 4 + jj
                    bg, bb = nb(), nb()
                    for k in range(8):
                        mm(PB(bg), wsl[sg].ap[:, k, jj * 128:(jj + 1) * 128], hT.ap[:, k, :], k == 0, k == 7,
                           wsl[sg].pg() + hT.pg(), PK(bg))
                    for k in range(8):
                        mm(PB(bb), wsl[sbm].ap[:, k, jj * 128:(jj + 1) * 128], yT.ap[:, k, :], k == 0, k == 7,
                           wsl[sbm].pg() + yT.pg(), PK(bb))
                    et = etmp[ecnt % 2]
                    ecnt += 1
                    act(et.ap, PB(bg), AF.Sigmoid, PK(bg) + pv.pg(), et.pg(), bias=pvc(l, 124 + m * 8 + c), scale=1.0)
                    ysc = ysum.ap[:, c, :]
                    yk = ysum.pg(c * 2048, (c + 1) * 2048)
                    if m == 0:
                        tt(ysc, et.ap, PB(bb), ALU.mult, et.pg() + PK(bb), yk)
                    else:
                        tt(et.ap, et.ap, PB(bb), ALU.mult, et.pg() + PK(bb), et.pg())
                        tt(ysc, ysc, et.ap, ALU.add, et.pg() + yk, yk)

        def out_proj(l):
            cp(hT.ap, ysum.ap, ysum.pg(), hT.pg())
            for half in range(2):
                s = wslab(wo_d[l], 0, 8, half * 512, 512)
                for jj in range(4):
                    c = half * 4 + jj
                    b = nb()
                    for k in range(8):
                        mm(PB(b), wsl[s].ap[:, k, jj * 128:(jj + 1) * 128], hT.ap[:, k, :], k == 0, k == 7,
                           wsl[s].pg() + hT.pg(), PK(b))
                    act(ytmp.ap[:, c, :], PB(b), AF.Copy, PK(b), ytmp.pg(c * 2048, (c + 1) * 2048))
            post_norm_add(l, 24, 1.0)

        a_b = smal.ap[:, 0:16]
        esink = smal.ap[:, 16:32]
        dtb = smal.ap[:, 32:96].rearrange("p (i h) -> p i h", i=4)
        dab = smal.ap[:, 96:160].rearrange("p (i h) -> p i h", i=4)
        acs = smal2.ap[:, 0:32]
        dec_out = smal2.ap[:, 32:48]
        dec_st = smal2.ap[:, 48:64]
        cdec = smal2.ap[:, 64:80]
        ssq = smal2.ap[:, 80:81]
        bn6 = smal2.ap[:, 96:120].rearrange("p (h s) -> p h s", h=4)
        mv = smal2.ap[:, 120:128].rearrange("p (h s) -> p h s", h=4)
        rstd4 = smal2.ap[:, 128:132]
        den = smal2.ap[:, 136:152]

        def ssd_tile(l, ti):
            wl = win_d[l]
            for s2 in range(2):
                proj_tm(wl, C_Z + s2 * 512, 512,
                        lambda i, b, s2=s2: act(sz.ap[:, i, s2 * 512:(s2 + 1) * 512], PB(b), AF.Silu, PK(b), sz.pg()))
            sl = wslab(wl, 0, 8, C_DT, 16)
            bd = nb()
            for i in range(4):
                for k in range(8):
                    mm(PB(bd)[:, i * 16:(i + 1) * 16], hT.ap[:, k, i * 128:(i + 1) * 128], wsl[sl].ap[:, k, 0:16], k == 0, k == 7,
                       wsl[sl].pg() + hT.pg(), PK(bd))
            tt(dtb, PB(bd)[:, 0:64].rearrange("p (i h) -> p i h", i=4), pvc(l, 148, 16).unsqueeze(1).broadcast_to([128, 4, 16]),
               ALU.add, PK(bd) + pv.pg(), smal.pg())
            act(dtb, dtb, AF.Exp, smal.pg(), smal.pg())
            act(dtb, dtb, AF.Ln, smal.pg(), smal.pg(), bias=1.0, scale=1.0)
            tt(dab, dtb, a_b.unsqueeze(1).broadcast_to([128, 4, 16]), ALU.mult, smal.pg(), smal.pg())
            ccnt = [0]

            def xbc_consumer(c, b):
                rw = raw[ccnt[0] % 2]
                ca = cacc[ccnt[0] % 2]
                xtt = xsTt[ccnt[0] % 2]
                ccnt[0] += 1
                cp(rw.ap[:, 0:3], hist.ap[:, c, :], hist.pg(), rw.pg())
                act(rw.ap[:, 3:515], PB(b), AF.Copy, PK(b), rw.pg())
                cp(hist.ap[:, c, :], rw.ap[:, 512:515], rw.pg(), hist.pg())
                wc = lambda k: pvc(l, 64 + c * 4 + k)
                ts1(ca.ap, rw.ap[:, 0:512], wc(0), ALU.mult, rw.pg() + pv.pg(), ca.pg())
                for k in range(1, 4):
                    stt(ca.ap, rw.ap[:, k:k + 512], wc(k), ca.ap, ALU.mult, ALU.add, rw.pg() + pv.pg() + ca.pg(), ca.pg())
                if c < 8:
                    act(xtt.ap, ca.ap, AF.Silu, ca.pg() + pv.pg(), xtt.pg(), bias=pvc(l, 112 + c), scale=1.0)
                    dst_src = xtt
                else:
                    act(bcT.ap[:, c - 8, :], ca.ap, AF.Silu, ca.pg() + pv.pg(), bcT.pg(), bias=pvc(l, 112 + c), scale=1.0)
                if c < 10:
                    bt = nb()
                    for i in range(4):
                        src = xtt.ap[:, i * 128:(i + 1) * 128] if c < 8 else bcT.ap[:, c - 8, i * 128:(i + 1) * 128]
                        tr(PBb(bt)[:, i * 128:(i + 1) * 128], src, identb, (xtt.pg() if c < 8 else bcT.pg()) + cbf.pg(), PK(bt))
                    srcv = PBb(bt)[:, 0:512].rearrange("p (i f) -> p i f", i=4)
                    if c < 8:
                        cp(xs_tok.ap[:, :, c * 128:(c + 1) * 128], srcv, PK(bt), xs_tok.pg())
                    else:
                        cp(B_tok.ap[:, :, (c - 8) * 128:(c - 7) * 128], srcv, PK(bt), B_tok.pg())

            proj_fm(wl, C_XBC, 12, xbc_consumer)
            for i in range(4):
                cs_ = slice(i * 128, (i + 1) * 128)
                bs = nb()
                mm(PB(bs)[:, 0:16], triu, dab[:, i, :], True, True, cst.pg() + smal.pg(), PK(bs))
                mm(PB(bs)[:, 16:32], onesf, dab[:, i, :], True, True, cst.pg() + smal.pg(), PK(bs))
                cp(acs, PB(bs)[:, 0:32], PK(bs), smal2.pg())
                act(dec_out, acs[:, 0:16], AF.Exp, smal2.pg(), smal2.pg())
                tt(dec_st, acs[:, 16:32], acs[:, 0:16], ALU.subtract, smal2.pg(), smal2.pg())
                act(dec_st, dec_st, AF.Exp, smal2.pg(), smal2.pg())
                act(cdec, acs[:, 16:32], AF.Exp, smal2.pg(), smal2.pg())
                xs3 = xs_tok.ap[:, i, :].rearrange("p (h d) -> p h d", h=16)
                v3 = lambda b_: b_.ap.rearrange("p (h d) -> p h d", h=16)
                bc16 = lambda a_: a_.unsqueeze(2).broadcast_to([128, 16, 64])
                tt(v3(xdt), xs3, bc16(dtb[:, i, :]), ALU.mult, xs_tok.pg() + smal.pg(), xdt.pg())
                tt(v3(xdd), v3(xdt), bc16(dec_st), ALU.mult, xdt.pg() + smal2.pg(), xdd.pg())
                tt(rhsm.ap.rearrange("p (h l) -> p h l", h=16), triu.unsqueeze(1).broadcast_to([128, 16, 128]),
                   dab[:, i, :].unsqueeze(2).broadcast_to([128, 16, 128]), ALU.mult, cst.pg() + smal.pg(), rhsm.pg())
                b4 = nb4()
                for q in range(4):
                    mm(PB(b4 + q), slow, rhsm.ap[:, q * 512:(q + 1) * 512], True, True, cst.pg() + rhsm.pg(), PK(b4 + q))
                for q in range(4):
                    act(Eb.ap[:, q * 4:(q + 1) * 4, :], PB(b4 + q).rearrange("p (h l) -> p h l", h=4), AF.Exp, PK(b4 + q), Eb.pg())
                bcb = nb()
                for g in range(2):
                    mm(PB(bcb)[:, g * 128:(g + 1) * 128], bcT.ap[:, g, cs_], bcT.ap[:, 2 + g, cs_], True, True, bcT.pg(), PK(bcb))
                tt(cbm.ap, PB(bcb)[:, 0:256].rearrange("p (g l) -> p g l", g=2), causT.unsqueeze(1).broadcast_to([128, 2, 128]),
                   ALU.mult, PK(bcb) + cst.pg(), cbm.pg())
                for g in range(2):
                    tt(MTb.ap[:, 8 * g:8 * g + 8, :], Eb.ap[:, 8 * g:8 * g + 8, :],
                       cbm.ap[:, g, :].unsqueeze(1).broadcast_to([128, 8, 128]), ALU.mult, Eb.pg() + cbm.pg(), MTb.pg())
                bo = nb2()
                for g in range(2):
                    mm(PB(bo, 2)[:, g * 512:(g + 1) * 512], bcT.ap[:, 2 + g, cs_], sstb.ap[:, g * 512:(g + 1) * 512], True, True,
                       bcT.pg() + sstb.pg(), PK(bo, 2))
                by = nb2()
                for h in range(16):
                    mm(PB(by, 2)[:, h * 64:(h + 1) * 64], MTb.ap[:, h, :], xdt.ap[:, h * 64:(h + 1) * 64], True, True,
                       MTb.pg() + xdt.pg(), PK(by, 2))
                y0, y1 = yf
                tt(v3(y0), PB(bo, 2).rearrange("p (h d) -> p h d", h=16), bc16(dec_out), ALU.mult, PK(bo, 2) + smal2.pg(), y0.pg())
                tt(y0.ap, PB(by, 2), y0.ap, ALU.add, PK(by, 2) + y0.pg(), y0.pg())
                tt(v3(y1), xs3, bc16(pvc(l, 180, 16)), ALU.mult, xs_tok.pg() + pv.pg(), y1.pg())
                tt(y0.ap, y0.ap, y1.ap, ALU.add, y0.pg() + y1.pg(), y0.pg())
                bst = nb2()
                for g in range(2):
                    mm(PB(bst, 2)[:, g * 512:(g + 1) * 512], B_tok.ap[:, i, g * 128:(g + 1) * 128], xdd.ap[:, g * 512:(g + 1) * 512],
                       True, True, B_tok.pg() + xdd.pg(), PK(bst, 2))
                tt(v3(sst), v3(sst), bc16(cdec), ALU.mult, sst.pg() + smal2.pg(), sst.pg())
                tt(sst.ap, sst.ap, PB(bst, 2), ALU.add, sst.pg() + PK(bst, 2), sst.pg())
                cp(sstb.ap, sst.ap, sst.pg(), sstb.pg())
                tt(y0.ap, y0.ap, sz.ap[:, i, :], ALU.mult, y0.pg() + sz.pg(), y0.pg())
                ms(ssq, 0.0, smal2.pg())
                act(y1.ap, y0.ap, AF.Square, y0.pg(), y1.pg() + smal2.pg(), accum_out=ssq)
                act(ssq, ssq, AF.Ln, smal2.pg(), smal2.pg(), bias=EPS, scale=1.0 / 1024.0)
                act(ssq, ssq, AF.Exp, smal2.pg(), smal2.pg(), scale=-0.5)
                ts1(ynb.ap, y0.ap, ssq, ALU.mult, y0.pg() + smal2.pg(), ynb.pg())
                to_featmajor(i, l, 48)

        def ret_tile(l, ti):
            wl = win_d[l]
            t0 = ti * T
            dma("sp", cst_t.ap, cs_d[:, :, t0:t0 + T].rearrange("a p t -> p a t"), (), cst_t.pg(), "cs")
            cosv, sinv = cst_t.ap[:, 0, :], cst_t.ap[:, 1, :]
            qcnt = [0]

            def qk_consumer(which, scale):
                def f(h, b):
                    q_ = qf[qcnt[0] % 2]
                    t1 = t12[qcnt[0] % 2]
                    qcnt[0] += 1
                    act(q_.ap, PB(b), AF.Copy, PK(b), q_.pg(), scale=scale)
                    b2 = nb()
                    mm(PB(b2), Rm, q_.ap, True, True, cst.pg() + q_.pg(), PK(b2))
                    tt(t1.ap, PB(b2), sinv, ALU.mult, PK(b2) + cst_t.pg(), t1.pg())
                    tt(q_.ap, q_.ap, cosv, ALU.mult, q_.pg() + cst_t.pg(), q_.pg())
                    dst = which.ap[:, h, :]
                    tt(dst, q_.ap, t1.ap, ALU.add, q_.pg() + t1.pg(), which.pg(h * 1024, (h + 1) * 1024))
                return f

            proj_fm(wl, C_RQ, 4, qk_consumer(qTr, 1.0))
            proj_fm(wl, C_RK, 4, qk_consumer(kTr, 128.0 ** -0.5))
            for s2 in range(2):
                proj_tm(wl, C_RV + s2 * 512, 512,
                        lambda i, b, s2=s2: act(v_tok.ap[:, i, s2 * 512:(s2 + 1) * 512], PB(b), AF.Copy, PK(b), v_tok.pg()))
            for s2 in range(2):
                proj_tm(wl, C_RG + s2 * 512, 512,
                        lambda i, b, s2=s2: act(srg.ap[:, i, s2 * 512:(s2 + 1) * 512], PB(b), AF.Silu, PK(b), srg.pg()))
            for i in range(4):
                cs_ = slice(i * 128, (i + 1) * 128)
                bsd = nb()
                for h in range(4):
                    mm(PB(bsd)[:, h * 128:(h + 1) * 128], kTr.ap[:, h, cs_], qTr.ap[:, h, cs_], True, True, kTr.pg() + qTr.pg(), PK(bsd))
                tt(SDT.ap, PB(bsd).rearrange("p (h i) -> p h i", h=4), dmatT, ALU.mult, PK(bsd) + cst.pg(), SDT.pg())
                bk = nb()
                for h in range(4):
                    tr(PBb(bk)[:, h * 128:(h + 1) * 128], kTr.ap[:, h, cs_], identb, kTr.pg() + cbf.pg(), PK(bk))
                tt(kz.ap, PBb(bk)[:, 0:512].rearrange("p (h d) -> p h d", h=4), zeta.unsqueeze(2).broadcast_to([128, 4, 128]),
                   ALU.mult, PK(bk) + cst.pg(), kz.pg())
                tt(qx.ap, qTr.ap[:, :, cs_], xiT, ALU.mult, qTr.pg() + cst.pg(), qx.pg())
                bo = nb2()
                for h in range(4):
                    o_ = PB(bo, 2)[:, h * 256:(h + 1) * 256]
                    mm(o_, SDT.ap[:, h, :], v_tok.ap[:, i, h * 256:(h + 1) * 256], True, False, SDT.pg() + v_tok.pg(), PK(bo, 2))
                    mm(o_, qx.ap[:, h, :], rstb.ap[:, h * 256:(h + 1) * 256], False, True, qx.pg() + rstb.pg(), PK(bo, 2))
                bkv = nb2()
                for h in range(4):
                    mm(PB(bkv, 2)[:, h * 256:(h + 1) * 256], kz.ap[:, h, :], v_tok.ap[:, i, h * 256:(h + 1) * 256], True, True,
                       kz.pg() + v_tok.pg(), PK(bkv, 2))
                y0, y1 = yf
                act(y0.ap, PB(bo, 2), AF.Copy, PK(bo, 2), y0.pg())
                for h in range(4):
                    sl_ = slice(h * 256, (h + 1) * 256)
                    stt(rst.ap[:, sl_], rst.ap[:, sl_], math.exp(128.0 * LOGG[h]), PB(bkv, 2)[:, sl_], ALU.mult, ALU.add,
                        rst.pg() + PK(bkv, 2), rst.pg())
                cp(rstb.ap, rst.ap, rst.pg(), rstb.pg())
                for h in range(4):
                    S.add("dve", lambda e, h=h: e.bn_stats(out=bn6[:, h, :], in_=y0.ap[:, h * 256:(h + 1) * 256]), y0.pg(), smal2.pg())
                for h in range(4):
                    S.add("dve", lambda e, h=h: e.bn_aggr(out=mv[:, h, :], in_=bn6[:, h, :]), smal2.pg(), smal2.pg())
                act(rstd4, mv[:, :, 1], AF.Ln, smal2.pg(), smal2.pg(), bias=EPS, scale=1.0)
                act(rstd4, rstd4, AF.Exp, smal2.pg(), smal2.pg(), scale=-0.5)
                for h in range(4):
                    sl_ = slice(h * 256, (h + 1) * 256)
                    ts(y0.ap[:, sl_], y0.ap[:, sl_], mv[:, h, 0:1], rstd4[:, h:h + 1], ALU.subtract, ALU.mult,
                       y0.pg() + smal2.pg(), y0.pg())
                tt(ynb.ap, y0.ap, srg.ap[:, i, :], ALU.mult, y0.pg() + srg.pg(), ynb.pg())
                to_featmajor(i, l, 56)

        def att_tile(l, ti):
            wl = win_d[l]
            ms(qTm.ap, 0.0, qTm.pg())

            def q_consumer(c, b):
                act(qTm.ap[0:64, 2 * c, :], PB(b)[0:64, :], AF.Copy, PK(b), qTm.pg(2 * c * 1024, (2 * c + 2) * 1024), scale=0.125)
                act(qTm.ap[64:128, 2 * c + 1, :], PB(b)[64:128, :], AF.Copy, PK(b), qTm.pg(2 * c * 1024, (2 * c + 2) * 1024), scale=0.125)

            proj_fm(wl, C_AQ, 8, q_consumer)
            s = wring[0][wcnt[0] % len(wring[0])]
            wcnt[0] += 1
            for g in range(2):
                for dup in range(2):
                    wslab(wl, 0, 8, C_AK + g * 64, 64, dst_c0=g * 128 + dup * 64, slot=s)
            wslab(wl, 0, 8, C_AV, 128, dst_c0=256, slot=s)
            for g in range(2):
                b = nb()
                for k in range(8):
                    mm(PB(b), wsl[s].ap[:, k, g * 128:(g + 1) * 128], hT.ap[:, k, :], k == 0, k == 7, wsl[s].pg() + hT.pg(), PK(b))
                act(kTa.ap[:, g, 128:640], PB(b), AF.Copy, PK(b), kTa.pg())
            bv = nb()
            for i in range(4):
                for k in range(8):
                    mm(PB(bv)[:, i * 128:(i + 1) * 128], hT.ap[:, k, i * 128:(i + 1) * 128], wsl[s].ap[:, k, 256:384], k == 0, k == 7,
                       wsl[s].pg() + hT.pg(), PK(bv))
            vsrc = PB(bv).rearrange("p (i g d) -> p i g d", i=4, g=2)
            cp(vtok.ap[:, 1:5, :, 0:64], vsrc, PK(bv), vtok.pg())
            cp(vtok.ap[:, 1:5, :, 64:128], vsrc, PK(bv), vtok.pg())
            scnt = 0
            if stop < 4.3:
                return
            for i in range(4):
                ci = ti * 4 + i
                blks = [1] if ci == 0 else [0, 1]
                for blk in blks:
                    koff = i * 128 + blk * 128
                    for hg in range(4):
                        b = nb()
                        for hh in range(4):
                            h = hg * 4 + hh
                            g = h // 8
                            mm(PB(b)[:, hh * 128:(hh + 1) * 128], kTa.ap[:, g, koff:koff + 128], qTm.ap[:, h, i * 128:(i + 1) * 128],
                               True, True, kTa.pg() + qTm.pg(), PK(b))
                        stp = stmp[scnt % 2]
                        scnt += 1
                        tt(stp.ap.rearrange("p (h q) -> p h q", h=4), PB(b).rearrange("p (h q) -> p h q", h=4),
                           biasT.ap[:, blk, hg * 4:(hg + 1) * 4, :], ALU.add, PK(b) + biasT.pg(), stp.pg())
                        act(PT.ap[:, blk, hg * 4:(hg + 1) * 4, :], stp.ap.rearrange("p (h q) -> p h q", h=4), AF.Exp, stp.pg(), PT.pg())
                if stop < 4.5:
                    continue
                dsb = yf[0]
                for g in range(2):
                    bo = nb2()
                    bd = nb2()
                    for half in range(2):
                        hs = 8 * g + 4 * half
                        for n_, blk in enumerate(blks):
                            rhs_ = PTf.ap[:, blk, hs * 128:(hs + 4) * 128]
                            mm(PB(bo, 2)[:, half * 512:(half + 1) * 512], vtok.ap[:, i + blk, g, :], rhs_,
                               n_ == 0, n_ == len(blks) - 1, PT.pg() + vtok.pg(), PK(bo, 2))
                    if stop < 4.7:
                        continue
                    for half in range(2):
                        hs = 8 * g + 4 * half
                        for n_, blk in enumerate(blks):
                            rhs_ = PTf.ap[:, blk, hs * 128:(hs + 4) * 128]
                            mm(PB(bd, 2)[:, half * 512:(half + 1) * 512], cbf.ap[:, 256:384], rhs_,
                               n_ == 0, n_ == len(blks) - 1, PT.pg() + cbf.pg(), PK(bd, 2))
                    if stop < 4.9:
                        continue
                    tt(dsb.ap.rearrange("p (h q) -> p h q", h=8), PB(bd, 2).rearrange("p (h q) -> p h q", h=8),
                       esink[:, 8 * g:8 * g + 8].unsqueeze(2).broadcast_to([128, 8, 128]), ALU.add, PK(bd, 2) + smal.pg(), dsb.pg())
                    S.add("dve", lambda e: e.reciprocal(out=dsb.ap, in_=dsb.ap), dsb.pg(), dsb.pg())
                    o4 = PB(bo, 2).rearrange("p (j two q) -> p j two q", j=4, two=2)
                    d4 = dsb.ap.rearrange("p (j two q) -> p j two q", j=4, two=2)
                    qs = slice(i * 128, (i + 1) * 128)
                    tt(yT.ap[0:64, 4 * g:4 * g + 4, qs], o4[0:64, :, 0, :], d4[0:64, :, 0, :], ALU.mult, PK(bo, 2) + dsb.pg(), yT.pg())
                    tt(yT.ap[64:128, 4 * g:4 * g + 4, qs], o4[64:128, :, 1, :], d4[64:128, :, 1, :], ALU.mult, PK(bo, 2) + dsb.pg(), yT.pg())
            cp(vtok.ap[:, 0, :, :], vtok.ap[:, 4, :, :], vtok.pg(), vtok.pg())
            cp(kTa.ap[:, :, 0:128], kTa.ap[:, :, 512:640], kTa.pg(), kTa.pg())

        xin = yf[0]

        def load_tile(l, ti):
            t0 = ti * T
            if l == 0:
                for i in range(4):
                    dma("sp", xin.ap, x_d[t0 + i * 128:t0 + (i + 1) * 128, :], (), xin.pg(), "xin")
                    b = nb2()
                    for c in range(8):
                        tr(PB(b, 2)[:, c * 128:(c + 1) * 128], xin.ap[:, c * 128:(c + 1) * 128], identf, xin.pg() + cst.pg(), PK(b, 2))
                    cp(xt.ap[:, :, i * 128:(i + 1) * 128], PB(b, 2).rearrange("p (c t) -> p c t", c=8), PK(b, 2), xt.pg())
            else:
                dma("sp", xt.ap, xs_d[:, :, t0:t0 + T], [("xd", ti)], xt.pg(), "xt")

        def store_tile(l, ti):
            t0 = ti * T
            if l == L - 1:
                for i in range(4):
                    b = nb2()
                    for c in range(8):
                        tr(PB(b, 2)[:, c * 128:(c + 1) * 128], xt.ap[:, c, i * 128:(i + 1) * 128], identf, xt.pg() + cst.pg(), PK(b, 2))
                    cp(xin.ap, PB(b, 2), PK(b, 2), xin.pg())
                    dma("sp", out_d[t0 + i * 128:t0 + (i + 1) * 128, :], xin.ap, xin.pg(), [("od", ti, i)], "xout")
            else:
                dma("sp", xs_d[:, :, t0:t0 + T], xt.ap, xt.pg(), [("xd", ti)], "xst")

        for l in range(L):
            ms(sst.ap, 0.0, sst.pg())
            ms(sstb.ap, 0.0, sstb.pg())
            ms(rst.ap, 0.0, rst.pg())
            ms(rstb.ap, 0.0, rstb.pg())
            ms(hist.ap, 0.0, hist.pg())
            ms(kTa.ap, 0.0, kTa.pg())
            act(a_b, pvc(l, 164, 16), AF.Exp, pv.pg(), smal.pg())
            ts1(a_b, a_b, -1.0, ALU.mult, smal.pg(), smal.pg())
            act(esink, pvc(l, 196, 16), AF.Exp, pv.pg(), smal.pg())
            for ti in range(NT):
                load_tile(l, ti)
                if stop >= 1:
                    ffn(l, w1i_d, w1o_d, 0, 8)
                if stop >= 2:
                    pre_norm(l, 16)
                    ssd_tile(l, ti)
                    if dbg and l == 0:
                        dma("pool", dbg_d[0, :, :, ti * T:(ti + 1) * T], yT.ap, yT.pg(), [("dbg", 0, ti)], "dbg")
                if stop >= 3:
                    merge(l, 0)
                if stop >= 4:
                    ret_tile(l, ti)
                    if dbg and l == 0:
                        dma("pool", dbg_d[1, :, :, ti * T:(ti + 1) * T], yT.ap, yT.pg(), [("dbg", 1, ti)], "dbg")
                    merge(l, 1)
                if stop > 4.1:
                    att_tile(l, ti)
                    if dbg and l == 0:
                        dma("pool", dbg_d[2, :, :, ti * T:(ti + 1) * T], yT.ap, yT.pg(), [("dbg", 2, ti)], "dbg")
                    if stop >= 5:
                        merge(l, 2)
                if stop >= 6:
                    out_proj(l)
                if stop >= 7:
                    ffn(l, w2i_d, w2o_d, 32, 40)
                store_tile(l, ti)
        S.emit()
    return nc


def _t5_bucket(dist):
    is_small = dist < 16
    d = np.maximum(dist, 1).astype(np.float32)
    large = 16 + (np.log(d / 16) / math.log(128 / 16) * 16).astype(np.int32)
    large = np.minimum(large, 31)
    return np.where(is_small, dist, large)


def _consts(ntok):
    idx = np.arange(128)
    cst = np.zeros((128, NCST), np.float32)
    cst[:, 0:128] = np.eye(128)
    cst[:, 128:256] = (idx[:, None] <= idx[None, :])
    cst[:, 256:384] = 1.0
    cst[:, 384:512] = (idx[:, None] > idx[None, :])
    cst[:, 512:640] = (idx[None, :] >= idx[:, None])
    R = np.zeros((128, 128), np.float32)
    for dp in range(64):
        R[dp + 64, dp] = -1.0
        R[dp, dp + 64] = 1.0
    cst[:, 640:768] = R
    lg = np.array(LOGG, np.float64)
    diff = idx[None, :] - idx[:, None]
    dm = np.where(diff[None] >= 0, np.exp(np.maximum(diff, 0)[None] * lg[:, None, None]), 0.0)
    cst[:, 768:1280] = np.transpose(dm, (1, 0, 2)).reshape(128, 512)
    xi = np.exp((idx + 1.0)[None, :] * lg[:, None])
    cst[:, 1280:1792] = np.broadcast_to(xi.reshape(1, 512), (128, 512))
    cst[:, 1792:1796] = np.exp((127.0 - idx)[:, None] * lg[None, :])
    pos = np.arange(ntok, dtype=np.float32)
    inv = (1.0 / (np.float32(10000.0) ** (np.arange(0, 128, 2, dtype=np.float32) / np.float32(128)))).astype(np.float32)
    ang = pos[:, None] * inv[None, :]
    cos, sin = np.cos(ang).astype(np.float32).T, np.sin(ang).astype(np.float32).T
    cs = np.stack([np.concatenate([cos, cos], 0), np.concatenate([sin, sin], 0)], 0).astype(np.float32)
    return cst, np.ascontiguousarray(cs)


def _bias_table(rel_bias):
    qi = np.arange(128)[:, None]
    sj = np.arange(256)[None, :]
    dist = qi + 128 - sj
    mask = (dist >= 0) & (dist < 128)
    bb = rel_bias[_t5_bucket(np.maximum(dist, 0))]
    bb = np.where(mask[:, :, None], bb, np.float32(-1e30)).astype(np.float32)
    t = bb.reshape(128, 2, 128, 16)
    t = np.transpose(t, (2, 1, 3, 0))
    return np.ascontiguousarray(t.reshape(128, 2 * 16 * 128))


def _pack_params(L, p):
    fm = lambda v, n: v.reshape(n, 128).T
    pv = np.zeros((128, L, NV), np.float32)
    for l in range(L):
        for j, nm in enumerate(["ffn1_pre_g", "ffn1_post_g", "mix_pre_g", "mix_post_g", "ffn2_pre_g", "ffn2_post_g",
                                "ssd_norm_g", "ret_gn_g"]):
            pv[:, l, j * 8:(j + 1) * 8] = fm(p[nm][l], 8)
        cw = p["conv_w"][l]
        pv[:, l, 64:112] = np.transpose(cw.reshape(4, 12, 128), (2, 1, 0)).reshape(128, 48)
        pv[:, l, 112:124] = fm(p["conv_b"][l], 12)
        pv[:, l, 124:148] = fm(p["b_gate"][l], 24)
        pv[:, l, 148:164] = p["dt_bias"][l][None, :]
        pv[:, l, 164:180] = p["a_log"][l][None, :]
        pv[:, l, 180:196] = p["d_skip"][l][None, :]
        pv[:, l, 196:212] = p["attn_sinks"][l][None, :]
    return np.ascontiguousarray(pv.reshape(128, L * NV))


_NC_CACHE = {}


def run(inputs, L=DEPTH, ntok=SEQ, ncores=8, dbg=False, stop=99):
    p = {k: np.asarray(v, dtype=np.float32) for k, v in inputs.items()}
    key = (L, ntok, dbg, stop)
    if key not in _NC_CACHE:
        _NC_CACHE[key] = build(L, ntok, dbg, stop)
    nc = _NC_CACHE[key]
    cst, cs = _consts(ntok)
    shared = {
        "w1i": np.ascontiguousarray(p["w_ffn1_in"][:L]), "w1o": np.ascontiguousarray(p["w_ffn1_out"][:L]),
        "w2i": np.ascontiguousarray(p["w_ffn2_in"][:L]), "w2o": np.ascontiguousarray(p["w_ffn2_out"][:L]),
        "win": np.ascontiguousarray(p["w_in"][:L]), "wbr": np.ascontiguousarray(p["w_branch"][:L]),
        "wo": np.ascontiguousarray(p["w_out"][:L]),
        "pv": _pack_params(L, p), "cst": cst, "cs": cs, "biasT": _bias_table(p["rel_bias"]),
    }
    in_maps = []
    for b in range(ncores):
        m = dict(shared)
        m["x"] = np.ascontiguousarray(p["x"][b, :ntok])
        in_maps.append(m)
    res = run_bass_kernel_spmd(nc, in_maps, core_ids=list(range(ncores)))
    return res


def kernel(**inputs):
    res = run(inputs)
    return np.stack([np.asarray(r["out"], dtype=np.float32) for r in res.results], axis=0)
```

```python
import math
from contextlib import ExitStack
import numpy as np
import concourse.bass as bass
import concourse.mybir as mybir
from concourse.bass_utils import run_bass_kernel_spmd

F32 = mybir.dt.float32
BF16 = mybir.dt.bfloat16
AF = mybir.ActivationFunctionType
ALU = mybir.AluOpType

D = 1024
DFF = 2816
INW = 10000
DEPTH = 4
SEQ = 2048
T = 512
EPS = 1e-6
NV = 212
NCST = 1796
ENGS = ("pe", "act", "dve", "pool", "sp")
C_Z, C_XBC, C_DT, C_RQ, C_RK, C_RV, C_RG, C_AQ, C_AK, C_AV, C_GATE = 0, 1024, 2560, 2576, 3088, 3600, 4624, 5648, 6672, 6800, 6928
LOGG = [math.log(1.0 - 2.0 ** (-5.0 - h)) for h in range(4)]


class Op:
    __slots__ = ("eng", "fn", "reads", "writes", "dma", "waits", "signal", "semkey", "deps", "sigval", "idx", "need")

    def __init__(self, eng, fn, reads, writes, dma):
        self.eng, self.fn, self.reads, self.writes, self.dma = eng, fn, reads, writes, dma
        self.waits = {}
        self.signal = False
        self.semkey = None


class Sched:
    def __init__(self, nc):
        self.nc = nc
        self.ops = []
        self.last_writer = {}
        self.readers = {}

    def add(self, eng, fn, reads=(), writes=(), dma=None):
        op = Op(eng, fn, tuple(reads), tuple(writes), dma)
        deps = set()
        for r in op.reads:
            w = self.last_writer.get(r)
            if w is not None:
                deps.add(w)
        for r in op.writes:
            w = self.last_writer.get(r)
            if w is not None:
                deps.add(w)
            for rd in self.readers.get(r, ()):
                deps.add(rd)
        op.deps = deps
        for r in op.reads:
            self.readers.setdefault(r, []).append(op)
        for r in op.writes:
            self.last_writer[r] = op
            self.readers[r] = []
        self.ops.append(op)
        return op

    @staticmethod
    def _skip(d, op):
        return d is op or (d.dma is None and op.dma is None and d.eng == "pe" and op.eng == "pe")

    def finalize(self):
        counters = {}
        for i, op in enumerate(self.ops):
            op.idx = i
        for op in self.ops:
            best = {}
            for d in op.deps:
                if self._skip(d, op):
                    continue
                k = ("dma", d.dma) if d.dma is not None else ("eng", d.eng)
                if k not in best or d.idx > best[k].idx:
                    best[k] = d
            op.need = list(best.values())
            for d in op.need:
                d.signal = True
        for op in self.ops:
            if op.dma is not None:
                op.semkey = ("dma", op.dma)
                op.signal = True
            else:
                op.semkey = ("eng", op.eng)
            if op.signal:
                counters[op.semkey] = counters.get(op.semkey, 0) + (16 if op.dma is not None else 1)
                op.sigval = counters[op.semkey]
        seen = {e: {} for e in ENGS}
        for op in self.ops:
            for d in op.need:
                k, v = d.semkey, d.sigval
                if seen[op.eng].get(k, 0) >= v:
                    continue
                if op.waits.get(k, 0) < v:
                    op.waits[k] = v
            for k, v in op.waits.items():
                seen[op.eng][k] = v
        self.semkeys = list(counters.keys())
        self.final_counts = counters

    def emit(self):
        nc = self.nc
        self.finalize()
        with ExitStack() as st:
            sems = {}
            for i, k in enumerate(self.semkeys):
                sems[k] = st.enter_context(nc.semaphore("s%d" % i))
            block = st.enter_context(nc.Block())
            per = {e: [op for op in self.ops if op.eng == e] for e in ENGS}

            def run(e, ops):
                for op in ops:
                    for k, v in op.waits.items():
                        e.wait_ge(sems[k], v)
                    ins = op.fn(e)
                    if op.signal:
                        ins.then_inc(sems[op.semkey], 16 if op.dma is not None else 1)

            @block.tensor
            def _(e):
                run(e, per["pe"])

            @block.scalar
            def _(e):
                run(e, per["act"])

            @block.vector
            def _(e):
                run(e, per["dve"])

            @block.gpsimd
            def _(e):
                run(e, per["pool"])

            @block.sync
            def _(e):
                run(e, per["sp"])
                for k in self.semkeys:
                    e.wait_ge(sems[k], self.final_counts[k])


class Buf:
    def __init__(self, mem, off, nbytes, dtype, shape=None):
        self.off, self.nbytes = off, nbytes
        if shape is not None:
            n = 1
            for d_ in shape:
                n *= d_
            assert n * (4 if dtype == F32 else 2) == nbytes, (shape, nbytes)
        v = mem[:, off // 2:(off + nbytes) // 2]
        if dtype == F32:
            v = v.bitcast(F32)
        if shape is not None and len(shape) == 2:
            v = v.rearrange("p (a b) -> p a b", a=shape[0])
        elif shape is not None and len(shape) == 3:
            v = v.rearrange("p (a b c) -> p a b c", a=shape[0], b=shape[1])
        self.ap = v

    def pg(self, lo=0, hi=None):
        hi = self.nbytes if hi is None else hi
        return [("pg", i) for i in range((self.off + lo) // 1024, (self.off + hi - 1) // 1024 + 1)]


def build(L=DEPTH, NTOK=SEQ, dbg=False, stop=99):
    NT = NTOK // T
    nc = bass.Bass("TRN2", target_bir_lowering=False)
    dr = lambda n, s, k="ExternalInput": nc.dram_tensor(n, s, F32, kind=k).ap()
    x_d = dr("x", [NTOK, D])
    w1i_d, w1o_d = dr("w1i", [L, D, 2 * DFF]), dr("w1o", [L, DFF, D])
    w2i_d, w2o_d = dr("w2i", [L, D, 2 * DFF]), dr("w2o", [L, DFF, D])
    win_d, wbr_d, wo_d = dr("win", [L, D, INW]), dr("wbr", [L, 3, D, D]), dr("wo", [L, D, D])
    pv_d, cst_d = dr("pv", [128, L * NV]), dr("cst", [128, NCST])
    cs_d = dr("cs", [2, 128, NTOK])
    bias_d = dr("biasT", [128, 2 * 16 * 128])
    out_d = dr("out", [NTOK, D], "ExternalOutput")
    xs_d = dr("xscr", [128, 8, NTOK], "Internal")
    dbg_d = dr("dbgo", [3, 128, 8, NTOK], "ExternalOutput") if dbg else None

    with ExitStack() as st:
        TOTAL = 206 * 1024
        mem = st.enter_context(nc.sbuf_tensor("mem", [128, TOTAL // 2], BF16))
        ps = st.enter_context(nc.psum_tensor("ps", [128, 4096], F32))
        S = Sched(nc)
        cur = [0]

        def alloc(nbytes, dtype, shape=None):
            nb_ = (nbytes + 1023) // 1024 * 1024
            b = Buf(mem, cur[0], nbytes, dtype, shape)
            cur[0] += nb_
            assert cur[0] <= TOTAL, cur[0]
            return b

        cst = alloc(NCST * 4, F32)
        pv = alloc(L * NV * 4, F32)
        cbf = alloc(3 * 128 * 2, BF16)
        biasT = alloc(2 * 16 * 128 * 2, BF16, (2, 16, 128))
        xt = alloc(8 * T * 4, F32, (8, T))
        hT = alloc(8 * T * 2, BF16, (8, T))
        ytmp = alloc(8 * T * 4, F32, (8, T))
        rs = alloc(T * 4, F32)
        wsl = [alloc(8 * 512 * 2, BF16, (8, 512)) for _ in range(3)]
        ysum = alloc(8 * T * 4, F32, (8, T))
        yT = alloc(8 * T * 2, BF16, (8, T))
        etmp = [alloc(T * 4, F32) for _ in range(2)]
        sst = alloc(1024 * 4, F32)
        sstb = alloc(1024 * 2, BF16)
        rst = alloc(1024 * 4, F32)
        rstb = alloc(1024 * 2, BF16)
        hist = alloc(12 * 3 * 4, F32, (12, 3))
        kTa = alloc(2 * 640 * 2, BF16, (2, 640))
        vtok = alloc(5 * 2 * 128 * 2, BF16, (5, 2, 128))
        smal = alloc(1024, F32)
        smal2 = alloc(1024, F32)
        A0 = cur[0]

        def arena(off, nbytes, dtype, shape=None):
            assert A0 + off + nbytes <= TOTAL, (A0, off, nbytes)
            return Buf(mem, A0 + off, nbytes, dtype, shape)

        K = 1024
        sq = arena(0, 8 * K, BF16, (8, T))
        hid = arena(8 * K, 22 * K, BF16, (22, T))
        wsl.extend(arena(30 * K + i * 8 * K, 8 * K, BF16, (8, 512)) for i in range(4))
        wring = [[0, 1, 2]]
        ynb = arena(0, 2 * K, BF16)
        yf = [arena(2 * K + i * 4 * K, 4 * K, F32) for i in range(2)]
        MX = 10 * K
        sz = arena(MX, 8 * K, BF16, (4, 1024))
        xs_tok = arena(MX + 8 * K, 8 * K, BF16, (4, 1024))
        B_tok = arena(MX + 16 * K, 2 * K, BF16, (4, 256))
        bcT = arena(MX + 18 * K, 4 * K, BF16, (4, T))
        raw = [arena(MX + 22 * K + i * 3 * K, 515 * 4, F32) for i in range(2)]
        cacc = [arena(MX + 28 * K + i * 2 * K, 2 * K, F32) for i in range(2)]
        xsTt = [arena(MX + 32 * K + i * K, K, BF16) for i in range(2)]
        xdt = arena(MX + 34 * K, 2 * K, BF16)
        xdd = arena(MX + 36 * K, 2 * K, BF16)
        rhsm = arena(MX + 38 * K, 8 * K, F32)
        Eb = arena(MX + 46 * K, 4 * K, BF16, (16, 128))
        MTb = arena(MX + 50 * K, 4 * K, BF16, (16, 128))
        cbm = arena(MX + 54 * K, 512, BF16, (2, 128))
        qf = [arena(MX + i * 2 * K, 2 * K, F32) for i in range(2)]
        t12 = [arena(MX + 4 * K + i * 2 * K, 2 * K, F32) for i in range(2)]
        qTr = arena(MX + 8 * K, 4 * K, BF16, (4, T))
        kTr = arena(MX + 12 * K, 4 * K, BF16, (4, T))
        v_tok = arena(MX + 16 * K, 8 * K, BF16, (4, 1024))
        srg = arena(MX + 24 * K, 8 * K, BF16, (4, 1024))
        SDT = arena(MX + 32 * K, K, BF16, (4, 128))
        kz = arena(MX + 33 * K, K, BF16, (4, 128))
        qx = arena(MX + 34 * K, K, BF16, (4, 128))
        cst_t = arena(MX + 35 * K, 4 * K, F32, (2, T))
        qTm = arena(MX, 16 * K, BF16, (16, T))
        stmp = [arena(MX + 16 * K + i * 2 * K, 2 * K, F32) for i in range(2)]
        PT = arena(MX + 20 * K, 8 * K, BF16, (2, 16, 128))
        PTf = arena(MX + 20 * K, 8 * K, BF16, (2, 2048))

        identf = cst.ap[:, 0:128]
        triu = cst.ap[:, 128:256]
        onesf = cst.ap[:, 256:384]
        slow = cst.ap[:, 384:512]
        causT = cst.ap[:, 512:640]
        Rm = cst.ap[:, 640:768]
        dmatT = cst.ap[:, 768:1280].rearrange("p (h i) -> p h i", h=4)
        xiT = cst.ap[:, 1280:1792].rearrange("p (h i) -> p h i", h=4)
        zeta = cst.ap[:, 1792:1796]
        identb = cbf.ap[:, 0:128]
        onesb = cbf.ap[:, 128:256]
        one1b = cbf.ap[:, 256:257]

        pbank = [0]

        def nb():
            b = pbank[0] % 8
            pbank[0] += 1
            return b

        def nb2():
            if pbank[0] % 2:
                pbank[0] += 1
            b = pbank[0] % 8
            pbank[0] += 2
            return b

        def nb4():
            while pbank[0] % 4:
                pbank[0] += 1
            b = pbank[0] % 8
            pbank[0] += 4
            return b

        PB = lambda b, n=1: ps[:, b * 512:(b + n) * 512]
        PBb = lambda b: ps[:, b * 512:(b + 1) * 512].bitcast(BF16)
        PK = lambda b, n=1: [("ps", b + i) for i in range(n)]

        def mm(out, lhsT, rhs, start, stop, r, w):
            S.add("pe", lambda e: e.matmul(out, lhsT=lhsT, rhs=rhs, start=start, stop=stop), r, w)

        def tr(out, in_, ident, r, w):
            S.add("pe", lambda e: e.transpose(out, in_, ident), r, w)

        def act(out, in_, func, r, w, **kw):
            S.add("act", lambda e: e.activation(out=out, in_=in_, func=func, **kw), r, w)

        def tt(out, in0, in1, op, r, w, eng="dve"):
            S.add(eng, lambda e: e.tensor_tensor(out=out, in0=in0, in1=in1, op=op), r, w)

        def stt(out, in0, scalar, in1, op0, op1, r, w):
            S.add("dve", lambda e: e.scalar_tensor_tensor(out=out, in0=in0, scalar=scalar, in1=in1, op0=op0, op1=op1), r, w)

        def ts(out, in0, s1, s2, op0, op1, r, w):
            S.add("dve", lambda e: e.tensor_scalar(out=out, in0=in0, scalar1=s1, scalar2=s2, op0=op0, op1=op1), r, w)

        def ts1(out, in0, s1, op, r, w):
            S.add("dve", lambda e: e.tensor_single_scalar(out=out, in_=in0, scalar=s1, op=op), r, w)

        def cp(out, in_, r, w, eng="dve"):
            S.add(eng, lambda e: e.tensor_copy(out=out, in_=in_), r, w)

        def ms(ap, val, w, eng="dve"):
            S.add(eng, lambda e: e.memset(ap, val), (), w)

        def dma(eng, out, in_, r, w, key):
            S.add(eng, lambda e: e.dma_start(out=out, in_=in_), r, w, dma=key)

        wcnt = [0]

        def wslab(w2d, k0, nk, c0, ncols, dst_c0=0, slot=None):
            if slot is None:
                slot = wring[0][wcnt[0] % len(wring[0])]
                wcnt[0] += 1
            src = w2d[k0 * 128:(k0 + nk) * 128, c0:c0 + ncols].rearrange("(k p) n -> p k n", p=128)
            dst = wsl[slot].ap[:, 0:nk, dst_c0:dst_c0 + ncols]
            dma("pool", dst, src, (), wsl[slot].pg(), ("w", slot))
            return slot

        dma("sp", cst.ap, cst_d, (), cst.pg(), "cst")
        dma("sp", pv.ap, pv_d, (), pv.pg(), "pv")
        dma("pool", biasT.ap, bias_d.rearrange("p (b h q) -> p b h q", b=2, h=16), (), biasT.pg(), "bias")
        cp(identb, identf, cst.pg(), cbf.pg())
        ts1(onesb, onesf, 1.0 / 1024.0, ALU.mult, cst.pg(), cbf.pg())
        cp(cbf.ap[:, 256:384], onesf, cst.pg(), cbf.pg())

        def pvc(l, c, n=1):
            return pv.ap[:, l * NV + c:l * NV + c + n]

        def stats_from_sq():
            b = nb()
            for c in range(8):
                mm(PB(b), onesb, sq.ap[:, c, :], c == 0, c == 7, sq.pg(c * 1024, (c + 1) * 1024) + cbf.pg(), PK(b))
            act(rs.ap, PB(b), AF.Ln, PK(b), rs.pg(), bias=EPS, scale=1.0)
            act(rs.ap, rs.ap, AF.Exp, rs.pg(), rs.pg(), scale=-0.5)

        def pre_norm(l, gcol):
            for c in range(8):
                act(sq.ap[:, c, :], xt.ap[:, c, :], AF.Square, xt.pg(c * 2048, (c + 1) * 2048), sq.pg(c * 1024, (c + 1) * 1024))
            stats_from_sq()
            for c in range(8):
                stt(hT.ap[:, c, :], xt.ap[:, c, :], pvc(l, gcol + c), rs.ap, ALU.mult, ALU.mult,
                    xt.pg(c * 2048, (c + 1) * 2048) + pv.pg() + rs.pg(), hT.pg(c * 1024, (c + 1) * 1024))

        def evac_y(c, bank):
            yk = ytmp.pg(c * 2048, (c + 1) * 2048)
            act(ytmp.ap[:, c, :], PB(bank), AF.Copy, PK(bank), yk)
            act(sq.ap[:, c, :], ytmp.ap[:, c, :], AF.Square, yk, sq.pg(c * 1024, (c + 1) * 1024))

        def post_norm_add(l, gcol, half):
            stats_from_sq()
            for c in range(8):
                yk = ytmp.pg(c * 2048, (c + 1) * 2048)
                xk = xt.pg(c * 2048, (c + 1) * 2048)
                stt(ytmp.ap[:, c, :], ytmp.ap[:, c, :], pvc(l, gcol + c), rs.ap, ALU.mult, ALU.mult, yk + pv.pg() + rs.pg(), yk)
                stt(xt.ap[:, c, :], ytmp.ap[:, c, :], half, xt.ap[:, c, :], ALU.mult, ALU.add, yk + xk, xk)

        def ffn(l, wi_d, wo_d_, gpre, gpost):
            pre_norm(l, gpre)
            wring[0] = [0, 1, 2, 3, 4, 5, 6]
            wi = wi_d[l]
            wo_ = wo_d_[l]
            j = 0
            ecnt = 0
            for g0 in range(0, DFF, 512):
                gw = min(512, DFF - g0)
                sa = wslab(wi, 0, 8, g0, gw)
                sb = wslab(wi, 0, 8, DFF + g0, gw)
                for jj in range(gw // 128):
                    ba, bb = nb(), nb()
                    for k in range(8):
                        mm(PB(ba), wsl[sa].ap[:, k, jj * 128:(jj + 1) * 128], hT.ap[:, k, :], k == 0, k == 7,
                           wsl[sa].pg() + hT.pg(), PK(ba))
                    for k in range(8):
                        mm(PB(bb), wsl[sb].ap[:, k, jj * 128:(jj + 1) * 128], hT.ap[:, k, :], k == 0, k == 7,
                           wsl[sb].pg() + hT.pg(), PK(bb))
                    et = etmp[ecnt % 2]
                    ecnt += 1
                    act(et.ap, PB(ba), AF.Silu, PK(ba), et.pg())
                    tt(hid.ap[:, j, :], et.ap, PB(bb), ALU.mult, et.pg() + PK(bb), hid.pg(j * 1024, (j + 1) * 1024))
                    j += 1
            for mg in range(2):
                b0 = nb4()
                for kg in range(3):
                    k0 = kg * 8
                    nk = min(8, 22 - k0)
                    s = wslab(wo_, k0, nk, mg * 512, 512)
                    for m in range(4):
                        for k in range(nk):
                            mm(PB(b0 + m), wsl[s].ap[:, k, m * 128:(m + 1) * 128], hid.ap[:, k0 + k, :],
                               kg == 0 and k == 0, kg == 2 and k == nk - 1,
                               wsl[s].pg() + hid.pg((k0 + k) * 1024, (k0 + k + 1) * 1024), PK(b0 + m))
                for m in range(4):
                    c = mg * 4 + m
                    evac_y(c, b0 + m)
            wring[0] = [0, 1, 2]
            post_norm_add(l, gpost, 0.5)

        def proj_fm(wl, col0, nch, consumer, dup=None):
            ci = 0
            while ci < nch:
                n = min(4, nch - ci)
                s = wslab(wl, 0, 8, col0 + ci * 128, n * 128)
                for jj in range(n):
                    b = nb()
                    for k in range(8):
                        mm(PB(b), wsl[s].ap[:, k, jj * 128:(jj + 1) * 128], hT.ap[:, k, :], k == 0, k == 7,
                           wsl[s].pg() + hT.pg(), PK(b))
                    consumer(ci + jj, b)
                ci += n

        def proj_tm(wl, col0, ncols, consumer):
            s = wslab(wl, 0, 8, col0, ncols)
            for i in range(4):
                b = nb()
                for k in range(8):
                    mm(PB(b)[:, 0:ncols], hT.ap[:, k, i * 128:(i + 1) * 128], wsl[s].ap[:, k, 0:ncols], k == 0, k == 7,
                       wsl[s].pg() + hT.pg(), PK(b))
                consumer(i, b)

        def to_featmajor(i, l, gcol):
            b = nb()
            for c in range(8):
                tr(PBb(b)[:, c * 128:(c + 1) * 128], ynb.ap[:, c * 128:(c + 1) * 128], identb, ynb.pg() + cbf.pg(), PK(b))
            src = PBb(b).rearrange("p (c t) -> p c t", c=8)
            dst = yT.ap[:, :, i * 128:(i + 1) * 128]
            if gcol is None:
                cp(dst, src, PK(b), yT.pg())
            else:
                tt(dst, src, pvc(l, gcol, 8).unsqueeze(2).broadcast_to([128, 8, 128]), ALU.mult, PK(b) + pv.pg(), yT.pg())

        def merge(l, m):
            wl = win_d[l]
            wb = wbr_d[l, m]
            ecnt = 0
            for half in range(2):
                sg = wslab(wl, 0, 8, C_GATE + m * 1024 + half * 512, 512)
                sbm = wslab(wb, 0, 8, half * 512, 512)
                for jj in range(4):
                    c = half * 4 + jj
                    bg, bb = nb(), nb()
                    for k in range(8):
                        mm(PB(bg), wsl[sg].ap[:, k, jj * 128:(jj + 1) * 128], hT.ap[:, k, :], k == 0, k == 7,
                           wsl[sg].pg() + hT.pg(), PK(bg))
                    for k in range(8):
                        mm(PB(bb), wsl[sbm].ap[:, k, jj * 128:(jj + 1) * 128], yT.ap[:, k, :], k == 0, k == 7,
                           wsl[sbm].pg() + yT.pg(), PK(bb))
                    et = etmp[ecnt % 2]
                    ecnt += 1
                    act(et.ap, PB(bg), AF.Sigmoid, PK(bg) + pv.pg(), et.pg(), bias=pvc(l, 124 + m * 8 + c), scale=1.0)
                    ysc = ysum.ap[:, c, :]
                    yk = ysum.pg(c * 2048, (c + 1) * 2048)
                    if m == 0:
                        tt(ysc, et.ap, PB(bb), ALU.mult, et.pg() + PK(bb), yk)
                    else:
                        tt(et.ap, et.ap, PB(bb), ALU.mult, et.pg() + PK(bb), et.pg())
                        tt(ysc, ysc, et.ap, ALU.add, et.pg() + yk, yk)

        def out_proj(l):
            cp(hT.ap, ysum.ap, ysum.pg(), hT.pg())
            for half in range(2):
                s = wslab(wo_d[l], 0, 8, half * 512, 512)
                for jj in range(4):
                    c = half * 4 + jj
                    b = nb()
                    for k in range(8):
                        mm(PB(b), wsl[s].ap[:, k, jj * 128:(jj + 1) * 128], hT.ap[:, k, :], k == 0, k == 7,
                           wsl[s].pg() + hT.pg(), PK(b))
                    evac_y(c, b)
            post_norm_add(l, 24, 1.0)

        a_b = smal.ap[:, 0:16]
        esink = smal.ap[:, 16:32]
        dtb = smal.ap[:, 32:96].rearrange("p (i h) -> p i h", i=4)
        dab = smal.ap[:, 96:160].rearrange("p (i h) -> p i h", i=4)
        acs = smal2.ap[:, 0:32]
        dec_out = smal2.ap[:, 32:48]
        dec_st = smal2.ap[:, 48:64]
        cdec = smal2.ap[:, 64:80]
        ssq = smal2.ap[:, 80:81]
        bn6 = smal2.ap[:, 96:120].rearrange("p (h s) -> p h s", h=4)
        mv = smal2.ap[:, 120:128].rearrange("p (h s) -> p h s", h=4)
        rstd4 = smal2.ap[:, 128:132]
        den = smal2.ap[:, 136:152]

        def ssd_tile(l, ti):
            wl = win_d[l]
            for s2 in range(2):
                proj_tm(wl, C_Z + s2 * 512, 512,
                        lambda i, b, s2=s2: act(sz.ap[:, i, s2 * 512:(s2 + 1) * 512], PB(b), AF.Silu, PK(b), sz.pg()))
            sl = wslab(wl, 0, 8, C_DT, 16)
            bd = nb()
            for i in range(4):
                for k in range(8):
                    mm(PB(bd)[:, i * 16:(i + 1) * 16], hT.ap[:, k, i * 128:(i + 1) * 128], wsl[sl].ap[:, k, 0:16], k == 0, k == 7,
                       wsl[sl].pg() + hT.pg(), PK(bd))
            tt(dtb, PB(bd)[:, 0:64].rearrange("p (i h) -> p i h", i=4), pvc(l, 148, 16).unsqueeze(1).broadcast_to([128, 4, 16]),
               ALU.add, PK(bd) + pv.pg(), smal.pg())
            act(dtb, dtb, AF.Exp, smal.pg(), smal.pg())
            act(dtb, dtb, AF.Ln, smal.pg(), smal.pg(), bias=1.0, scale=1.0)
            tt(dab, dtb, a_b.unsqueeze(1).broadcast_to([128, 4, 16]), ALU.mult, smal.pg(), smal.pg())
            ccnt = [0]

            def xbc_consumer(c, b):
                rw = raw[ccnt[0] % 2]
                ca = cacc[ccnt[0] % 2]
                xtt = xsTt[ccnt[0] % 2]
                ccnt[0] += 1
                cp(rw.ap[:, 0:3], hist.ap[:, c, :], hist.pg(), rw.pg())
                act(rw.ap[:, 3:515], PB(b), AF.Copy, PK(b), rw.pg())
                cp(hist.ap[:, c, :], rw.ap[:, 512:515], rw.pg(), hist.pg())
                wc = lambda k: pvc(l, 64 + c * 4 + k)
                ts1(ca.ap, rw.ap[:, 0:512], wc(0), ALU.mult, rw.pg() + pv.pg(), ca.pg())
                for k in range(1, 4):
                    stt(ca.ap, rw.ap[:, k:k + 512], wc(k), ca.ap, ALU.mult, ALU.add, rw.pg() + pv.pg() + ca.pg(), ca.pg())
                if c < 8:
                    act(xtt.ap, ca.ap, AF.Silu, ca.pg() + pv.pg(), xtt.pg(), bias=pvc(l, 112 + c), scale=1.0)
                    dst_src = xtt
                else:
                    act(bcT.ap[:, c - 8, :], ca.ap, AF.Silu, ca.pg() + pv.pg(), bcT.pg(), bias=pvc(l, 112 + c), scale=1.0)
                if c < 10:
                    bt = nb()
                    for i in range(4):
                        src = xtt.ap[:, i * 128:(i + 1) * 128] if c < 8 else bcT.ap[:, c - 8, i * 128:(i + 1) * 128]
                        tr(PBb(bt)[:, i * 128:(i + 1) * 128], src, identb, (xtt.pg() if c < 8 else bcT.pg()) + cbf.pg(), PK(bt))
                    srcv = PBb(bt)[:, 0:512].rearrange("p (i f) -> p i f", i=4)
                    if c < 8:
                        cp(xs_tok.ap[:, :, c * 128:(c + 1) * 128], srcv, PK(bt), xs_tok.pg())
                    else:
                        cp(B_tok.ap[:, :, (c - 8) * 128:(c - 7) * 128], srcv, PK(bt), B_tok.pg())

            proj_fm(wl, C_XBC, 12, xbc_consumer)
            for i in range(4):
                cs_ = slice(i * 128, (i + 1) * 128)
                bs = nb()
                mm(PB(bs)[:, 0:16], triu, dab[:, i, :], True, True, cst.pg() + smal.pg(), PK(bs))
                mm(PB(bs)[:, 16:32], onesf, dab[:, i, :], True, True, cst.pg() + smal.pg(), PK(bs))
                cp(acs, PB(bs)[:, 0:32], PK(bs), smal2.pg())
                act(dec_out, acs[:, 0:16], AF.Exp, smal2.pg(), smal2.pg())
                tt(dec_st, acs[:, 16:32], acs[:, 0:16], ALU.subtract, smal2.pg(), smal2.pg())
                act(dec_st, dec_st, AF.Exp, smal2.pg(), smal2.pg())
                act(cdec, acs[:, 16:32], AF.Exp, smal2.pg(), smal2.pg())
                xs3 = xs_tok.ap[:, i, :].rearrange("p (h d) -> p h d", h=16)
                v3 = lambda b_: b_.ap.rearrange("p (h d) -> p h d", h=16)
                bc16 = lambda a_: a_.unsqueeze(2).broadcast_to([128, 16, 64])
                tt(v3(xdt), xs3, bc16(dtb[:, i, :]), ALU.mult, xs_tok.pg() + smal.pg(), xdt.pg())
                tt(v3(xdd), v3(xdt), bc16(dec_st), ALU.mult, xdt.pg() + smal2.pg(), xdd.pg())
                tt(rhsm.ap.rearrange("p (h l) -> p h l", h=16), triu.unsqueeze(1).broadcast_to([128, 16, 128]),
                   dab[:, i, :].unsqueeze(2).broadcast_to([128, 16, 128]), ALU.mult, cst.pg() + smal.pg(), rhsm.pg())
                b4 = nb4()
                for q in range(4):
                    mm(PB(b4 + q), slow, rhsm.ap[:, q * 512:(q + 1) * 512], True, True, cst.pg() + rhsm.pg(), PK(b4 + q))
                for q in range(4):
                    act(Eb.ap[:, q * 4:(q + 1) * 4, :], PB(b4 + q).rearrange("p (h l) -> p h l", h=4), AF.Exp, PK(b4 + q), Eb.pg())
                bcb = nb()
                for g in range(2):
                    mm(PB(bcb)[:, g * 128:(g + 1) * 128], bcT.ap[:, g, cs_], bcT.ap[:, 2 + g, cs_], True, True, bcT.pg(), PK(bcb))
                tt(cbm.ap, PB(bcb)[:, 0:256].rearrange("p (g l) -> p g l", g=2), causT.unsqueeze(1).broadcast_to([128, 2, 128]),
                   ALU.mult, PK(bcb) + cst.pg(), cbm.pg())
                for g in range(2):
                    tt(MTb.ap[:, 8 * g:8 * g + 8, :], Eb.ap[:, 8 * g:8 * g + 8, :],
                       cbm.ap[:, g, :].unsqueeze(1).broadcast_to([128, 8, 128]), ALU.mult, Eb.pg() + cbm.pg(), MTb.pg())
                bo = nb2()
                for g in range(2):
                    mm(PB(bo, 2)[:, g * 512:(g + 1) * 512], bcT.ap[:, 2 + g, cs_], sstb.ap[:, g * 512:(g + 1) * 512], True, True,
                       bcT.pg() + sstb.pg(), PK(bo, 2))
                by = nb2()
                for h in range(16):
                    mm(PB(by, 2)[:, h * 64:(h + 1) * 64], MTb.ap[:, h, :], xdt.ap[:, h * 64:(h + 1) * 64], True, True,
                       MTb.pg() + xdt.pg(), PK(by, 2))
                y0, y1 = yf
                tt(v3(y0), PB(bo, 2).rearrange("p (h d) -> p h d", h=16), bc16(dec_out), ALU.mult, PK(bo, 2) + smal2.pg(), y0.pg())
                tt(y0.ap, PB(by, 2), y0.ap, ALU.add, PK(by, 2) + y0.pg(), y0.pg())
                tt(v3(y1), xs3, bc16(pvc(l, 180, 16)), ALU.mult, xs_tok.pg() + pv.pg(), y1.pg())
                tt(y0.ap, y0.ap, y1.ap, ALU.add, y0.pg() + y1.pg(), y0.pg())
                bst = nb2()
                for g in range(2):
                    mm(PB(bst, 2)[:, g * 512:(g + 1) * 512], B_tok.ap[:, i, g * 128:(g + 1) * 128], xdd.ap[:, g * 512:(g + 1) * 512],
                       True, True, B_tok.pg() + xdd.pg(), PK(bst, 2))
                tt(v3(sst), v3(sst), bc16(cdec), ALU.mult, sst.pg() + smal2.pg(), sst.pg())
                tt(sst.ap, sst.ap, PB(bst, 2), ALU.add, sst.pg() + PK(bst, 2), sst.pg())
                cp(sstb.ap, sst.ap, sst.pg(), sstb.pg())
                tt(y0.ap, y0.ap, sz.ap[:, i, :], ALU.mult, y0.pg() + sz.pg(), y0.pg())
                ms(ssq, 0.0, smal2.pg())
                act(y1.ap, y0.ap, AF.Square, y0.pg(), y1.pg() + smal2.pg(), accum_out=ssq)
                act(ssq, ssq, AF.Ln, smal2.pg(), smal2.pg(), bias=EPS, scale=1.0 / 1024.0)
                act(ssq, ssq, AF.Exp, smal2.pg(), smal2.pg(), scale=-0.5)
                ts1(ynb.ap, y0.ap, ssq, ALU.mult, y0.pg() + smal2.pg(), ynb.pg())
                to_featmajor(i, l, 48)

        def ret_tile(l, ti):
            wl = win_d[l]
            t0 = ti * T
            dma("sp", cst_t.ap, cs_d[:, :, t0:t0 + T].rearrange("a p t -> p a t"), (), cst_t.pg(), "cs")
            cosv, sinv = cst_t.ap[:, 0, :], cst_t.ap[:, 1, :]
            qcnt = [0]

            def qk_consumer(which, scale):
                def f(h, b):
                    q_ = qf[qcnt[0] % 2]
                    t1 = t12[qcnt[0] % 2]
                    qcnt[0] += 1
                    act(q_.ap, PB(b), AF.Copy, PK(b), q_.pg(), scale=scale)
                    b2 = nb()
                    mm(PB(b2), Rm, q_.ap, True, True, cst.pg() + q_.pg(), PK(b2))
                    tt(t1.ap, PB(b2), sinv, ALU.mult, PK(b2) + cst_t.pg(), t1.pg())
                    tt(q_.ap, q_.ap, cosv, ALU.mult, q_.pg() + cst_t.pg(), q_.pg())
                    dst = which.ap[:, h, :]
                    tt(dst, q_.ap, t1.ap, ALU.add, q_.pg() + t1.pg(), which.pg(h * 1024, (h + 1) * 1024))
                return f

            proj_fm(wl, C_RQ, 4, qk_consumer(qTr, 1.0))
            proj_fm(wl, C_RK, 4, qk_consumer(kTr, 128.0 ** -0.5))
            for s2 in range(2):
                proj_tm(wl, C_RV + s2 * 512, 512,
                        lambda i, b, s2=s2: act(v_tok.ap[:, i, s2 * 512:(s2 + 1) * 512], PB(b), AF.Copy, PK(b), v_tok.pg()))
            for s2 in range(2):
                proj_tm(wl, C_RG + s2 * 512, 512,
                        lambda i, b, s2=s2: act(srg.ap[:, i, s2 * 512:(s2 + 1) * 512], PB(b), AF.Silu, PK(b), srg.pg()))
            for i in range(4):
                cs_ = slice(i * 128, (i + 1) * 128)
                bsd = nb()
                for h in range(4):
                    mm(PB(bsd)[:, h * 128:(h + 1) * 128], kTr.ap[:, h, cs_], qTr.ap[:, h, cs_], True, True, kTr.pg() + qTr.pg(), PK(bsd))
                tt(SDT.ap, PB(bsd).rearrange("p (h i) -> p h i", h=4), dmatT, ALU.mult, PK(bsd) + cst.pg(), SDT.pg())
                bk = nb()
                for h in range(4):
                    tr(PBb(bk)[:, h * 128:(h + 1) * 128], kTr.ap[:, h, cs_], identb, kTr.pg() + cbf.pg(), PK(bk))
                tt(kz.ap, PBb(bk)[:, 0:512].rearrange("p (h d) -> p h d", h=4), zeta.unsqueeze(2).broadcast_to([128, 4, 128]),
                   ALU.mult, PK(bk) + cst.pg(), kz.pg())
                tt(qx.ap, qTr.ap[:, :, cs_], xiT, ALU.mult, qTr.pg() + cst.pg(), qx.pg())
                bo = nb2()
                for h in range(4):
                    o_ = PB(bo, 2)[:, h * 256:(h + 1) * 256]
                    mm(o_, SDT.ap[:, h, :], v_tok.ap[:, i, h * 256:(h + 1) * 256], True, False, SDT.pg() + v_tok.pg(), PK(bo, 2))
                    mm(o_, qx.ap[:, h, :], rstb.ap[:, h * 256:(h + 1) * 256], False, True, qx.pg() + rstb.pg(), PK(bo, 2))
                bkv = nb2()
                for h in range(4):
                    mm(PB(bkv, 2)[:, h * 256:(h + 1) * 256], kz.ap[:, h, :], v_tok.ap[:, i, h * 256:(h + 1) * 256], True, True,
                       kz.pg() + v_tok.pg(), PK(bkv, 2))
                y0, y1 = yf
                act(y0.ap, PB(bo, 2), AF.Copy, PK(bo, 2), y0.pg())
                for h in range(4):
                    sl_ = slice(h * 256, (h + 1) * 256)
                    stt(rst.ap[:, sl_], rst.ap[:, sl_], math.exp(128.0 * LOGG[h]), PB(bkv, 2)[:, sl_], ALU.mult, ALU.add,
                        rst.pg() + PK(bkv, 2), rst.pg())
                cp(rstb.ap, rst.ap, rst.pg(), rstb.pg())
                for h in range(4):
                    S.add("dve", lambda e, h=h: e.bn_stats(out=bn6[:, h, :], in_=y0.ap[:, h * 256:(h + 1) * 256]), y0.pg(), smal2.pg())
                for h in range(4):
                    S.add("dve", lambda e, h=h: e.bn_aggr(out=mv[:, h, :], in_=bn6[:, h, :]), smal2.pg(), smal2.pg())
                act(rstd4, mv[:, :, 1], AF.Ln, smal2.pg(), smal2.pg(), bias=EPS, scale=1.0)
                act(rstd4, rstd4, AF.Exp, smal2.pg(), smal2.pg(), scale=-0.5)
                for h in range(4):
                    sl_ = slice(h * 256, (h + 1) * 256)
                    ts(y0.ap[:, sl_], y0.ap[:, sl_], mv[:, h, 0:1], rstd4[:, h:h + 1], ALU.subtract, ALU.mult,
                       y0.pg() + smal2.pg(), y0.pg())
                tt(ynb.ap, y0.ap, srg.ap[:, i, :], ALU.mult, y0.pg() + srg.pg(), ynb.pg())
                to_featmajor(i, l, 56)

        def att_tile(l, ti):
            wl = win_d[l]
            ms(qTm.ap, 0.0, qTm.pg())

            def q_consumer(c, b):
                act(qTm.ap[0:64, 2 * c, :], PB(b)[0:64, :], AF.Copy, PK(b), qTm.pg(2 * c * 1024, (2 * c + 2) * 1024), scale=0.125)
                act(qTm.ap[64:128, 2 * c + 1, :], PB(b)[64:128, :], AF.Copy, PK(b), qTm.pg(2 * c * 1024, (2 * c + 2) * 1024), scale=0.125)

            proj_fm(wl, C_AQ, 8, q_consumer)
            s = wring[0][wcnt[0] % len(wring[0])]
            wcnt[0] += 1
            for g in range(2):
                for dup in range(2):
                    wslab(wl, 0, 8, C_AK + g * 64, 64, dst_c0=g * 128 + dup * 64, slot=s)
            wslab(wl, 0, 8, C_AV, 128, dst_c0=256, slot=s)
            for g in range(2):
                b = nb()
                for k in range(8):
                    mm(PB(b), wsl[s].ap[:, k, g * 128:(g + 1) * 128], hT.ap[:, k, :], k == 0, k == 7, wsl[s].pg() + hT.pg(), PK(b))
                act(kTa.ap[:, g, 128:640], PB(b), AF.Copy, PK(b), kTa.pg())
            bv = nb()
            for i in range(4):
                for k in range(8):
                    mm(PB(bv)[:, i * 128:(i + 1) * 128], hT.ap[:, k, i * 128:(i + 1) * 128], wsl[s].ap[:, k, 256:384], k == 0, k == 7,
                       wsl[s].pg() + hT.pg(), PK(bv))
            vsrc = PB(bv).rearrange("p (i g d) -> p i g d", i=4, g=2)
            cp(vtok.ap[:, 1:5, :, 0:64], vsrc, PK(bv), vtok.pg())
            cp(vtok.ap[:, 1:5, :, 64:128], vsrc, PK(bv), vtok.pg())
            scnt = 0
            if stop < 4.3:
                return
            for i in range(4):
                ci = ti * 4 + i
                blks = [1] if ci == 0 else [0, 1]
                for blk in blks:
                    koff = i * 128 + blk * 128
                    for hg in range(4):
                        b = nb()
                        for hh in range(4):
                            h = hg * 4 + hh
                            g = h // 8
                            mm(PB(b)[:, hh * 128:(hh + 1) * 128], kTa.ap[:, g, koff:koff + 128], qTm.ap[:, h, i * 128:(i + 1) * 128],
                               True, True, kTa.pg() + qTm.pg(), PK(b))
                        stp = stmp[scnt % 2]
                        scnt += 1
                        tt(stp.ap.rearrange("p (h q) -> p h q", h=4), PB(b).rearrange("p (h q) -> p h q", h=4),
                           biasT.ap[:, blk, hg * 4:(hg + 1) * 4, :], ALU.add, PK(b) + biasT.pg(), stp.pg())
                        act(PT.ap[:, blk, hg * 4:(hg + 1) * 4, :], stp.ap.rearrange("p (h q) -> p h q", h=4), AF.Exp, stp.pg(), PT.pg())
                if stop < 4.5:
                    continue
                dsb = yf[0]
                for g in range(2):
                    bo = nb2()
                    bd = nb2()
                    for half in range(2):
                        hs = 8 * g + 4 * half
                        for n_, blk in enumerate(blks):
                            rhs_ = PTf.ap[:, blk, hs * 128:(hs + 4) * 128]
                            mm(PB(bo, 2)[:, half * 512:(half + 1) * 512], vtok.ap[:, i + blk, g, :], rhs_,
                               n_ == 0, n_ == len(blks) - 1, PT.pg() + vtok.pg(), PK(bo, 2))
                    if stop < 4.7:
                        continue
                    for half in range(2):
                        hs = 8 * g + 4 * half
                        for n_, blk in enumerate(blks):
                            rhs_ = PTf.ap[:, blk, hs * 128:(hs + 4) * 128]
                            mm(PB(bd, 2)[:, half * 512:(half + 1) * 512], cbf.ap[:, 256:384], rhs_,
                               n_ == 0, n_ == len(blks) - 1, PT.pg() + cbf.pg(), PK(bd, 2))
                    if stop < 4.9:
                        continue
                    tt(dsb.ap.rearrange("p (h q) -> p h q", h=8), PB(bd, 2).rearrange("p (h q) -> p h q", h=8),
                       esink[:, 8 * g:8 * g + 8].unsqueeze(2).broadcast_to([128, 8, 128]), ALU.add, PK(bd, 2) + smal.pg(), dsb.pg())
                    S.add("dve", lambda e: e.reciprocal(out=dsb.ap, in_=dsb.ap), dsb.pg(), dsb.pg())
                    o4 = PB(bo, 2).rearrange("p (j two q) -> p j two q", j=4, two=2)
                    d4 = dsb.ap.rearrange("p (j two q) -> p j two q", j=4, two=2)
                    qs = slice(i * 128, (i + 1) * 128)
                    tt(yT.ap[0:64, 4 * g:4 * g + 4, qs], o4[0:64, :, 0, :], d4[0:64, :, 0, :], ALU.mult, PK(bo, 2) + dsb.pg(), yT.pg())
                    tt(yT.ap[64:128, 4 * g:4 * g + 4, qs], o4[64:128, :, 1, :], d4[64:128, :, 1, :], ALU.mult, PK(bo, 2) + dsb.pg(), yT.pg())
            cp(vtok.ap[:, 0, :, :], vtok.ap[:, 4, :, :], vtok.pg(), vtok.pg())
            cp(kTa.ap[:, :, 0:128], kTa.ap[:, :, 512:640], kTa.pg(), kTa.pg())

        xin = yf[0]

        def load_tile(l, ti):
            t0 = ti * T
            if l == 0:
                for i in range(4):
                    dma("sp", xin.ap, x_d[t0 + i * 128:t0 + (i + 1) * 128, :], (), xin.pg(), "xin")
                    b = nb2()
                    for c in range(8):
                        tr(PB(b, 2)[:, c * 128:(c + 1) * 128], xin.ap[:, c * 128:(c + 1) * 128], identf, xin.pg() + cst.pg(), PK(b, 2))
                    cp(xt.ap[:, :, i * 128:(i + 1) * 128], PB(b, 2).rearrange("p (c t) -> p c t", c=8), PK(b, 2), xt.pg())
            else:
                dma("sp", xt.ap, xs_d[:, :, t0:t0 + T], [("xd", ti)], xt.pg(), "xt")

        def store_tile(l, ti):
            t0 = ti * T
            if l == L - 1:
                for i in range(4):
                    b = nb2()
                    for c in range(8):
                        tr(PB(b, 2)[:, c * 128:(c + 1) * 128], xt.ap[:, c, i * 128:(i + 1) * 128], identf, xt.pg() + cst.pg(), PK(b, 2))
                    cp(xin.ap, PB(b, 2), PK(b, 2), xin.pg())
                    dma("sp", out_d[t0 + i * 128:t0 + (i + 1) * 128, :], xin.ap, xin.pg(), [("od", ti, i)], "xout")
            else:
                dma("sp", xs_d[:, :, t0:t0 + T], xt.ap, xt.pg(), [("xd", ti)], "xst")

        for l in range(L):
            ms(sst.ap, 0.0, sst.pg())
            ms(sstb.ap, 0.0, sstb.pg())
            ms(rst.ap, 0.0, rst.pg())
            ms(rstb.ap, 0.0, rstb.pg())
            ms(hist.ap, 0.0, hist.pg())
            ms(kTa.ap, 0.0, kTa.pg())
            act(a_b, pvc(l, 164, 16), AF.Exp, pv.pg(), smal.pg())
            ts1(a_b, a_b, -1.0, ALU.mult, smal.pg(), smal.pg())
            act(esink, pvc(l, 196, 16), AF.Exp, pv.pg(), smal.pg())
            for ti in range(NT):
                load_tile(l, ti)
                if stop >= 1:
                    ffn(l, w1i_d, w1o_d, 0, 8)
                if stop >= 2:
                    pre_norm(l, 16)
                    ssd_tile(l, ti)
                    if dbg and l == 0:
                        dma("pool", dbg_d[0, :, :, ti * T:(ti + 1) * T], yT.ap, yT.pg(), [("dbg", 0, ti)], "dbg")
                if stop >= 3:
                    merge(l, 0)
                if stop >= 4:
                    ret_tile(l, ti)
                    if dbg and l == 0:
                        dma("pool", dbg_d[1, :, :, ti * T:(ti + 1) * T], yT.ap, yT.pg(), [("dbg", 1, ti)], "dbg")
                    merge(l, 1)
                if stop > 4.1:
                    att_tile(l, ti)
                    if dbg and l == 0:
                        dma("pool", dbg_d[2, :, :, ti * T:(ti + 1) * T], yT.ap, yT.pg(), [("dbg", 2, ti)], "dbg")
                    if stop >= 5:
                        merge(l, 2)
                if stop >= 6:
                    out_proj(l)
                if stop >= 7:
                    ffn(l, w2i_d, w2o_d, 32, 40)
                store_tile(l, ti)
        S.emit()
    return nc


def _t5_bucket(dist):
    is_small = dist < 16
    d = np.maximum(dist, 1).astype(np.float32)
    large = 16 + (np.log(d / 16) / math.log(128 / 16) * 16).astype(np.int32)
    large = np.minimum(large, 31)
    return np.where(is_small, dist, large)


def _consts(ntok):
    idx = np.arange(128)
    cst = np.zeros((128, NCST), np.float32)
    cst[:, 0:128] = np.eye(128)
    cst[:, 128:256] = (idx[:, None] <= idx[None, :])
    cst[:, 256:384] = 1.0
    cst[:, 384:512] = (idx[:, None] > idx[None, :])
    cst[:, 512:640] = (idx[None, :] >= idx[:, None])
    R = np.zeros((128, 128), np.float32)
    for dp in range(64):
        R[dp + 64, dp] = -1.0
        R[dp, dp + 64] = 1.0
    cst[:, 640:768] = R
    lg = np.array(LOGG, np.float64)
    diff = idx[None, :] - idx[:, None]
    dm = np.where(diff[None] >= 0, np.exp(np.maximum(diff, 0)[None] * lg[:, None, None]), 0.0)
    cst[:, 768:1280] = np.transpose(dm, (1, 0, 2)).reshape(128, 512)
    xi = np.exp((idx + 1.0)[None, :] * lg[:, None])
    cst[:, 1280:1792] = np.broadcast_to(xi.reshape(1, 512), (128, 512))
    cst[:, 1792:1796] = np.exp((127.0 - idx)[:, None] * lg[None, :])
    pos = np.arange(ntok, dtype=np.float32)
    inv = (1.0 / (np.float32(10000.0) ** (np.arange(0, 128, 2, dtype=np.float32) / np.float32(128)))).astype(np.float32)
    ang = pos[:, None] * inv[None, :]
    cos, sin = np.cos(ang).astype(np.float32).T, np.sin(ang).astype(np.float32).T
    cs = np.stack([np.concatenate([cos, cos], 0), np.concatenate([sin, sin], 0)], 0).astype(np.float32)
    return cst, np.ascontiguousarray(cs)


def _bias_table(rel_bias):
    qi = np.arange(128)[:, None]
    sj = np.arange(256)[None, :]
    dist = qi + 128 - sj
    mask = (dist >= 0) & (dist < 128)
    bb = rel_bias[_t5_bucket(np.maximum(dist, 0))]
    bb = np.where(mask[:, :, None], bb, np.float32(-1e30)).astype(np.float32)
    t = bb.reshape(128, 2, 128, 16)
    t = np.transpose(t, (2, 1, 3, 0))
    return np.ascontiguousarray(t.reshape(128, 2 * 16 * 128))


def _pack_params(L, p):
    fm = lambda v, n: v.reshape(n, 128).T
    pv = np.zeros((128, L, NV), np.float32)
    for l in range(L):
        for j, nm in enumerate(["ffn1_pre_g", "ffn1_post_g", "mix_pre_g", "mix_post_g", "ffn2_pre_g", "ffn2_post_g",
                                "ssd_norm_g", "ret_gn_g"]):
            pv[:, l, j * 8:(j + 1) * 8] = fm(p[nm][l], 8)
        cw = p["conv_w"][l]
        pv[:, l, 64:112] = np.transpose(cw.reshape(4, 12, 128), (2, 1, 0)).reshape(128, 48)
        pv[:, l, 112:124] = fm(p["conv_b"][l], 12)
        pv[:, l, 124:148] = fm(p["b_gate"][l], 24)
        pv[:, l, 148:164] = p["dt_bias"][l][None, :]
        pv[:, l, 164:180] = p["a_log"][l][None, :]
        pv[:, l, 180:196] = p["d_skip"][l][None, :]
        pv[:, l, 196:212] = p["attn_sinks"][l][None, :]
    return np.ascontiguousarray(pv.reshape(128, L * NV))


_NC_CACHE = {}


def run(inputs, L=DEPTH, ntok=SEQ, ncores=8, dbg=False, stop=99):
    p = {k: np.asarray(v, dtype=np.float32) for k, v in inputs.items()}
    key = (L, ntok, dbg, stop)
    if key not in _NC_CACHE:
        _NC_CACHE[key] = build(L, ntok, dbg, stop)
    nc = _NC_CACHE[key]
    cst, cs = _consts(ntok)
    shared = {
        "w1i": np.ascontiguousarray(p["w_ffn1_in"][:L]), "w1o": np.ascontiguousarray(p["w_ffn1_out"][:L]),
        "w2i": np.ascontiguousarray(p["w_ffn2_in"][:L]), "w2o": np.ascontiguousarray(p["w_ffn2_out"][:L]),
        "win": np.ascontiguousarray(p["w_in"][:L]), "wbr": np.ascontiguousarray(p["w_branch"][:L]),
        "wo": np.ascontiguousarray(p["w_out"][:L]),
        "pv": _pack_params(L, p), "cst": cst, "cs": cs, "biasT": _bias_table(p["rel_bias"]),
    }
    in_maps = []
    for b in range(ncores):
        m = dict(shared)
        m["x"] = np.ascontiguousarray(p["x"][b, :ntok])
        in_maps.append(m)
    res = run_bass_kernel_spmd(nc, in_maps, core_ids=list(range(ncores)))
    return res


def kernel(**inputs):
    res = run(inputs)
    return np.stack([np.asarray(r["out"], dtype=np.float32) for r in res.results], axis=0)
```

```python
import math
from contextlib import ExitStack
import numpy as np
import concourse.bass as bass
import concourse.mybir as mybir
from concourse.bass_utils import run_bass_kernel_spmd

F32 = mybir.dt.float32
BF16 = mybir.dt.bfloat16
AF = mybir.ActivationFunctionType
ALU = mybir.AluOpType

D = 1024
DFF = 2816
INW = 10000
DEPTH = 4
SEQ = 2048
T = 512
EPS = 1e-6
NV = 212
NCST = 1796
ENGS = ("pe", "act", "dve", "pool", "sp")
C_Z, C_XBC, C_DT, C_RQ, C_RK, C_RV, C_RG, C_AQ, C_AK, C_AV, C_GATE = 0, 1024, 2560, 2576, 3088, 3600, 4624, 5648, 6672, 6800, 6928
LOGG = [math.log(1.0 - 2.0 ** (-5.0 - h)) for h in range(4)]


class Op:
    __slots__ = ("eng", "fn", "reads", "writes", "dma", "waits", "signal", "semkey", "deps", "sigval", "idx", "need", "ndma")

    def __init__(self, eng, fn, reads, writes, dma):
        self.eng, self.fn, self.reads, self.writes, self.dma = eng, fn, reads, writes, dma
        self.waits = {}
        self.signal = False
        self.semkey = None


class Sched:
    def __init__(self, nc):
        self.nc = nc
        self.ops = []
        self.last_writer = {}
        self.readers = {}

    def add(self, eng, fn, reads=(), writes=(), dma=None, ndma=1):
        op = Op(eng, fn, tuple(reads), tuple(writes), dma)
        op.ndma = ndma
        deps = set()
        for r in op.reads:
            w = self.last_writer.get(r)
            if w is not None:
                deps.add(w)
        for r in op.writes:
            w = self.last_writer.get(r)
            if w is not None:
                deps.add(w)
            for rd in self.readers.get(r, ()):
                deps.add(rd)
        op.deps = deps
        for r in op.reads:
            self.readers.setdefault(r, []).append(op)
        for r in op.writes:
            self.last_writer[r] = op
            self.readers[r] = []
        self.ops.append(op)
        return op

    @staticmethod
    def _skip(d, op):
        return d is op or (d.dma is None and op.dma is None and d.eng == "pe" and op.eng == "pe")

    def finalize(self):
        counters = {}
        for i, op in enumerate(self.ops):
            op.idx = i
        for op in self.ops:
            best = {}
            for d in op.deps:
                if self._skip(d, op):
                    continue
                k = ("dma", d.dma) if d.dma is not None else ("eng", d.eng)
                if k not in best or d.idx > best[k].idx:
                    best[k] = d
            op.need = list(best.values())
            for d in op.need:
                d.signal = True
        for op in self.ops:
            if op.dma is not None:
                op.semkey = ("dma", op.dma)
                op.signal = True
            else:
                op.semkey = ("eng", op.eng)
            if op.signal:
                counters[op.semkey] = counters.get(op.semkey, 0) + (16 * op.ndma if op.dma is not None else 1)
                op.sigval = counters[op.semkey]
        seen = {e: {} for e in ENGS}
        for op in self.ops:
            for d in op.need:
                k, v = d.semkey, d.sigval
                if seen[op.eng].get(k, 0) >= v:
                    continue
                if op.waits.get(k, 0) < v:
                    op.waits[k] = v
            for k, v in op.waits.items():
                seen[op.eng][k] = v
        self.semkeys = list(counters.keys())
        self.final_counts = counters

    def emit(self):
        nc = self.nc
        self.finalize()
        with ExitStack() as st:
            sems = {}
            for i, k in enumerate(self.semkeys):
                sems[k] = st.enter_context(nc.semaphore("s%d" % i))
            block = st.enter_context(nc.Block())
            per = {e: [op for op in self.ops if op.eng == e] for e in ENGS}

            def run(e, ops):
                for op in ops:
                    for k, v in op.waits.items():
                        e.wait_ge(sems[k], v)
                    ins = op.fn(e)
                    if op.signal:
                        if isinstance(ins, list):
                            for x_ in ins:
                                x_.then_inc(sems[op.semkey], 16)
                        else:
                            ins.then_inc(sems[op.semkey], 16 if op.dma is not None else 1)

            @block.tensor
            def _(e):
                run(e, per["pe"])

            @block.scalar
            def _(e):
                run(e, per["act"])

            @block.vector
            def _(e):
                run(e, per["dve"])

            @block.gpsimd
            def _(e):
                run(e, per["pool"])

            @block.sync
            def _(e):
                run(e, per["sp"])
                for k in self.semkeys:
                    e.wait_ge(sems[k], self.final_counts[k])


class Buf:
    def __init__(self, mem, off, nbytes, dtype, shape=None):
        self.off, self.nbytes = off, nbytes
        if shape is not None:
            n = 1
            for d_ in shape:
                n *= d_
            assert n * (4 if dtype == F32 else 2) == nbytes, (shape, nbytes)
        v = mem[:, off // 2:(off + nbytes) // 2]
        if dtype == F32:
            v = v.bitcast(F32)
        if shape is not None and len(shape) == 2:
            v = v.rearrange("p (a b) -> p a b", a=shape[0])
        elif shape is not None and len(shape) == 3:
            v = v.rearrange("p (a b c) -> p a b c", a=shape[0], b=shape[1])
        self.ap = v

    def pg(self, lo=0, hi=None):
        hi = self.nbytes if hi is None else hi
        return [("pg", i) for i in range((self.off + lo) // 1024, (self.off + hi - 1) // 1024 + 1)]


def build(L=DEPTH, NTOK=SEQ, dbg=False, stop=99):
    NT = NTOK // T
    nc = bass.Bass("TRN2", target_bir_lowering=False)
    dr = lambda n, s, k="ExternalInput": nc.dram_tensor(n, s, F32, kind=k).ap()
    x_d = dr("x", [NTOK, D])
    w1i_d, w1o_d = dr("w1i", [L, D, 2 * DFF]), dr("w1o", [L, DFF, D])
    w2i_d, w2o_d = dr("w2i", [L, D, 2 * DFF]), dr("w2o", [L, DFF, D])
    win_d, wbr_d, wo_d = dr("win", [L, D, INW]), dr("wbr", [L, 3, D, D]), dr("wo", [L, D, D])
    pv_d, cst_d = dr("pv", [128, L * NV]), dr("cst", [128, NCST])
    cs_d = dr("cs", [2, 128, NTOK])
    bias_d = dr("biasT", [128, 2 * 16 * 128])
    out_d = dr("out", [NTOK, D], "ExternalOutput")
    xs_d = dr("xscr", [128, 8, NTOK], "Internal")
    dbg_d = dr("dbgo", [3, 128, 8, NTOK], "ExternalOutput") if dbg else None

    with ExitStack() as st:
        TOTAL = 206 * 1024
        mem = st.enter_context(nc.sbuf_tensor("mem", [128, TOTAL // 2], BF16))
        ps = st.enter_context(nc.psum_tensor("ps", [128, 4096], F32))
        S = Sched(nc)
        cur = [0]

        def alloc(nbytes, dtype, shape=None):
            nb_ = (nbytes + 1023) // 1024 * 1024
            b = Buf(mem, cur[0], nbytes, dtype, shape)
            cur[0] += nb_
            assert cur[0] <= TOTAL, cur[0]
            return b

        cst = alloc(NCST * 4, F32)
        pv = alloc(L * NV * 4, F32)
        cbf = alloc(3 * 128 * 2, BF16)
        biasT = alloc(2 * 16 * 128 * 2, BF16, (2, 16, 128))
        xt = alloc(8 * T * 4, F32, (8, T))
        hT = alloc(8 * T * 2, BF16, (8, T))
        ytmp = alloc(8 * T * 4, F32, (8, T))
        rs = alloc(T * 4, F32)
        wsl = [alloc(8 * 512 * 2, BF16, (8, 512)) for _ in range(3)]
        ysum = alloc(8 * T * 4, F32, (8, T))
        yT = alloc(8 * T * 2, BF16, (8, T))
        etmp = [alloc(T * 4, F32) for _ in range(2)]
        sst = alloc(1024 * 4, F32)
        sstb = alloc(1024 * 2, BF16)
        rst = alloc(1024 * 4, F32)
        rstb = alloc(1024 * 2, BF16)
        hist = alloc(12 * 3 * 4, F32, (12, 3))
        kTa = alloc(2 * 640 * 2, BF16, (2, 640))
        vtok = alloc(5 * 2 * 128 * 2, BF16, (5, 2, 128))
        smal = alloc(1024, F32)
        smal2 = alloc(1024, F32)
        A0 = cur[0]

        def arena(off, nbytes, dtype, shape=None):
            assert A0 + off + nbytes <= TOTAL, (A0, off, nbytes)
            return Buf(mem, A0 + off, nbytes, dtype, shape)

        K = 1024
        sq = arena(0, 8 * K, BF16, (8, T))
        hid = arena(8 * K, 22 * K, BF16, (22, T))
        wsl.extend(arena(30 * K + i * 8 * K, 8 * K, BF16, (8, 512)) for i in range(4))
        wring = [[0, 1, 2]]
        ynb = arena(0, 2 * K, BF16)
        yf = [arena(2 * K + i * 4 * K, 4 * K, F32) for i in range(2)]
        MX = 10 * K
        sz = arena(MX, 8 * K, BF16, (4, 1024))
        xs_tok = arena(MX + 8 * K, 8 * K, BF16, (4, 1024))
        B_tok = arena(MX + 16 * K, 2 * K, BF16, (4, 256))
        bcT = arena(MX + 18 * K, 4 * K, BF16, (4, T))
        raw = [arena(MX + 22 * K + i * 3 * K, 515 * 4, F32) for i in range(2)]
        cacc = [arena(MX + 28 * K + i * 2 * K, 2 * K, F32) for i in range(2)]
        xsTt = [arena(MX + 32 * K + i * K, K, BF16) for i in range(2)]
        xdt = arena(MX + 34 * K, 2 * K, BF16)
        xdd = arena(MX + 36 * K, 2 * K, BF16)
        rhsm = arena(MX + 38 * K, 8 * K, F32)
        Eb = arena(MX + 46 * K, 4 * K, BF16, (16, 128))
        MTb = arena(MX + 50 * K, 4 * K, BF16, (16, 128))
        cbm = arena(MX + 54 * K, 512, BF16, (2, 128))
        qf = [arena(MX + i * 2 * K, 2 * K, F32) for i in range(2)]
        t12 = [arena(MX + 4 * K + i * 2 * K, 2 * K, F32) for i in range(2)]
        qTr = arena(MX + 8 * K, 4 * K, BF16, (4, T))
        kTr = arena(MX + 12 * K, 4 * K, BF16, (4, T))
        v_tok = arena(MX + 16 * K, 8 * K, BF16, (4, 1024))
        srg = arena(MX + 24 * K, 8 * K, BF16, (4, 1024))
        SDT = arena(MX + 32 * K, K, BF16, (4, 128))
        kz = arena(MX + 33 * K, K, BF16, (4, 128))
        qx = arena(MX + 34 * K, K, BF16, (4, 128))
        cst_t = arena(MX + 35 * K, 4 * K, F32, (2, T))
        qTm = arena(MX, 16 * K, BF16, (16, T))
        stmp = [arena(MX + 16 * K + i * 2 * K, 2 * K, F32) for i in range(2)]
        PT = arena(MX + 20 * K, 8 * K, BF16, (2, 16, 128))
        PTf = arena(MX + 20 * K, 8 * K, BF16, (2, 2048))

        identf = cst.ap[:, 0:128]
        triu = cst.ap[:, 128:256]
        onesf = cst.ap[:, 256:384]
        slow = cst.ap[:, 384:512]
        causT = cst.ap[:, 512:640]
        Rm = cst.ap[:, 640:768]
        dmatT = cst.ap[:, 768:1280].rearrange("p (h i) -> p h i", h=4)
        xiT = cst.ap[:, 1280:1792].rearrange("p (h i) -> p h i", h=4)
        zeta = cst.ap[:, 1792:1796]
        identb = cbf.ap[:, 0:128]
        onesb = cbf.ap[:, 128:256]
        one1b = cbf.ap[:, 256:257]

        pbank = [0]

        def nb():
            b = pbank[0] % 8
            pbank[0] += 1
            return b

        def nb2():
            if pbank[0] % 2:
                pbank[0] += 1
            b = pbank[0] % 8
            pbank[0] += 2
            return b

        def nb4():
            while pbank[0] % 4:
                pbank[0] += 1
            b = pbank[0] % 8
            pbank[0] += 4
            return b

        PB = lambda b, n=1: ps[:, b * 512:(b + n) * 512]
        PBb = lambda b: ps[:, b * 512:(b + 1) * 512].bitcast(BF16)
        PK = lambda b, n=1: [("ps", b + i) for i in range(n)]

        def mm(out, lhsT, rhs, start, stop, r, w):
            S.add("pe", lambda e: e.matmul(out, lhsT=lhsT, rhs=rhs, start=start, stop=stop), r, w)

        def tr(out, in_, ident, r, w):
            S.add("pe", lambda e: e.transpose(out, in_, ident), r, w)

        def act(out, in_, func, r, w, **kw):
            S.add("act", lambda e: e.activation(out=out, in_=in_, func=func, **kw), r, w)

        def tt(out, in0, in1, op, r, w, eng="dve"):
            S.add(eng, lambda e: e.tensor_tensor(out=out, in0=in0, in1=in1, op=op), r, w)

        def stt(out, in0, scalar, in1, op0, op1, r, w):
            S.add("dve", lambda e: e.scalar_tensor_tensor(out=out, in0=in0, scalar=scalar, in1=in1, op0=op0, op1=op1), r, w)

        def ts(out, in0, s1, s2, op0, op1, r, w):
            S.add("dve", lambda e: e.tensor_scalar(out=out, in0=in0, scalar1=s1, scalar2=s2, op0=op0, op1=op1), r, w)

        def ts1(out, in0, s1, op, r, w):
            S.add("dve", lambda e: e.tensor_single_scalar(out=out, in_=in0, scalar=s1, op=op), r, w)

        def cp(out, in_, r, w, eng="dve"):
            S.add(eng, lambda e: e.tensor_copy(out=out, in_=in_), r, w)

        def ms(ap, val, w, eng="dve"):
            S.add(eng, lambda e: e.memset(ap, val), (), w)

        def dma(eng, out, in_, r, w, key):
            S.add(eng, lambda e: e.dma_start(out=out, in_=in_), r, w, dma=key)

        wcnt = [0]

        def wslab(w2d, k0, nk, c0, ncols, dst_c0=0, slot=None):
            if slot is None:
                slot = wring[0][wcnt[0] % len(wring[0])]
                wcnt[0] += 1
            src = w2d[k0 * 128:(k0 + nk) * 128, c0:c0 + ncols].rearrange("(k p) n -> p k n", p=128)
            dst = wsl[slot].ap[:, 0:nk, dst_c0:dst_c0 + ncols]
            dma("pool", dst, src, (), wsl[slot].pg(), ("w", slot))
            return slot

        dma("sp", cst.ap, cst_d, (), cst.pg(), "cst")
        dma("sp", pv.ap, pv_d, (), pv.pg(), "pv")
        dma("pool", biasT.ap, bias_d.rearrange("p (b h q) -> p b h q", b=2, h=16), (), biasT.pg(), "bias")
        cp(identb, identf, cst.pg(), cbf.pg())
        ts1(onesb, onesf, 1.0 / 1024.0, ALU.mult, cst.pg(), cbf.pg())
        cp(cbf.ap[:, 256:384], onesf, cst.pg(), cbf.pg())

        def pvc(l, c, n=1):
            return pv.ap[:, l * NV + c:l * NV + c + n]

        def stats_from_sq():
            b = nb()
            for c in range(8):
                mm(PB(b), onesb, sq.ap[:, c, :], c == 0, c == 7, sq.pg(c * 1024, (c + 1) * 1024) + cbf.pg(), PK(b))
            act(rs.ap, PB(b), AF.Ln, PK(b), rs.pg(), bias=EPS, scale=1.0)
            act(rs.ap, rs.ap, AF.Exp, rs.pg(), rs.pg(), scale=-0.5)

        def pre_norm(l, gcol):
            for c in range(8):
                act(sq.ap[:, c, :], xt.ap[:, c, :], AF.Square, xt.pg(c * 2048, (c + 1) * 2048), sq.pg(c * 1024, (c + 1) * 1024))
            stats_from_sq()
            for c in range(8):
                stt(hT.ap[:, c, :], xt.ap[:, c, :], pvc(l, gcol + c), rs.ap, ALU.mult, ALU.mult,
                    xt.pg(c * 2048, (c + 1) * 2048) + pv.pg() + rs.pg(), hT.pg(c * 1024, (c + 1) * 1024))

        def evac_y(c, bank):
            yk = ytmp.pg(c * 2048, (c + 1) * 2048)
            act(ytmp.ap[:, c, :], PB(bank), AF.Copy, PK(bank), yk)
            act(sq.ap[:, c, :], ytmp.ap[:, c, :], AF.Square, yk, sq.pg(c * 1024, (c + 1) * 1024))

        def post_norm_add(l, gcol, half):
            stats_from_sq()
            for c in range(8):
                yk = ytmp.pg(c * 2048, (c + 1) * 2048)
                xk = xt.pg(c * 2048, (c + 1) * 2048)
                stt(ytmp.ap[:, c, :], ytmp.ap[:, c, :], pvc(l, gcol + c), rs.ap, ALU.mult, ALU.mult, yk + pv.pg() + rs.pg(), yk)
                stt(xt.ap[:, c, :], ytmp.ap[:, c, :], half, xt.ap[:, c, :], ALU.mult, ALU.add, yk + xk, xk)

        def ffn(l, wi_d, wo_d_, gpre, gpost):
            pre_norm(l, gpre)
            wring[0] = [0, 1, 2, 3, 4, 5, 6]
            wi = wi_d[l]
            wo_ = wo_d_[l]
            j = 0
            ecnt = 0
            for g0 in range(0, DFF, 512):
                gw = min(512, DFF - g0)
                sa = wslab(wi, 0, 8, g0, gw)
                sb = wslab(wi, 0, 8, DFF + g0, gw)
                for jj in range(gw // 128):
                    ba, bb = nb(), nb()
                    for k in range(8):
                        mm(PB(ba), wsl[sa].ap[:, k, jj * 128:(jj + 1) * 128], hT.ap[:, k, :], k == 0, k == 7,
                           wsl[sa].pg() + hT.pg(), PK(ba))
                    for k in range(8):
                        mm(PB(bb), wsl[sb].ap[:, k, jj * 128:(jj + 1) * 128], hT.ap[:, k, :], k == 0, k == 7,
                           wsl[sb].pg() + hT.pg(), PK(bb))
                    et = etmp[ecnt % 2]
                    ecnt += 1
                    act(et.ap, PB(ba), AF.Silu, PK(ba), et.pg())
                    tt(hid.ap[:, j, :], et.ap, PB(bb), ALU.mult, et.pg() + PK(bb), hid.pg(j * 1024, (j + 1) * 1024))
                    j += 1
            for mg in range(2):
                b0 = nb4()
                for kg in range(3):
                    k0 = kg * 8
                    nk = min(8, 22 - k0)
                    s = wslab(wo_, k0, nk, mg * 512, 512)
                    for m in range(4):
                        for k in range(nk):
                            mm(PB(b0 + m), wsl[s].ap[:, k, m * 128:(m + 1) * 128], hid.ap[:, k0 + k, :],
                               kg == 0 and k == 0, kg == 2 and k == nk - 1,
                               wsl[s].pg() + hid.pg((k0 + k) * 1024, (k0 + k + 1) * 1024), PK(b0 + m))
                for m in range(4):
                    c = mg * 4 + m
                    evac_y(c, b0 + m)
            wring[0] = [0, 1, 2]
            post_norm_add(l, gpost, 0.5)

        def proj_fm(wl, col0, nch, consumer, dup=None):
            ci = 0
            while ci < nch:
                n = min(4, nch - ci)
                s = wslab(wl, 0, 8, col0 + ci * 128, n * 128)
                for jj in range(n):
                    b = nb()
                    for k in range(8):
                        mm(PB(b), wsl[s].ap[:, k, jj * 128:(jj + 1) * 128], hT.ap[:, k, :], k == 0, k == 7,
                           wsl[s].pg() + hT.pg(), PK(b))
                    consumer(ci + jj, b)
                ci += n

        def proj_tm(wl, col0, ncols, consumer):
            s = wslab(wl, 0, 8, col0, ncols)
            for i in range(4):
                b = nb()
                for k in range(8):
                    mm(PB(b)[:, 0:ncols], hT.ap[:, k, i * 128:(i + 1) * 128], wsl[s].ap[:, k, 0:ncols], k == 0, k == 7,
                       wsl[s].pg() + hT.pg(), PK(b))
                consumer(i, b)

        def to_featmajor(i, l, gcol):
            b = nb()
            for c in range(8):
                tr(PBb(b)[:, c * 128:(c + 1) * 128], ynb.ap[:, c * 128:(c + 1) * 128], identb, ynb.pg() + cbf.pg(), PK(b))
            src = PBb(b).rearrange("p (c t) -> p c t", c=8)
            dst = yT.ap[:, :, i * 128:(i + 1) * 128]
            if gcol is None:
                cp(dst, src, PK(b), yT.pg())
            else:
                tt(dst, src, pvc(l, gcol, 8).unsqueeze(2).broadcast_to([128, 8, 128]), ALU.mult, PK(b) + pv.pg(), yT.pg())

        def merge(l, m):
            wl = win_d[l]
            wb = wbr_d[l, m]
            ecnt = 0
            for half in range(2):
                sg = wslab(wl, 0, 8, C_GATE + m * 1024 + half * 512, 512)
                sbm = wslab(wb, 0, 8, half * 512, 512)
                for jj in range(4):
                    c = half * 4 + jj
                    bg, bb = nb(), nb()
                    for k in range(8):
                        mm(PB(bg), wsl[sg].ap[:, k, jj * 128:(jj + 1) * 128], hT.ap[:, k, :], k == 0, k == 7,
                           wsl[sg].pg() + hT.pg(), PK(bg))
                    for k in range(8):
                        mm(PB(bb), wsl[sbm].ap[:, k, jj * 128:(jj + 1) * 128], yT.ap[:, k, :], k == 0, k == 7,
                           wsl[sbm].pg() + yT.pg(), PK(bb))
                    et = etmp[ecnt % 2]
                    ecnt += 1
                    act(et.ap, PB(bg), AF.Sigmoid, PK(bg) + pv.pg(), et.pg(), bias=pvc(l, 124 + m * 8 + c), scale=1.0)
                    ysc = ysum.ap[:, c, :]
                    yk = ysum.pg(c * 2048, (c + 1) * 2048)
                    if m == 0:
                        tt(ysc, et.ap, PB(bb), ALU.mult, et.pg() + PK(bb), yk)
                    else:
                        tt(et.ap, et.ap, PB(bb), ALU.mult, et.pg() + PK(bb), et.pg())
                        tt(ysc, ysc, et.ap, ALU.add, et.pg() + yk, yk)

        def out_proj(l):
            cp(hT.ap, ysum.ap, ysum.pg(), hT.pg())
            for half in range(2):
                s = wslab(wo_d[l], 0, 8, half * 512, 512)
                for jj in range(4):
                    c = half * 4 + jj
                    b = nb()
                    for k in range(8):
                        mm(PB(b), wsl[s].ap[:, k, jj * 128:(jj + 1) * 128], hT.ap[:, k, :], k == 0, k == 7,
                           wsl[s].pg() + hT.pg(), PK(b))
                    evac_y(c, b)
            post_norm_add(l, 24, 1.0)

        a_b = smal.ap[:, 0:16]
        esink = smal.ap[:, 16:32]
        dtb = smal.ap[:, 32:96].rearrange("p (i h) -> p i h", i=4)
        dab = smal.ap[:, 96:160].rearrange("p (i h) -> p i h", i=4)
        acs = smal2.ap[:, 0:32]
        dec_out = smal2.ap[:, 32:48]
        dec_st = smal2.ap[:, 48:64]
        cdec = smal2.ap[:, 64:80]
        ssq = smal2.ap[:, 80:81]
        bn6 = smal2.ap[:, 96:120].rearrange("p (h s) -> p h s", h=4)
        mv = smal2.ap[:, 120:128].rearrange("p (h s) -> p h s", h=4)
        rstd4 = smal2.ap[:, 128:132]
        den = smal2.ap[:, 136:152]

        def ssd_tile(l, ti):
            wl = win_d[l]
            for s2 in range(2):
                proj_tm(wl, C_Z + s2 * 512, 512,
                        lambda i, b, s2=s2: act(sz.ap[:, i, s2 * 512:(s2 + 1) * 512], PB(b), AF.Silu, PK(b), sz.pg()))
            sl = wslab(wl, 0, 8, C_DT, 16)
            bd = nb()
            for i in range(4):
                for k in range(8):
                    mm(PB(bd)[:, i * 16:(i + 1) * 16], hT.ap[:, k, i * 128:(i + 1) * 128], wsl[sl].ap[:, k, 0:16], k == 0, k == 7,
                       wsl[sl].pg() + hT.pg(), PK(bd))
            tt(dtb, PB(bd)[:, 0:64].rearrange("p (i h) -> p i h", i=4), pvc(l, 148, 16).unsqueeze(1).broadcast_to([128, 4, 16]),
               ALU.add, PK(bd) + pv.pg(), smal.pg())
            act(dtb, dtb, AF.Exp, smal.pg(), smal.pg())
            act(dtb, dtb, AF.Ln, smal.pg(), smal.pg(), bias=1.0, scale=1.0)
            tt(dab, dtb, a_b.unsqueeze(1).broadcast_to([128, 4, 16]), ALU.mult, smal.pg(), smal.pg())
            ccnt = [0]

            def xbc_consumer(c, b):
                rw = raw[ccnt[0] % 2]
                ca = cacc[ccnt[0] % 2]
                xtt = xsTt[ccnt[0] % 2]
                ccnt[0] += 1
                cp(rw.ap[:, 0:3], hist.ap[:, c, :], hist.pg(), rw.pg())
                act(rw.ap[:, 3:515], PB(b), AF.Copy, PK(b), rw.pg())
                cp(hist.ap[:, c, :], rw.ap[:, 512:515], rw.pg(), hist.pg())
                wc = lambda k: pvc(l, 64 + c * 4 + k)
                ts1(ca.ap, rw.ap[:, 0:512], wc(0), ALU.mult, rw.pg() + pv.pg(), ca.pg())
                for k in range(1, 4):
                    stt(ca.ap, rw.ap[:, k:k + 512], wc(k), ca.ap, ALU.mult, ALU.add, rw.pg() + pv.pg() + ca.pg(), ca.pg())
                if c < 8:
                    act(xtt.ap, ca.ap, AF.Silu, ca.pg() + pv.pg(), xtt.pg(), bias=pvc(l, 112 + c), scale=1.0)
                    dst_src = xtt
                else:
                    act(bcT.ap[:, c - 8, :], ca.ap, AF.Silu, ca.pg() + pv.pg(), bcT.pg(), bias=pvc(l, 112 + c), scale=1.0)
                if c < 10:
                    pend.append((c, xtt))

            def xbc_transposes(c, xtt):
                bt = nb()
                for i in range(4):
                    src = xtt.ap[:, i * 128:(i + 1) * 128] if c < 8 else bcT.ap[:, c - 8, i * 128:(i + 1) * 128]
                    tr(PBb(bt)[:, i * 128:(i + 1) * 128], src, identb, (xtt.pg() if c < 8 else bcT.pg()) + cbf.pg(), PK(bt))
                srcv = PBb(bt)[:, 0:512].rearrange("p (i f) -> p i f", i=4)
                if c < 8:
                    cp(xs_tok.ap[:, :, c * 128:(c + 1) * 128], srcv, PK(bt), xs_tok.pg())
                else:
                    cp(B_tok.ap[:, :, (c - 8) * 128:(c - 7) * 128], srcv, PK(bt), B_tok.pg())

            pend = []

            def xbc_consumer2(c, b):
                if len(pend) >= 2:
                    xbc_transposes(*pend.pop(0))
                xbc_consumer(c, b)

            proj_fm(wl, C_XBC, 12, xbc_consumer2)
            while pend:
                xbc_transposes(*pend.pop(0))
            for i in range(4):
                cs_ = slice(i * 128, (i + 1) * 128)
                bs = nb()
                mm(PB(bs)[:, 0:16], triu, dab[:, i, :], True, True, cst.pg() + smal.pg(), PK(bs))
                mm(PB(bs)[:, 16:32], onesf, dab[:, i, :], True, True, cst.pg() + smal.pg(), PK(bs))
                cp(acs, PB(bs)[:, 0:32], PK(bs), smal2.pg())
                act(dec_out, acs[:, 0:16], AF.Exp, smal2.pg(), smal2.pg())
                tt(dec_st, acs[:, 16:32], acs[:, 0:16], ALU.subtract, smal2.pg(), smal2.pg())
                act(dec_st, dec_st, AF.Exp, smal2.pg(), smal2.pg())
                act(cdec, acs[:, 16:32], AF.Exp, smal2.pg(), smal2.pg())
                xs3 = xs_tok.ap[:, i, :].rearrange("p (h d) -> p h d", h=16)
                v3 = lambda b_: b_.ap.rearrange("p (h d) -> p h d", h=16)
                bc16 = lambda a_: a_.unsqueeze(2).broadcast_to([128, 16, 64])
                tt(v3(xdt), xs3, bc16(dtb[:, i, :]), ALU.mult, xs_tok.pg() + smal.pg(), xdt.pg())
                tt(v3(xdd), v3(xdt), bc16(dec_st), ALU.mult, xdt.pg() + smal2.pg(), xdd.pg())
                tt(rhsm.ap.rearrange("p (h l) -> p h l", h=16), triu.unsqueeze(1).broadcast_to([128, 16, 128]),
                   dab[:, i, :].unsqueeze(2).broadcast_to([128, 16, 128]), ALU.mult, cst.pg() + smal.pg(), rhsm.pg())
                b4 = nb4()
                for q in range(4):
                    mm(PB(b4 + q), slow, rhsm.ap[:, q * 512:(q + 1) * 512], True, True, cst.pg() + rhsm.pg(), PK(b4 + q))
                for q in range(4):
                    act(Eb.ap[:, q * 4:(q + 1) * 4, :], PB(b4 + q).rearrange("p (h l) -> p h l", h=4), AF.Exp, PK(b4 + q), Eb.pg())
                bcb = nb()
                for g in range(2):
                    mm(PB(bcb)[:, g * 128:(g + 1) * 128], bcT.ap[:, g, cs_], bcT.ap[:, 2 + g, cs_], True, True, bcT.pg(), PK(bcb))
                tt(cbm.ap, PB(bcb)[:, 0:256].rearrange("p (g l) -> p g l", g=2), causT.unsqueeze(1).broadcast_to([128, 2, 128]),
                   ALU.mult, PK(bcb) + cst.pg(), cbm.pg())
                for g in range(2):
                    tt(MTb.ap[:, 8 * g:8 * g + 8, :], Eb.ap[:, 8 * g:8 * g + 8, :],
                       cbm.ap[:, g, :].unsqueeze(1).broadcast_to([128, 8, 128]), ALU.mult, Eb.pg() + cbm.pg(), MTb.pg())
                bo = nb2()
                for g in range(2):
                    mm(PB(bo, 2)[:, g * 512:(g + 1) * 512], bcT.ap[:, 2 + g, cs_], sstb.ap[:, g * 512:(g + 1) * 512], True, True,
                       bcT.pg() + sstb.pg(), PK(bo, 2))
                by = nb2()
                for h in range(16):
                    mm(PB(by, 2)[:, h * 64:(h + 1) * 64], MTb.ap[:, h, :], xdt.ap[:, h * 64:(h + 1) * 64], True, True,
                       MTb.pg() + xdt.pg(), PK(by, 2))
                y0, y1 = yf
                tt(v3(y0), PB(bo, 2).rearrange("p (h d) -> p h d", h=16), bc16(dec_out), ALU.mult, PK(bo, 2) + smal2.pg(), y0.pg())
                tt(y0.ap, PB(by, 2), y0.ap, ALU.add, PK(by, 2) + y0.pg(), y0.pg())
                tt(v3(y1), xs3, bc16(pvc(l, 180, 16)), ALU.mult, xs_tok.pg() + pv.pg(), y1.pg())
                tt(y0.ap, y0.ap, y1.ap, ALU.add, y0.pg() + y1.pg(), y0.pg())
                bst = nb2()
                for g in range(2):
                    mm(PB(bst, 2)[:, g * 512:(g + 1) * 512], B_tok.ap[:, i, g * 128:(g + 1) * 128], xdd.ap[:, g * 512:(g + 1) * 512],
                       True, True, B_tok.pg() + xdd.pg(), PK(bst, 2))
                tt(v3(sst), v3(sst), bc16(cdec), ALU.mult, sst.pg() + smal2.pg(), sst.pg())
                tt(sst.ap, sst.ap, PB(bst, 2), ALU.add, sst.pg() + PK(bst, 2), sst.pg())
                cp(sstb.ap, sst.ap, sst.pg(), sstb.pg())
                tt(y0.ap, y0.ap, sz.ap[:, i, :], ALU.mult, y0.pg() + sz.pg(), y0.pg())
                ms(ssq, 0.0, smal2.pg())
                act(y1.ap, y0.ap, AF.Square, y0.pg(), y1.pg() + smal2.pg(), accum_out=ssq)
                act(ssq, ssq, AF.Ln, smal2.pg(), smal2.pg(), bias=EPS, scale=1.0 / 1024.0)
                act(ssq, ssq, AF.Exp, smal2.pg(), smal2.pg(), scale=-0.5)
                if i > 0:
                    to_featmajor(i - 1, l, 48)
                ts1(ynb.ap, y0.ap, ssq, ALU.mult, y0.pg() + smal2.pg(), ynb.pg())
            to_featmajor(3, l, 48)

        def ret_tile(l, ti):
            wl = win_d[l]
            t0 = ti * T
            dma("sp", cst_t.ap, cs_d[:, :, t0:t0 + T].rearrange("a p t -> p a t"), (), cst_t.pg(), "cs")
            cosv, sinv = cst_t.ap[:, 0, :], cst_t.ap[:, 1, :]
            qcnt = [0]

            def qk_consumer(which, scale):
                def f(h, b):
                    q_ = qf[qcnt[0] % 2]
                    t1 = t12[qcnt[0] % 2]
                    qcnt[0] += 1
                    act(q_.ap, PB(b), AF.Copy, PK(b), q_.pg(), scale=scale)
                    b2 = nb()
                    mm(PB(b2), Rm, q_.ap, True, True, cst.pg() + q_.pg(), PK(b2))
                    tt(t1.ap, PB(b2), sinv, ALU.mult, PK(b2) + cst_t.pg(), t1.pg())
                    tt(q_.ap, q_.ap, cosv, ALU.mult, q_.pg() + cst_t.pg(), q_.pg())
                    dst = which.ap[:, h, :]
                    tt(dst, q_.ap, t1.ap, ALU.add, q_.pg() + t1.pg(), which.pg(h * 1024, (h + 1) * 1024))
                return f

            proj_fm(wl, C_RQ, 4, qk_consumer(qTr, 1.0))
            proj_fm(wl, C_RK, 4, qk_consumer(kTr, 128.0 ** -0.5))
            for s2 in range(2):
                proj_tm(wl, C_RV + s2 * 512, 512,
                        lambda i, b, s2=s2: act(v_tok.ap[:, i, s2 * 512:(s2 + 1) * 512], PB(b), AF.Copy, PK(b), v_tok.pg()))
            for s2 in range(2):
                proj_tm(wl, C_RG + s2 * 512, 512,
                        lambda i, b, s2=s2: act(srg.ap[:, i, s2 * 512:(s2 + 1) * 512], PB(b), AF.Silu, PK(b), srg.pg()))
            for i in range(4):
                cs_ = slice(i * 128, (i + 1) * 128)
                bsd = nb()
                for h in range(4):
                    mm(PB(bsd)[:, h * 128:(h + 1) * 128], kTr.ap[:, h, cs_], qTr.ap[:, h, cs_], True, True, kTr.pg() + qTr.pg(), PK(bsd))
                tt(SDT.ap, PB(bsd).rearrange("p (h i) -> p h i", h=4), dmatT, ALU.mult, PK(bsd) + cst.pg(), SDT.pg())
                bk = nb()
                for h in range(4):
                    tr(PBb(bk)[:, h * 128:(h + 1) * 128], kTr.ap[:, h, cs_], identb, kTr.pg() + cbf.pg(), PK(bk))
                tt(kz.ap, PBb(bk)[:, 0:512].rearrange("p (h d) -> p h d", h=4), zeta.unsqueeze(2).broadcast_to([128, 4, 128]),
                   ALU.mult, PK(bk) + cst.pg(), kz.pg())
                tt(qx.ap, qTr.ap[:, :, cs_], xiT, ALU.mult, qTr.pg() + cst.pg(), qx.pg())
                bo = nb2()
                for h in range(4):
                    o_ = PB(bo, 2)[:, h * 256:(h + 1) * 256]
                    mm(o_, SDT.ap[:, h, :], v_tok.ap[:, i, h * 256:(h + 1) * 256], True, False, SDT.pg() + v_tok.pg(), PK(bo, 2))
                    mm(o_, qx.ap[:, h, :], rstb.ap[:, h * 256:(h + 1) * 256], False, True, qx.pg() + rstb.pg(), PK(bo, 2))
                bkv = nb2()
                for h in range(4):
                    mm(PB(bkv, 2)[:, h * 256:(h + 1) * 256], kz.ap[:, h, :], v_tok.ap[:, i, h * 256:(h + 1) * 256], True, True,
                       kz.pg() + v_tok.pg(), PK(bkv, 2))
                y0, y1 = yf
                act(y0.ap, PB(bo, 2), AF.Copy, PK(bo, 2), y0.pg())
                for h in range(4):
                    sl_ = slice(h * 256, (h + 1) * 256)
                    stt(rst.ap[:, sl_], rst.ap[:, sl_], math.exp(128.0 * LOGG[h]), PB(bkv, 2)[:, sl_], ALU.mult, ALU.add,
                        rst.pg() + PK(bkv, 2), rst.pg())
                cp(rstb.ap, rst.ap, rst.pg(), rstb.pg())
                for h in range(4):
                    S.add("dve", lambda e, h=h: e.bn_stats(out=bn6[:, h, :], in_=y0.ap[:, h * 256:(h + 1) * 256]), y0.pg(), smal2.pg())
                for h in range(4):
                    S.add("dve", lambda e, h=h: e.bn_aggr(out=mv[:, h, :], in_=bn6[:, h, :]), smal2.pg(), smal2.pg())
                act(rstd4, mv[:, :, 1], AF.Ln, smal2.pg(), smal2.pg(), bias=EPS, scale=1.0)
                act(rstd4, rstd4, AF.Exp, smal2.pg(), smal2.pg(), scale=-0.5)
                for h in range(4):
                    sl_ = slice(h * 256, (h + 1) * 256)
                    ts(y0.ap[:, sl_], y0.ap[:, sl_], mv[:, h, 0:1], rstd4[:, h:h + 1], ALU.subtract, ALU.mult,
                       y0.pg() + smal2.pg(), y0.pg())
                if i > 0:
                    to_featmajor(i - 1, l, 56)
                tt(ynb.ap, y0.ap, srg.ap[:, i, :], ALU.mult, y0.pg() + srg.pg(), ynb.pg())
            to_featmajor(3, l, 56)

        def att_tile(l, ti):
            wl = win_d[l]
            ms(qTm.ap, 0.0, qTm.pg())

            def q_consumer(c, b):
                act(qTm.ap[0:64, 2 * c, :], PB(b)[0:64, :], AF.Copy, PK(b), qTm.pg(2 * c * 1024, (2 * c + 2) * 1024), scale=0.125)
                act(qTm.ap[64:128, 2 * c + 1, :], PB(b)[64:128, :], AF.Copy, PK(b), qTm.pg(2 * c * 1024, (2 * c + 2) * 1024), scale=0.125)

            proj_fm(wl, C_AQ, 8, q_consumer)
            s = wring[0][wcnt[0] % len(wring[0])]
            wcnt[0] += 1
            pieces = [(C_AK + g * 64, 64, g * 128 + dup * 64) for g in range(2) for dup in range(2)] + [(C_AV, 128, 256)]

            def kv_dmas(e, s=s, pieces=pieces):
                out_ = []
                for c0, ncols, d0 in pieces:
                    src = wl[0:1024, c0:c0 + ncols].rearrange("(k p) n -> p k n", p=128)
                    out_.append(e.dma_start(out=wsl[s].ap[:, 0:8, d0:d0 + ncols], in_=src))
                return out_

            S.add("pool", kv_dmas, (), wsl[s].pg(), dma=("w", s), ndma=len(pieces))
            for g in range(2):
                b = nb()
                for k in range(8):
                    mm(PB(b), wsl[s].ap[:, k, g * 128:(g + 1) * 128], hT.ap[:, k, :], k == 0, k == 7, wsl[s].pg() + hT.pg(), PK(b))
                act(kTa.ap[:, g, 128:640], PB(b), AF.Copy, PK(b), kTa.pg())
            bv = nb()
            for i in range(4):
                for k in range(8):
                    mm(PB(bv)[:, i * 128:(i + 1) * 128], hT.ap[:, k, i * 128:(i + 1) * 128], wsl[s].ap[:, k, 256:384], k == 0, k == 7,
                       wsl[s].pg() + hT.pg(), PK(bv))
            vsrc = PB(bv).rearrange("p (i g d) -> p i g d", i=4, g=2)
            cp(vtok.ap[:, 1:5, :, 0:64], vsrc, PK(bv), vtok.pg())
            cp(vtok.ap[:, 1:5, :, 64:128], vsrc, PK(bv), vtok.pg())
            scnt = 0
            if stop < 4.3:
                return
            for i in range(4):
                ci = ti * 4 + i
                blks = [1] if ci == 0 else [0, 1]
                for blk in blks:
                    koff = i * 128 + blk * 128
                    for hg in range(4):
                        b = nb()
                        for hh in range(4):
                            h = hg * 4 + hh
                            g = h // 8
                            mm(PB(b)[:, hh * 128:(hh + 1) * 128], kTa.ap[:, g, koff:koff + 128], qTm.ap[:, h, i * 128:(i + 1) * 128],
                               True, True, kTa.pg() + qTm.pg(), PK(b))
                        stp = stmp[scnt % 2]
                        scnt += 1
                        tt(stp.ap.rearrange("p (h q) -> p h q", h=4), PB(b).rearrange("p (h q) -> p h q", h=4),
                           biasT.ap[:, blk, hg * 4:(hg + 1) * 4, :], ALU.add, PK(b) + biasT.pg(), stp.pg())
                        act(PT.ap[:, blk, hg * 4:(hg + 1) * 4, :], stp.ap.rearrange("p (h q) -> p h q", h=4), AF.Exp, stp.pg(), PT.pg())
                if stop < 4.5:
                    continue
                dsb = yf[0]
                for g in range(2):
                    bo = nb2()
                    bd = nb2()
                    for half in range(2):
                        hs = 8 * g + 4 * half
                        for n_, blk in enumerate(blks):
                            rhs_ = PTf.ap[:, blk, hs * 128:(hs + 4) * 128]
                            mm(PB(bo, 2)[:, half * 512:(half + 1) * 512], vtok.ap[:, i + blk, g, :], rhs_,
                               n_ == 0, n_ == len(blks) - 1, PT.pg() + vtok.pg(), PK(bo, 2))
                    if stop < 4.7:
                        continue
                    for half in range(2):
                        hs = 8 * g + 4 * half
                        for n_, blk in enumerate(blks):
                            rhs_ = PTf.ap[:, blk, hs * 128:(hs + 4) * 128]
                            mm(PB(bd, 2)[:, half * 512:(half + 1) * 512], cbf.ap[:, 256:384], rhs_,
                               n_ == 0, n_ == len(blks) - 1, PT.pg() + cbf.pg(), PK(bd, 2))
                    if stop < 4.9:
                        continue
                    tt(dsb.ap.rearrange("p (h q) -> p h q", h=8), PB(bd, 2).rearrange("p (h q) -> p h q", h=8),
                       esink[:, 8 * g:8 * g + 8].unsqueeze(2).broadcast_to([128, 8, 128]), ALU.add, PK(bd, 2) + smal.pg(), dsb.pg())
                    S.add("dve", lambda e: e.reciprocal(out=dsb.ap, in_=dsb.ap), dsb.pg(), dsb.pg())
                    o4 = PB(bo, 2).rearrange("p (j two q) -> p j two q", j=4, two=2)
                    d4 = dsb.ap.rearrange("p (j two q) -> p j two q", j=4, two=2)
                    qs = slice(i * 128, (i + 1) * 128)
                    tt(yT.ap[0:64, 4 * g:4 * g + 4, qs], o4[0:64, :, 0, :], d4[0:64, :, 0, :], ALU.mult, PK(bo, 2) + dsb.pg(), yT.pg())
                    tt(yT.ap[64:128, 4 * g:4 * g + 4, qs], o4[64:128, :, 1, :], d4[64:128, :, 1, :], ALU.mult, PK(bo, 2) + dsb.pg(), yT.pg())
            cp(vtok.ap[:, 0, :, :], vtok.ap[:, 4, :, :], vtok.pg(), vtok.pg())
            cp(kTa.ap[:, :, 0:128], kTa.ap[:, :, 512:640], kTa.pg(), kTa.pg())

        xin = yf[0]

        def load_tile(l, ti):
            t0 = ti * T
            if l == 0:
                for i in range(4):
                    dma("sp", xin.ap, x_d[t0 + i * 128:t0 + (i + 1) * 128, :], (), xin.pg(), "xin")
                    b = nb2()
                    for c in range(8):
                        tr(PB(b, 2)[:, c * 128:(c + 1) * 128], xin.ap[:, c * 128:(c + 1) * 128], identf, xin.pg() + cst.pg(), PK(b, 2))
                    cp(xt.ap[:, :, i * 128:(i + 1) * 128], PB(b, 2).rearrange("p (c t) -> p c t", c=8), PK(b, 2), xt.pg())
            else:
                dma("sp", xt.ap, xs_d[:, :, t0:t0 + T], [("xd", ti)], xt.pg(), "xt")

        def store_tile(l, ti):
            t0 = ti * T
            if l == L - 1:
                for i in range(4):
                    b = nb2()
                    for c in range(8):
                        tr(PB(b, 2)[:, c * 128:(c + 1) * 128], xt.ap[:, c, i * 128:(i + 1) * 128], identf, xt.pg() + cst.pg(), PK(b, 2))
                    cp(xin.ap, PB(b, 2), PK(b, 2), xin.pg())
                    dma("sp", out_d[t0 + i * 128:t0 + (i + 1) * 128, :], xin.ap, xin.pg(), [("od", ti, i)], "xout")
            else:
                dma("sp", xs_d[:, :, t0:t0 + T], xt.ap, xt.pg(), [("xd", ti)], "xst")

        for l in range(L):
            ms(sst.ap, 0.0, sst.pg())
            ms(sstb.ap, 0.0, sstb.pg())
            ms(rst.ap, 0.0, rst.pg())
            ms(rstb.ap, 0.0, rstb.pg())
            ms(hist.ap, 0.0, hist.pg())
            ms(kTa.ap, 0.0, kTa.pg())
            act(a_b, pvc(l, 164, 16), AF.Exp, pv.pg(), smal.pg())
            ts1(a_b, a_b, -1.0, ALU.mult, smal.pg(), smal.pg())
            act(esink, pvc(l, 196, 16), AF.Exp, pv.pg(), smal.pg())
            for ti in range(NT):
                load_tile(l, ti)
                if stop >= 1:
                    ffn(l, w1i_d, w1o_d, 0, 8)
                if stop >= 2:
                    pre_norm(l, 16)
                    ssd_tile(l, ti)
                    if dbg and l == 0:
                        dma("pool", dbg_d[0, :, :, ti * T:(ti + 1) * T], yT.ap, yT.pg(), [("dbg", 0, ti)], "dbg")
                if stop >= 3:
                    merge(l, 0)
                if stop >= 4:
                    ret_tile(l, ti)
                    if dbg and l == 0:
                        dma("pool", dbg_d[1, :, :, ti * T:(ti + 1) * T], yT.ap, yT.pg(), [("dbg", 1, ti)], "dbg")
                    merge(l, 1)
                if stop > 4.1:
                    att_tile(l, ti)
                    if dbg and l == 0:
                        dma("pool", dbg_d[2, :, :, ti * T:(ti + 1) * T], yT.ap, yT.pg(), [("dbg", 2, ti)], "dbg")
                    if stop >= 5:
                        merge(l, 2)
                if stop >= 6:
                    out_proj(l)
                if stop >= 7:
                    ffn(l, w2i_d, w2o_d, 32, 40)
                store_tile(l, ti)
        S.emit()
    return nc


def _t5_bucket(dist):
    is_small = dist < 16
    d = np.maximum(dist, 1).astype(np.float32)
    large = 16 + (np.log(d / 16) / math.log(128 / 16) * 16).astype(np.int32)
    large = np.minimum(large, 31)
    return np.where(is_small, dist, large)


def _consts(ntok):
    idx = np.arange(128)
    cst = np.zeros((128, NCST), np.float32)
    cst[:, 0:128] = np.eye(128)
    cst[:, 128:256] = (idx[:, None] <= idx[None, :])
    cst[:, 256:384] = 1.0
    cst[:, 384:512] = (idx[:, None] > idx[None, :])
    cst[:, 512:640] = (idx[None, :] >= idx[:, None])
    R = np.zeros((128, 128), np.float32)
    for dp in range(64):
        R[dp + 64, dp] = -1.0
        R[dp, dp + 64] = 1.0
    cst[:, 640:768] = R
    lg = np.array(LOGG, np.float64)
    diff = idx[None, :] - idx[:, None]
    dm = np.where(diff[None] >= 0, np.exp(np.maximum(diff, 0)[None] * lg[:, None, None]), 0.0)
    cst[:, 768:1280] = np.transpose(dm, (1, 0, 2)).reshape(128, 512)
    xi = np.exp((idx + 1.0)[None, :] * lg[:, None])
    cst[:, 1280:1792] = np.broadcast_to(xi.reshape(1, 512), (128, 512))
    cst[:, 1792:1796] = np.exp((127.0 - idx)[:, None] * lg[None, :])
    pos = np.arange(ntok, dtype=np.float32)
    inv = (1.0 / (np.float32(10000.0) ** (np.arange(0, 128, 2, dtype=np.float32) / np.float32(128)))).astype(np.float32)
    ang = pos[:, None] * inv[None, :]
    cos, sin = np.cos(ang).astype(np.float32).T, np.sin(ang).astype(np.float32).T
    cs = np.stack([np.concatenate([cos, cos], 0), np.concatenate([sin, sin], 0)], 0).astype(np.float32)
    return cst, np.ascontiguousarray(cs)


def _bias_table(rel_bias):
    qi = np.arange(128)[:, None]
    sj = np.arange(256)[None, :]
    dist = qi + 128 - sj
    mask = (dist >= 0) & (dist < 128)
    bb = rel_bias[_t5_bucket(np.maximum(dist, 0))]
    bb = np.where(mask[:, :, None], bb, np.float32(-1e30)).astype(np.float32)
    t = bb.reshape(128, 2, 128, 16)
    t = np.transpose(t, (2, 1, 3, 0))
    return np.ascontiguousarray(t.reshape(128, 2 * 16 * 128))


def _pack_params(L, p):
    fm = lambda v, n: v.reshape(n, 128).T
    pv = np.zeros((128, L, NV), np.float32)
    for l in range(L):
        for j, nm in enumerate(["ffn1_pre_g", "ffn1_post_g", "mix_pre_g", "mix_post_g", "ffn2_pre_g", "ffn2_post_g",
                                "ssd_norm_g", "ret_gn_g"]):
            pv[:, l, j * 8:(j + 1) * 8] = fm(p[nm][l], 8)
        cw = p["conv_w"][l]
        pv[:, l, 64:112] = np.transpose(cw.reshape(4, 12, 128), (2, 1, 0)).reshape(128, 48)
        pv[:, l, 112:124] = fm(p["conv_b"][l], 12)
        pv[:, l, 124:148] = fm(p["b_gate"][l], 24)
        pv[:, l, 148:164] = p["dt_bias"][l][None, :]
        pv[:, l, 164:180] = p["a_log"][l][None, :]
        pv[:, l, 180:196] = p["d_skip"][l][None, :]
        pv[:, l, 196:212] = p["attn_sinks"][l][None, :]
    return np.ascontiguousarray(pv.reshape(128, L * NV))


_NC_CACHE = {}


def run(inputs, L=DEPTH, ntok=SEQ, ncores=8, dbg=False, stop=99):
    p = {k: np.asarray(v, dtype=np.float32) for k, v in inputs.items()}
    key = (L, ntok, dbg, stop)
    if key not in _NC_CACHE:
        _NC_CACHE[key] = build(L, ntok, dbg, stop)
    nc = _NC_CACHE[key]
    cst, cs = _consts(ntok)
    shared = {
        "w1i": np.ascontiguousarray(p["w_ffn1_in"][:L]), "w1o": np.ascontiguousarray(p["w_ffn1_out"][:L]),
        "w2i": np.ascontiguousarray(p["w_ffn2_in"][:L]), "w2o": np.ascontiguousarray(p["w_ffn2_out"][:L]),
        "win": np.ascontiguousarray(p["w_in"][:L]), "wbr": np.ascontiguousarray(p["w_branch"][:L]),
        "wo": np.ascontiguousarray(p["w_out"][:L]),
        "pv": _pack_params(L, p), "cst": cst, "cs": cs, "biasT": _bias_table(p["rel_bias"]),
    }
    in_maps = []
    for b in range(ncores):
        m = dict(shared)
        m["x"] = np.ascontiguousarray(p["x"][b, :ntok])
        in_maps.append(m)
    res = run_bass_kernel_spmd(nc, in_maps, core_ids=list(range(ncores)))
    return res


def kernel(**inputs):
    res = run(inputs)
    return np.stack([np.asarray(r["out"], dtype=np.float32) for r in res.results], axis=0)
```

```python
import math
from contextlib import ExitStack
import numpy as np
import concourse.bass as bass
import concourse.mybir as mybir
from concourse.bass_utils import run_bass_kernel_spmd

F32 = mybir.dt.float32
BF16 = mybir.dt.bfloat16
AF = mybir.ActivationFunctionType
ALU = mybir.AluOpType

D = 1024
DFF = 2816
INW = 10000
DEPTH = 4
SEQ = 2048
T = 512
EPS = 1e-6
NV = 212
NCST = 1796
ENGS = ("pe", "act", "dve", "pool", "sp")
C_Z, C_XBC, C_DT, C_RQ, C_RK, C_RV, C_RG, C_AQ, C_AK, C_AV, C_GATE = 0, 1024, 2560, 2576, 3088, 3600, 4624, 5648, 6672, 6800, 6928
LOGG = [math.log(1.0 - 2.0 ** (-5.0 - h)) for h in range(4)]


class Op:
    __slots__ = ("eng", "fn", "reads", "writes", "dma", "waits", "signal", "semkey", "deps", "sigval", "idx", "need", "ndma")

    def __init__(self, eng, fn, reads, writes, dma):
        self.eng, self.fn, self.reads, self.writes, self.dma = eng, fn, reads, writes, dma
        self.waits = {}
        self.signal = False
        self.semkey = None


class Sched:
    def __init__(self, nc):
        self.nc = nc
        self.ops = []
        self.last_writer = {}
        self.readers = {}

    def add(self, eng, fn, reads=(), writes=(), dma=None, ndma=1):
        op = Op(eng, fn, tuple(reads), tuple(writes), dma)
        op.ndma = ndma
        deps = set()
        for r in op.reads:
            w = self.last_writer.get(r)
            if w is not None:
                deps.add(w)
        for r in op.writes:
            w = self.last_writer.get(r)
            if w is not None:
                deps.add(w)
            for rd in self.readers.get(r, ()):
                deps.add(rd)
        op.deps = deps
        for r in op.reads:
            self.readers.setdefault(r, []).append(op)
        for r in op.writes:
            self.last_writer[r] = op
            self.readers[r] = []
        self.ops.append(op)
        return op

    @staticmethod
    def _skip(d, op):
        return d is op or (d.dma is None and op.dma is None and d.eng == "pe" and op.eng == "pe")

    def finalize(self):
        counters = {}
        for i, op in enumerate(self.ops):
            op.idx = i
        for op in self.ops:
            best = {}
            for d in op.deps:
                if self._skip(d, op):
                    continue
                k = ("dma", d.dma) if d.dma is not None else ("eng", d.eng)
                if k not in best or d.idx > best[k].idx:
                    best[k] = d
            op.need = list(best.values())
            for d in op.need:
                d.signal = True
        for op in self.ops:
            if op.dma is not None:
                op.semkey = ("dma", op.dma)
                op.signal = True
            else:
                op.semkey = ("eng", op.eng)
            if op.signal:
                counters[op.semkey] = counters.get(op.semkey, 0) + (16 * op.ndma if op.dma is not None else 1)
                op.sigval = counters[op.semkey]
        seen = {e: {} for e in ENGS}
        for op in self.ops:
            for d in op.need:
                k, v = d.semkey, d.sigval
                if seen[op.eng].get(k, 0) >= v:
                    continue
                if op.waits.get(k, 0) < v:
                    op.waits[k] = v
            for k, v in op.waits.items():
                seen[op.eng][k] = v
        self.semkeys = list(counters.keys())
        self.final_counts = counters

    def emit(self):
        nc = self.nc
        self.finalize()
        with ExitStack() as st:
            sems = {}
            for i, k in enumerate(self.semkeys):
                sems[k] = st.enter_context(nc.semaphore("s%d" % i))
            block = st.enter_context(nc.Block())
            per = {e: [op for op in self.ops if op.eng == e] for e in ENGS}

            def run(e, ops):
                for op in ops:
                    for k, v in op.waits.items():
                        e.wait_ge(sems[k], v)
                    ins = op.fn(e)
                    if op.signal:
                        if isinstance(ins, list):
                            for x_ in ins:
                                x_.then_inc(sems[op.semkey], 16)
                        else:
                            ins.then_inc(sems[op.semkey], 16 if op.dma is not None else 1)

            @block.tensor
            def _(e):
                run(e, per["pe"])

            @block.scalar
            def _(e):
                run(e, per["act"])

            @block.vector
            def _(e):
                run(e, per["dve"])

            @block.gpsimd
            def _(e):
                run(e, per["pool"])

            @block.sync
            def _(e):
                run(e, per["sp"])
                for k in self.semkeys:
                    e.wait_ge(sems[k], self.final_counts[k])


class Buf:
    def __init__(self, mem, off, nbytes, dtype, shape=None):
        self.off, self.nbytes = off, nbytes
        if shape is not None:
            n = 1
            for d_ in shape:
                n *= d_
            assert n * (4 if dtype == F32 else 2) == nbytes, (shape, nbytes)
        v = mem[:, off // 2:(off + nbytes) // 2]
        if dtype == F32:
            v = v.bitcast(F32)
        if shape is not None and len(shape) == 2:
            v = v.rearrange("p (a b) -> p a b", a=shape[0])
        elif shape is not None and len(shape) == 3:
            v = v.rearrange("p (a b c) -> p a b c", a=shape[0], b=shape[1])
        self.ap = v

    def pg(self, lo=0, hi=None):
        hi = self.nbytes if hi is None else hi
        return [("pg", i) for i in range((self.off + lo) // 1024, (self.off + hi - 1) // 1024 + 1)]


def build(L=DEPTH, NTOK=SEQ, dbg=False, stop=99):
    NT = NTOK // T
    nc = bass.Bass("TRN2", target_bir_lowering=False)
    dr = lambda n, s, k="ExternalInput": nc.dram_tensor(n, s, F32, kind=k).ap()
    x_d = dr("x", [NTOK, D])
    w1i_d, w1o_d = dr("w1i", [L, D, 2 * DFF]), dr("w1o", [L, DFF, D])
    w2i_d, w2o_d = dr("w2i", [L, D, 2 * DFF]), dr("w2o", [L, DFF, D])
    win_d, wbr_d, wo_d = dr("win", [L, D, INW]), dr("wbr", [L, 3, D, D]), dr("wo", [L, D, D])
    pv_d, cst_d = dr("pv", [128, L * NV]), dr("cst", [128, NCST])
    cs_d = dr("cs", [2, 128, NTOK])
    bias_d = dr("biasT", [128, 2 * 16 * 128])
    out_d = dr("out", [NTOK, D], "ExternalOutput")
    xs_d = dr("xscr", [128, 8, NTOK], "Internal")
    dbg_d = dr("dbgo", [3, 128, 8, NTOK], "ExternalOutput") if dbg else None

    with ExitStack() as st:
        TOTAL = 206 * 1024
        mem = st.enter_context(nc.sbuf_tensor("mem", [128, TOTAL // 2], BF16))
        ps = st.enter_context(nc.psum_tensor("ps", [128, 4096], F32))
        S = Sched(nc)
        cur = [0]

        def alloc(nbytes, dtype, shape=None):
            nb_ = (nbytes + 1023) // 1024 * 1024
            b = Buf(mem, cur[0], nbytes, dtype, shape)
            cur[0] += nb_
            assert cur[0] <= TOTAL, cur[0]
            return b

        cst = alloc(NCST * 4, F32)
        pv = alloc(L * NV * 4, F32)
        cbf = alloc(3 * 128 * 2, BF16)
        biasT = alloc(2 * 16 * 128 * 2, BF16, (2, 16, 128))
        xt = alloc(8 * T * 4, F32, (8, T))
        hT = alloc(8 * T * 2, BF16, (8, T))
        ytmp = alloc(8 * T * 4, F32, (8, T))
        rs = alloc(T * 4, F32)
        wsl = [alloc(8 * 512 * 2, BF16, (8, 512)) for _ in range(3)]
        ysum = alloc(8 * T * 4, F32, (8, T))
        yT = alloc(8 * T * 2, BF16, (8, T))
        etmp = [alloc(T * 4, F32) for _ in range(2)]
        sst = alloc(1024 * 4, F32)
        sstb = alloc(1024 * 2, BF16)
        rst = alloc(1024 * 4, F32)
        rstb = alloc(1024 * 2, BF16)
        hist = alloc(12 * 3 * 4, F32, (12, 3))
        kTa = alloc(2 * 640 * 2, BF16, (2, 640))
        vtok = alloc(5 * 2 * 128 * 2, BF16, (5, 2, 128))
        smal = alloc(1024, F32)
        smal2 = alloc(1024, F32)
        A0 = cur[0]

        def arena(off, nbytes, dtype, shape=None):
            assert A0 + off + nbytes <= TOTAL, (A0, off, nbytes)
            return Buf(mem, A0 + off, nbytes, dtype, shape)

        K = 1024
        sq = arena(0, 8 * K, BF16, (8, T))
        hid = arena(8 * K, 22 * K, BF16, (22, T))
        wsl.extend(arena(30 * K + i * 8 * K, 8 * K, BF16, (8, 512)) for i in range(4))
        wring = [[0, 1, 2]]
        ynb = arena(0, 2 * K, BF16)
        yf = [arena(2 * K + i * 4 * K, 4 * K, F32) for i in range(2)]
        MX = 10 * K
        sz = arena(MX, 8 * K, BF16, (4, 1024))
        xs_tok = arena(MX + 8 * K, 8 * K, BF16, (4, 1024))
        B_tok = arena(MX + 16 * K, 2 * K, BF16, (4, 256))
        bcT = arena(MX + 18 * K, 4 * K, BF16, (4, T))
        raw = [arena(MX + 22 * K + i * 3 * K, 515 * 4, F32) for i in range(2)]
        cacc = [arena(MX + 28 * K + i * 2 * K, 2 * K, F32) for i in range(2)]
        xsTt = [arena(MX + 32 * K + i * K, K, BF16) for i in range(2)]
        xdt = arena(MX + 34 * K, 2 * K, BF16)
        xdd = arena(MX + 36 * K, 2 * K, BF16)
        rhsm = arena(MX + 38 * K, 8 * K, F32)
        Eb = arena(MX + 46 * K, 4 * K, BF16, (16, 128))
        MTb = arena(MX + 50 * K, 4 * K, BF16, (16, 128))
        cbm = arena(MX + 54 * K, 512, BF16, (2, 128))
        qf = [arena(MX + i * 2 * K, 2 * K, F32) for i in range(2)]
        t12 = [arena(MX + 4 * K + i * 2 * K, 2 * K, F32) for i in range(2)]
        qTr = arena(MX + 8 * K, 4 * K, BF16, (4, T))
        kTr = arena(MX + 12 * K, 4 * K, BF16, (4, T))
        v_tok = arena(MX + 16 * K, 8 * K, BF16, (4, 1024))
        srg = arena(MX + 24 * K, 8 * K, BF16, (4, 1024))
        SDT = arena(MX + 32 * K, K, BF16, (4, 128))
        kz = arena(MX + 33 * K, K, BF16, (4, 128))
        qx = arena(MX + 34 * K, K, BF16, (4, 128))
        cst_t = arena(MX + 35 * K, 4 * K, F32, (2, T))
        qTm = arena(MX, 16 * K, BF16, (16, T))
        stmp = [arena(MX + 16 * K + i * 2 * K, 2 * K, F32) for i in range(2)]
        PT = arena(MX + 20 * K, 8 * K, BF16, (2, 16, 128))
        PTf = arena(MX + 20 * K, 8 * K, BF16, (2, 2048))
        PTb = arena(MX + 28 * K, 8 * K, BF16, (2, 16, 128))
        PTbf = arena(MX + 28 * K, 8 * K, BF16, (2, 2048))

        identf = cst.ap[:, 0:128]
        triu = cst.ap[:, 128:256]
        onesf = cst.ap[:, 256:384]
        slow = cst.ap[:, 384:512]
        causT = cst.ap[:, 512:640]
        Rm = cst.ap[:, 640:768]
        dmatT = cst.ap[:, 768:1280].rearrange("p (h i) -> p h i", h=4)
        xiT = cst.ap[:, 1280:1792].rearrange("p (h i) -> p h i", h=4)
        zeta = cst.ap[:, 1792:1796]
        identb = cbf.ap[:, 0:128]
        onesb = cbf.ap[:, 128:256]
        one1b = cbf.ap[:, 256:257]

        pbank = [0]

        def nb():
            b = pbank[0] % 8
            pbank[0] += 1
            return b

        def nb2():
            if pbank[0] % 2:
                pbank[0] += 1
            b = pbank[0] % 8
            pbank[0] += 2
            return b

        def nb4():
            while pbank[0] % 4:
                pbank[0] += 1
            b = pbank[0] % 8
            pbank[0] += 4
            return b

        PB = lambda b, n=1: ps[:, b * 512:(b + n) * 512]
        PBb = lambda b: ps[:, b * 512:(b + 1) * 512].bitcast(BF16)
        PK = lambda b, n=1: [("ps", b + i) for i in range(n)]

        def mm(out, lhsT, rhs, start, stop, r, w):
            S.add("pe", lambda e: e.matmul(out, lhsT=lhsT, rhs=rhs, start=start, stop=stop), r, w)

        def tr(out, in_, ident, r, w):
            S.add("pe", lambda e: e.transpose(out, in_, ident), r, w)

        def act(out, in_, func, r, w, **kw):
            S.add("act", lambda e: e.activation(out=out, in_=in_, func=func, **kw), r, w)

        def tt(out, in0, in1, op, r, w, eng="dve"):
            S.add(eng, lambda e: e.tensor_tensor(out=out, in0=in0, in1=in1, op=op), r, w)

        def stt(out, in0, scalar, in1, op0, op1, r, w):
            S.add("dve", lambda e: e.scalar_tensor_tensor(out=out, in0=in0, scalar=scalar, in1=in1, op0=op0, op1=op1), r, w)

        def ts(out, in0, s1, s2, op0, op1, r, w):
            S.add("dve", lambda e: e.tensor_scalar(out=out, in0=in0, scalar1=s1, scalar2=s2, op0=op0, op1=op1), r, w)

        def ts1(out, in0, s1, op, r, w):
            S.add("dve", lambda e: e.tensor_single_scalar(out=out, in_=in0, scalar=s1, op=op), r, w)

        def cp(out, in_, r, w, eng="dve"):
            S.add(eng, lambda e: e.tensor_copy(out=out, in_=in_), r, w)

        def ms(ap, val, w, eng="dve"):
            S.add(eng, lambda e: e.memset(ap, val), (), w)

        def dma(eng, out, in_, r, w, key):
            S.add(eng, lambda e: e.dma_start(out=out, in_=in_), r, w, dma=key)

        wcnt = [0]

        def wslab(w2d, k0, nk, c0, ncols, dst_c0=0, slot=None):
            if slot is None:
                slot = wring[0][wcnt[0] % len(wring[0])]
                wcnt[0] += 1
            src = w2d[k0 * 128:(k0 + nk) * 128, c0:c0 + ncols].rearrange("(k p) n -> p k n", p=128)
            dst = wsl[slot].ap[:, 0:nk, dst_c0:dst_c0 + ncols]
            dma("pool", dst, src, (), wsl[slot].pg(), ("w", slot))
            return slot

        dma("sp", cst.ap, cst_d, (), cst.pg(), "cst")
        dma("sp", pv.ap, pv_d, (), pv.pg(), "pv")
        dma("pool", biasT.ap, bias_d.rearrange("p (b h q) -> p b h q", b=2, h=16), (), biasT.pg(), "bias")
        cp(identb, identf, cst.pg(), cbf.pg())
        ts1(onesb, onesf, 1.0 / 1024.0, ALU.mult, cst.pg(), cbf.pg())
        cp(cbf.ap[:, 256:384], onesf, cst.pg(), cbf.pg())

        def pvc(l, c, n=1):
            return pv.ap[:, l * NV + c:l * NV + c + n]

        def stats_from_sq():
            b = nb()
            for c in range(8):
                mm(PB(b), onesb, sq.ap[:, c, :], c == 0, c == 7, sq.pg(c * 1024, (c + 1) * 1024) + cbf.pg(), PK(b))
            act(rs.ap, PB(b), AF.Ln, PK(b), rs.pg(), bias=EPS, scale=1.0)
            act(rs.ap, rs.ap, AF.Exp, rs.pg(), rs.pg(), scale=-0.5)

        def pre_norm(l, gcol):
            for c in range(8):
                act(sq.ap[:, c, :], xt.ap[:, c, :], AF.Square, xt.pg(c * 2048, (c + 1) * 2048), sq.pg(c * 1024, (c + 1) * 1024))
            stats_from_sq()
            for c in range(8):
                stt(hT.ap[:, c, :], xt.ap[:, c, :], pvc(l, gcol + c), rs.ap, ALU.mult, ALU.mult,
                    xt.pg(c * 2048, (c + 1) * 2048) + pv.pg() + rs.pg(), hT.pg(c * 1024, (c + 1) * 1024))

        def evac_y(c, bank):
            yk = ytmp.pg(c * 2048, (c + 1) * 2048)
            act(ytmp.ap[:, c, :], PB(bank), AF.Copy, PK(bank), yk)
            act(sq.ap[:, c, :], ytmp.ap[:, c, :], AF.Square, yk, sq.pg(c * 1024, (c + 1) * 1024))

        def post_norm_add(l, gcol, half):
            stats_from_sq()
            for c in range(8):
                yk = ytmp.pg(c * 2048, (c + 1) * 2048)
                xk = xt.pg(c * 2048, (c + 1) * 2048)
                stt(ytmp.ap[:, c, :], ytmp.ap[:, c, :], pvc(l, gcol + c), rs.ap, ALU.mult, ALU.mult, yk + pv.pg() + rs.pg(), yk)
                stt(xt.ap[:, c, :], ytmp.ap[:, c, :], half, xt.ap[:, c, :], ALU.mult, ALU.add, yk + xk, xk)

        def ffn(l, wi_d, wo_d_, gpre, gpost):
            pre_norm(l, gpre)
            wring[0] = [0, 1, 2, 3, 4, 5, 6]
            wi = wi_d[l]
            wo_ = wo_d_[l]
            j = 0
            ecnt = 0
            for g0 in range(0, DFF, 512):
                gw = min(512, DFF - g0)
                sa = wslab(wi, 0, 8, g0, gw)
                sb = wslab(wi, 0, 8, DFF + g0, gw)
                for jj in range(gw // 128):
                    ba, bb = nb(), nb()
                    for k in range(8):
                        mm(PB(ba), wsl[sa].ap[:, k, jj * 128:(jj + 1) * 128], hT.ap[:, k, :], k == 0, k == 7,
                           wsl[sa].pg() + hT.pg(), PK(ba))
                    for k in range(8):
                        mm(PB(bb), wsl[sb].ap[:, k, jj * 128:(jj + 1) * 128], hT.ap[:, k, :], k == 0, k == 7,
                           wsl[sb].pg() + hT.pg(), PK(bb))
                    et = etmp[ecnt % 2]
                    ecnt += 1
                    act(et.ap, PB(ba), AF.Silu, PK(ba), et.pg())
                    tt(hid.ap[:, j, :], et.ap, PB(bb), ALU.mult, et.pg() + PK(bb), hid.pg(j * 1024, (j + 1) * 1024))
                    j += 1
            for mg in range(2):
                b0 = nb4()
                for kg in range(3):
                    k0 = kg * 8
                    nk = min(8, 22 - k0)
                    s = wslab(wo_, k0, nk, mg * 512, 512)
                    for m in range(4):
                        for k in range(nk):
                            mm(PB(b0 + m), wsl[s].ap[:, k, m * 128:(m + 1) * 128], hid.ap[:, k0 + k, :],
                               kg == 0 and k == 0, kg == 2 and k == nk - 1,
                               wsl[s].pg() + hid.pg((k0 + k) * 1024, (k0 + k + 1) * 1024), PK(b0 + m))
                for m in range(4):
                    c = mg * 4 + m
                    evac_y(c, b0 + m)
            wring[0] = [0, 1, 2]
            post_norm_add(l, gpost, 0.5)

        def proj_fm(wl, col0, nch, consumer, dup=None):
            ci = 0
            while ci < nch:
                n = min(4, nch - ci)
                s = wslab(wl, 0, 8, col0 + ci * 128, n * 128)
                for jj in range(n):
                    b = nb()
                    for k in range(8):
                        mm(PB(b), wsl[s].ap[:, k, jj * 128:(jj + 1) * 128], hT.ap[:, k, :], k == 0, k == 7,
                           wsl[s].pg() + hT.pg(), PK(b))
                    consumer(ci + jj, b)
                ci += n

        def proj_tm(wl, col0, ncols, consumer):
            s = wslab(wl, 0, 8, col0, ncols)
            for i in range(4):
                b = nb()
                for k in range(8):
                    mm(PB(b)[:, 0:ncols], hT.ap[:, k, i * 128:(i + 1) * 128], wsl[s].ap[:, k, 0:ncols], k == 0, k == 7,
                       wsl[s].pg() + hT.pg(), PK(b))
                consumer(i, b)

        def to_featmajor(i, l, gcol):
            b = nb()
            for c in range(8):
                tr(PBb(b)[:, c * 128:(c + 1) * 128], ynb.ap[:, c * 128:(c + 1) * 128], identb, ynb.pg() + cbf.pg(), PK(b))
            src = PBb(b).rearrange("p (c t) -> p c t", c=8)
            dst = yT.ap[:, :, i * 128:(i + 1) * 128]
            if gcol is None:
                cp(dst, src, PK(b), yT.pg())
            else:
                tt(dst, src, pvc(l, gcol, 8).unsqueeze(2).broadcast_to([128, 8, 128]), ALU.mult, PK(b) + pv.pg(), yT.pg())

        def merge(l, m):
            wl = win_d[l]
            wb = wbr_d[l, m]
            ecnt = 0
            for half in range(2):
                sg = wslab(wl, 0, 8, C_GATE + m * 1024 + half * 512, 512)
                sbm = wslab(wb, 0, 8, half * 512, 512)
                for jj in range(4):
                    c = half * 4 + jj
                    bg, bb = nb(), nb()
                    for k in range(8):
                        mm(PB(bg), wsl[sg].ap[:, k, jj * 128:(jj + 1) * 128], hT.ap[:, k, :], k == 0, k == 7,
                           wsl[sg].pg() + hT.pg(), PK(bg))
                    for k in range(8):
                        mm(PB(bb), wsl[sbm].ap[:, k, jj * 128:(jj + 1) * 128], yT.ap[:, k, :], k == 0, k == 7,
                           wsl[sbm].pg() + yT.pg(), PK(bb))
                    et = etmp[ecnt % 2]
                    ecnt += 1
                    act(et.ap, PB(bg), AF.Sigmoid, PK(bg) + pv.pg(), et.pg(), bias=pvc(l, 124 + m * 8 + c), scale=1.0)
                    ysc = ysum.ap[:, c, :]
                    yk = ysum.pg(c * 2048, (c + 1) * 2048)
                    if m == 0:
                        tt(ysc, et.ap, PB(bb), ALU.mult, et.pg() + PK(bb), yk)
                    else:
                        tt(et.ap, et.ap, PB(bb), ALU.mult, et.pg() + PK(bb), et.pg())
                        tt(ysc, ysc, et.ap, ALU.add, et.pg() + yk, yk)

        def out_proj(l):
            cp(hT.ap, ysum.ap, ysum.pg(), hT.pg())
            for half in range(2):
                s = wslab(wo_d[l], 0, 8, half * 512, 512)
                for jj in range(4):
                    c = half * 4 + jj
                    b = nb()
                    for k in range(8):
                        mm(PB(b), wsl[s].ap[:, k, jj * 128:(jj + 1) * 128], hT.ap[:, k, :], k == 0, k == 7,
                           wsl[s].pg() + hT.pg(), PK(b))
                    evac_y(c, b)
            post_norm_add(l, 24, 1.0)

        a_b = smal.ap[:, 0:16]
        esink = smal.ap[:, 16:32]
        dtb = smal.ap[:, 32:96].rearrange("p (i h) -> p i h", i=4)
        dab = smal.ap[:, 96:160].rearrange("p (i h) -> p i h", i=4)
        acs = smal2.ap[:, 0:32]
        dec_out = smal2.ap[:, 32:48]
        dec_st = smal2.ap[:, 48:64]
        cdec = smal2.ap[:, 64:80]
        ssq = smal2.ap[:, 80:81]
        bn6 = smal2.ap[:, 96:120].rearrange("p (h s) -> p h s", h=4)
        mv = smal2.ap[:, 120:128].rearrange("p (h s) -> p h s", h=4)
        rstd4 = smal2.ap[:, 128:132]
        den = smal2.ap[:, 136:152]

        def ssd_tile(l, ti):
            wl = win_d[l]
            for s2 in range(2):
                proj_tm(wl, C_Z + s2 * 512, 512,
                        lambda i, b, s2=s2: act(sz.ap[:, i, s2 * 512:(s2 + 1) * 512], PB(b), AF.Silu, PK(b), sz.pg()))
            sl = wslab(wl, 0, 8, C_DT, 16)
            bd = nb()
            for i in range(4):
                for k in range(8):
                    mm(PB(bd)[:, i * 16:(i + 1) * 16], hT.ap[:, k, i * 128:(i + 1) * 128], wsl[sl].ap[:, k, 0:16], k == 0, k == 7,
                       wsl[sl].pg() + hT.pg(), PK(bd))
            tt(dtb, PB(bd)[:, 0:64].rearrange("p (i h) -> p i h", i=4), pvc(l, 148, 16).unsqueeze(1).broadcast_to([128, 4, 16]),
               ALU.add, PK(bd) + pv.pg(), smal.pg())
            act(dtb, dtb, AF.Exp, smal.pg(), smal.pg())
            act(dtb, dtb, AF.Ln, smal.pg(), smal.pg(), bias=1.0, scale=1.0)
            tt(dab, dtb, a_b.unsqueeze(1).broadcast_to([128, 4, 16]), ALU.mult, smal.pg(), smal.pg())
            ccnt = [0]

            def xbc_consumer(c, b):
                rw = raw[ccnt[0] % 2]
                ca = cacc[ccnt[0] % 2]
                xtt = xsTt[ccnt[0] % 2]
                ccnt[0] += 1
                cp(rw.ap[:, 0:3], hist.ap[:, c, :], hist.pg(), rw.pg())
                act(rw.ap[:, 3:515], PB(b), AF.Copy, PK(b), rw.pg())
                cp(hist.ap[:, c, :], rw.ap[:, 512:515], rw.pg(), hist.pg())
                wc = lambda k: pvc(l, 64 + c * 4 + k)
                ts1(ca.ap, rw.ap[:, 0:512], wc(0), ALU.mult, rw.pg() + pv.pg(), ca.pg())
                for k in range(1, 4):
                    stt(ca.ap, rw.ap[:, k:k + 512], wc(k), ca.ap, ALU.mult, ALU.add, rw.pg() + pv.pg() + ca.pg(), ca.pg())
                if c < 8:
                    act(xtt.ap, ca.ap, AF.Silu, ca.pg() + pv.pg(), xtt.pg(), bias=pvc(l, 112 + c), scale=1.0)
                    dst_src = xtt
                else:
                    act(bcT.ap[:, c - 8, :], ca.ap, AF.Silu, ca.pg() + pv.pg(), bcT.pg(), bias=pvc(l, 112 + c), scale=1.0)
                if c < 10:
                    pend.append((c, xtt))

            def xbc_transposes(c, xtt):
                bt = nb()
                for i in range(4):
                    src = xtt.ap[:, i * 128:(i + 1) * 128] if c < 8 else bcT.ap[:, c - 8, i * 128:(i + 1) * 128]
                    tr(PBb(bt)[:, i * 128:(i + 1) * 128], src, identb, (xtt.pg() if c < 8 else bcT.pg()) + cbf.pg(), PK(bt))
                srcv = PBb(bt)[:, 0:512].rearrange("p (i f) -> p i f", i=4)
                if c < 8:
                    cp(xs_tok.ap[:, :, c * 128:(c + 1) * 128], srcv, PK(bt), xs_tok.pg())
                else:
                    cp(B_tok.ap[:, :, (c - 8) * 128:(c - 7) * 128], srcv, PK(bt), B_tok.pg())

            pend = []

            def xbc_consumer2(c, b):
                if len(pend) >= 2:
                    xbc_transposes(*pend.pop(0))
                xbc_consumer(c, b)

            proj_fm(wl, C_XBC, 12, xbc_consumer2)
            while pend:
                xbc_transposes(*pend.pop(0))
            v3 = lambda b_: b_.ap.rearrange("p (h d) -> p h d", h=16)
            bc16 = lambda a_: a_.unsqueeze(2).broadcast_to([128, 16, 64])
            MT2 = [MTb, Buf(mem, etmp[0].off, 4 * K, BF16, (16, 128))]
            SV = lambda st_: (smal2.ap[:, 160 * st_ + 0:160 * st_ + 32], smal2.ap[:, 160 * st_ + 32:160 * st_ + 48],
                              smal2.ap[:, 160 * st_ + 48:160 * st_ + 64], smal2.ap[:, 160 * st_ + 64:160 * st_ + 80])
            svk = lambda st_: [("sv", st_)]

            def S1(i):
                st_ = i % 2
                acs_, dec_out_, dec_st_, cdec_ = SV(st_)
                cs_ = slice(i * 128, (i + 1) * 128)
                bs = nb()
                mm(PB(bs)[:, 0:16], triu, dab[:, i, :], True, True, cst.pg() + smal.pg(), PK(bs))
                mm(PB(bs)[:, 16:32], onesf, dab[:, i, :], True, True, cst.pg() + smal.pg(), PK(bs))
                cp(acs_, PB(bs)[:, 0:32], PK(bs), svk(st_))
                act(dec_out_, acs_[:, 0:16], AF.Exp, svk(st_), svk(st_))
                tt(dec_st_, acs_[:, 16:32], acs_[:, 0:16], ALU.subtract, svk(st_), svk(st_))
                act(dec_st_, dec_st_, AF.Exp, svk(st_), svk(st_))
                act(cdec_, acs_[:, 16:32], AF.Exp, svk(st_), svk(st_))
                tt(rhsm.ap.rearrange("p (h l) -> p h l", h=16), triu.unsqueeze(1).broadcast_to([128, 16, 128]),
                   dab[:, i, :].unsqueeze(2).broadcast_to([128, 16, 128]), ALU.mult, cst.pg() + smal.pg(), rhsm.pg())
                b4 = nb4()
                for q in range(4):
                    mm(PB(b4 + q), slow, rhsm.ap[:, q * 512:(q + 1) * 512], True, True, cst.pg() + rhsm.pg(), PK(b4 + q))
                for q in range(4):
                    act(Eb.ap[:, q * 4:(q + 1) * 4, :], PB(b4 + q).rearrange("p (h l) -> p h l", h=4), AF.Exp, PK(b4 + q), Eb.pg())
                bcb = nb()
                for g in range(2):
                    mm(PB(bcb)[:, g * 128:(g + 1) * 128], bcT.ap[:, g, cs_], bcT.ap[:, 2 + g, cs_], True, True, bcT.pg(), PK(bcb))
                tt(cbm.ap, PB(bcb)[:, 0:256].rearrange("p (g l) -> p g l", g=2), causT.unsqueeze(1).broadcast_to([128, 2, 128]),
                   ALU.mult, PK(bcb) + cst.pg(), cbm.pg())
                MT = MT2[st_]
                for g in range(2):
                    tt(MT.ap[:, 8 * g:8 * g + 8, :], Eb.ap[:, 8 * g:8 * g + 8, :],
                       cbm.ap[:, g, :].unsqueeze(1).broadcast_to([128, 8, 128]), ALU.mult, Eb.pg() + cbm.pg(), MT.pg())

            def S2(i):
                st_ = i % 2
                acs_, dec_out_, dec_st_, cdec_ = SV(st_)
                MT = MT2[st_]
                cs_ = slice(i * 128, (i + 1) * 128)
                xs3 = xs_tok.ap[:, i, :].rearrange("p (h d) -> p h d", h=16)
                tt(v3(xdt), xs3, bc16(dtb[:, i, :]), ALU.mult, xs_tok.pg() + smal.pg(), xdt.pg())
                tt(v3(xdd), v3(xdt), bc16(dec_st_), ALU.mult, xdt.pg() + svk(st_), xdd.pg())
                bo = nb2()
                for g in range(2):
                    mm(PB(bo, 2)[:, g * 512:(g + 1) * 512], bcT.ap[:, 2 + g, cs_], sstb.ap[:, g * 512:(g + 1) * 512], True, True,
                       bcT.pg() + sstb.pg(), PK(bo, 2))
                bst = nb2()
                for g in range(2):
                    mm(PB(bst, 2)[:, g * 512:(g + 1) * 512], B_tok.ap[:, i, g * 128:(g + 1) * 128], xdd.ap[:, g * 512:(g + 1) * 512],
                       True, True, B_tok.pg() + xdd.pg(), PK(bst, 2))
                by = nb2()
                for h in range(16):
                    mm(PB(by, 2)[:, h * 64:(h + 1) * 64], MT.ap[:, h, :], xdt.ap[:, h * 64:(h + 1) * 64], True, True,
                       MT.pg() + xdt.pg(), PK(by, 2))
                y0, y1 = yf
                tt(v3(y0), PB(bo, 2).rearrange("p (h d) -> p h d", h=16), bc16(dec_out_), ALU.mult, PK(bo, 2) + svk(st_), y0.pg())
                tt(v3(sst), v3(sst), bc16(cdec_), ALU.mult, sst.pg() + svk(st_), sst.pg())
                tt(sst.ap, sst.ap, PB(bst, 2), ALU.add, sst.pg() + PK(bst, 2), sst.pg())
                cp(sstb.ap, sst.ap, sst.pg(), sstb.pg())
                tt(y0.ap, PB(by, 2), y0.ap, ALU.add, PK(by, 2) + y0.pg(), y0.pg())
                tt(v3(y1), xs3, bc16(pvc(l, 180, 16)), ALU.mult, xs_tok.pg() + pv.pg(), y1.pg())
                tt(y0.ap, y0.ap, y1.ap, ALU.add, y0.pg() + y1.pg(), y0.pg())
                tt(y0.ap, y0.ap, sz.ap[:, i, :], ALU.mult, y0.pg() + sz.pg(), y0.pg())
                ms(ssq, 0.0, smal2.pg())
                act(y1.ap, y0.ap, AF.Square, y0.pg(), y1.pg() + smal2.pg(), accum_out=ssq)
                act(ssq, ssq, AF.Ln, smal2.pg(), smal2.pg(), bias=EPS, scale=1.0 / 1024.0)
                act(ssq, ssq, AF.Exp, smal2.pg(), smal2.pg(), scale=-0.5)
                if i > 0:
                    to_featmajor(i - 1, l, 48)
                ts1(ynb.ap, y0.ap, ssq, ALU.mult, y0.pg() + smal2.pg(), ynb.pg())

            S1(0)
            for i in range(4):
                if i < 3:
                    S1(i + 1)
                S2(i)
            to_featmajor(3, l, 48)

        def ret_tile(l, ti):
            wl = win_d[l]
            t0 = ti * T
            dma("sp", cst_t.ap, cs_d[:, :, t0:t0 + T].rearrange("a p t -> p a t"), (), cst_t.pg(), "cs")
            cosv, sinv = cst_t.ap[:, 0, :], cst_t.ap[:, 1, :]
            qcnt = [0]

            def qk_consumer(which, scale):
                def f(h, b):
                    q_ = qf[qcnt[0] % 2]
                    t1 = t12[qcnt[0] % 2]
                    qcnt[0] += 1
                    act(q_.ap, PB(b), AF.Copy, PK(b), q_.pg(), scale=scale)
                    b2 = nb()
                    mm(PB(b2), Rm, q_.ap, True, True, cst.pg() + q_.pg(), PK(b2))
                    tt(t1.ap, PB(b2), sinv, ALU.mult, PK(b2) + cst_t.pg(), t1.pg())
                    tt(q_.ap, q_.ap, cosv, ALU.mult, q_.pg() + cst_t.pg(), q_.pg())
                    dst = which.ap[:, h, :]
                    tt(dst, q_.ap, t1.ap, ALU.add, q_.pg() + t1.pg(), which.pg(h * 1024, (h + 1) * 1024))
                return f

            proj_fm(wl, C_RQ, 4, qk_consumer(qTr, 1.0))
            proj_fm(wl, C_RK, 4, qk_consumer(kTr, 128.0 ** -0.5))
            for s2 in range(2):
                proj_tm(wl, C_RV + s2 * 512, 512,
                        lambda i, b, s2=s2: act(v_tok.ap[:, i, s2 * 512:(s2 + 1) * 512], PB(b), AF.Copy, PK(b), v_tok.pg()))
            for s2 in range(2):
                proj_tm(wl, C_RG + s2 * 512, 512,
                        lambda i, b, s2=s2: act(srg.ap[:, i, s2 * 512:(s2 + 1) * 512], PB(b), AF.Silu, PK(b), srg.pg()))
            for i in range(4):
                cs_ = slice(i * 128, (i + 1) * 128)
                bsd = nb()
                for h in range(4):
                    mm(PB(bsd)[:, h * 128:(h + 1) * 128], kTr.ap[:, h, cs_], qTr.ap[:, h, cs_], True, True, kTr.pg() + qTr.pg(), PK(bsd))
                tt(SDT.ap, PB(bsd).rearrange("p (h i) -> p h i", h=4), dmatT, ALU.mult, PK(bsd) + cst.pg(), SDT.pg())
                bk = nb()
                for h in range(4):
                    tr(PBb(bk)[:, h * 128:(h + 1) * 128], kTr.ap[:, h, cs_], identb, kTr.pg() + cbf.pg(), PK(bk))
                tt(kz.ap, PBb(bk)[:, 0:512].rearrange("p (h d) -> p h d", h=4), zeta.unsqueeze(2).broadcast_to([128, 4, 128]),
                   ALU.mult, PK(bk) + cst.pg(), kz.pg())
                tt(qx.ap, qTr.ap[:, :, cs_], xiT, ALU.mult, qTr.pg() + cst.pg(), qx.pg())
                bo = nb2()
                for h in range(4):
                    o_ = PB(bo, 2)[:, h * 256:(h + 1) * 256]
                    mm(o_, SDT.ap[:, h, :], v_tok.ap[:, i, h * 256:(h + 1) * 256], True, False, SDT.pg() + v_tok.pg(), PK(bo, 2))
                    mm(o_, qx.ap[:, h, :], rstb.ap[:, h * 256:(h + 1) * 256], False, True, qx.pg() + rstb.pg(), PK(bo, 2))
                bkv = nb2()
                for h in range(4):
                    mm(PB(bkv, 2)[:, h * 256:(h + 1) * 256], kz.ap[:, h, :], v_tok.ap[:, i, h * 256:(h + 1) * 256], True, True,
                       kz.pg() + v_tok.pg(), PK(bkv, 2))
                y0, y1 = yf
                act(y0.ap, PB(bo, 2), AF.Copy, PK(bo, 2), y0.pg())
                for h in range(4):
                    sl_ = slice(h * 256, (h + 1) * 256)
                    stt(rst.ap[:, sl_], rst.ap[:, sl_], math.exp(128.0 * LOGG[h]), PB(bkv, 2)[:, sl_], ALU.mult, ALU.add,
                        rst.pg() + PK(bkv, 2), rst.pg())
                cp(rstb.ap, rst.ap, rst.pg(), rstb.pg())
                for h in range(4):
                    S.add("dve", lambda e, h=h: e.bn_stats(out=bn6[:, h, :], in_=y0.ap[:, h * 256:(h + 1) * 256]), y0.pg(), smal2.pg())
                for h in range(4):
                    S.add("dve", lambda e, h=h: e.bn_aggr(out=mv[:, h, :], in_=bn6[:, h, :]), smal2.pg(), smal2.pg())
                act(rstd4, mv[:, :, 1], AF.Ln, smal2.pg(), smal2.pg(), bias=EPS, scale=1.0)
                act(rstd4, rstd4, AF.Exp, smal2.pg(), smal2.pg(), scale=-0.5)
                for h in range(4):
                    sl_ = slice(h * 256, (h + 1) * 256)
                    ts(y0.ap[:, sl_], y0.ap[:, sl_], mv[:, h, 0:1], rstd4[:, h:h + 1], ALU.subtract, ALU.mult,
                       y0.pg() + smal2.pg(), y0.pg())
                if i > 0:
                    to_featmajor(i - 1, l, 56)
                tt(ynb.ap, y0.ap, srg.ap[:, i, :], ALU.mult, y0.pg() + srg.pg(), ynb.pg())
            to_featmajor(3, l, 56)

        def att_tile(l, ti):
            wl = win_d[l]
            ms(qTm.ap, 0.0, qTm.pg())

            def q_consumer(c, b):
                act(qTm.ap[0:64, 2 * c, :], PB(b)[0:64, :], AF.Copy, PK(b), qTm.pg(2 * c * 1024, (2 * c + 2) * 1024), scale=0.125)
                act(qTm.ap[64:128, 2 * c + 1, :], PB(b)[64:128, :], AF.Copy, PK(b), qTm.pg(2 * c * 1024, (2 * c + 2) * 1024), scale=0.125)

            proj_fm(wl, C_AQ, 8, q_consumer)
            s = wring[0][wcnt[0] % len(wring[0])]
            wcnt[0] += 1
            pieces = [(C_AK + g * 64, 64, g * 128 + dup * 64) for g in range(2) for dup in range(2)] + [(C_AV, 128, 256)]

            def kv_dmas(e, s=s, pieces=pieces):
                out_ = []
                for c0, ncols, d0 in pieces:
                    src = wl[0:1024, c0:c0 + ncols].rearrange("(k p) n -> p k n", p=128)
                    out_.append(e.dma_start(out=wsl[s].ap[:, 0:8, d0:d0 + ncols], in_=src))
                return out_

            S.add("pool", kv_dmas, (), wsl[s].pg(), dma=("w", s), ndma=len(pieces))
            for g in range(2):
                b = nb()
                for k in range(8):
                    mm(PB(b), wsl[s].ap[:, k, g * 128:(g + 1) * 128], hT.ap[:, k, :], k == 0, k == 7, wsl[s].pg() + hT.pg(), PK(b))
                act(kTa.ap[:, g, 128:640], PB(b), AF.Copy, PK(b), kTa.pg())
            bv = nb()
            for i in range(4):
                for k in range(8):
                    mm(PB(bv)[:, i * 128:(i + 1) * 128], hT.ap[:, k, i * 128:(i + 1) * 128], wsl[s].ap[:, k, 256:384], k == 0, k == 7,
                       wsl[s].pg() + hT.pg(), PK(bv))
            vsrc = PB(bv).rearrange("p (i g d) -> p i g d", i=4, g=2)
            cp(vtok.ap[:, 1:5, :, 0:64], vsrc, PK(bv), vtok.pg())
            cp(vtok.ap[:, 1:5, :, 64:128], vsrc, PK(bv), vtok.pg())
            scnt = [0]
            PTs = [(PT, PTf), (PTb, PTbf)]

            def st_phase(i):
                ci = ti * 4 + i
                blks = [1] if ci == 0 else [0, 1]
                PT_, _ = PTs[i % 2]
                for blk in blks:
                    koff = i * 128 + blk * 128
                    for hg in range(4):
                        b = nb()
                        for hh in range(4):
                            h = hg * 4 + hh
                            g = h // 8
                            mm(PB(b)[:, hh * 128:(hh + 1) * 128], kTa.ap[:, g, koff:koff + 128], qTm.ap[:, h, i * 128:(i + 1) * 128],
                               True, True, kTa.pg() + qTm.pg(), PK(b))
                        stp = stmp[scnt[0] % 2]
                        scnt[0] += 1
                        tt(stp.ap.rearrange("p (h q) -> p h q", h=4), PB(b).rearrange("p (h q) -> p h q", h=4),
                           biasT.ap[:, blk, hg * 4:(hg + 1) * 4, :], ALU.add, PK(b) + biasT.pg(), stp.pg())
                        act(PT_.ap[:, blk, hg * 4:(hg + 1) * 4, :], stp.ap.rearrange("p (h q) -> p h q", h=4), AF.Exp, stp.pg(), PT_.pg())

            def pv_phase(i):
                ci = ti * 4 + i
                blks = [1] if ci == 0 else [0, 1]
                PT_, PTf_ = PTs[i % 2]
                dsb = yf[0]
                for g in range(2):
                    bo = nb2()
                    bd = nb2()
                    for half in range(2):
                        hs = 8 * g + 4 * half
                        for n_, blk in enumerate(blks):
                            rhs_ = PTf_.ap[:, blk, hs * 128:(hs + 4) * 128]
                            mm(PB(bo, 2)[:, half * 512:(half + 1) * 512], vtok.ap[:, i + blk, g, :], rhs_,
                               n_ == 0, n_ == len(blks) - 1, PT_.pg() + vtok.pg(), PK(bo, 2))
                    for half in range(2):
                        hs = 8 * g + 4 * half
                        for n_, blk in enumerate(blks):
                            rhs_ = PTf_.ap[:, blk, hs * 128:(hs + 4) * 128]
                            mm(PB(bd, 2)[:, half * 512:(half + 1) * 512], cbf.ap[:, 256:384], rhs_,
                               n_ == 0, n_ == len(blks) - 1, PT_.pg() + cbf.pg(), PK(bd, 2))
                    tt(dsb.ap.rearrange("p (h q) -> p h q", h=8), PB(bd, 2).rearrange("p (h q) -> p h q", h=8),
                       esink[:, 8 * g:8 * g + 8].unsqueeze(2).broadcast_to([128, 8, 128]), ALU.add, PK(bd, 2) + smal.pg(), dsb.pg())
                    S.add("dve", lambda e: e.reciprocal(out=dsb.ap, in_=dsb.ap), dsb.pg(), dsb.pg())
                    o4 = PB(bo, 2).rearrange("p (j two q) -> p j two q", j=4, two=2)
                    d4 = dsb.ap.rearrange("p (j two q) -> p j two q", j=4, two=2)
                    qs = slice(i * 128, (i + 1) * 128)
                    tt(yT.ap[0:64, 4 * g:4 * g + 4, qs], o4[0:64, :, 0, :], d4[0:64, :, 0, :], ALU.mult, PK(bo, 2) + dsb.pg(), yT.pg())
                    tt(yT.ap[64:128, 4 * g:4 * g + 4, qs], o4[64:128, :, 1, :], d4[64:128, :, 1, :], ALU.mult, PK(bo, 2) + dsb.pg(), yT.pg())

            if stop < 4.3:
                return
            st_phase(0)
            for i in range(4):
                if i < 3:
                    st_phase(i + 1)
                pv_phase(i)
            cp(vtok.ap[:, 0, :, :], vtok.ap[:, 4, :, :], vtok.pg(), vtok.pg())
            cp(kTa.ap[:, :, 0:128], kTa.ap[:, :, 512:640], kTa.pg(), kTa.pg())

        xin = yf[0]

        def load_tile(l, ti):
            t0 = ti * T
            if l == 0:
                for i in range(4):
                    dma("sp", xin.ap, x_d[t0 + i * 128:t0 + (i + 1) * 128, :], (), xin.pg(), "xin")
                    b = nb2()
                    for c in range(8):
                        tr(PB(b, 2)[:, c * 128:(c + 1) * 128], xin.ap[:, c * 128:(c + 1) * 128], identf, xin.pg() + cst.pg(), PK(b, 2))
                    cp(xt.ap[:, :, i * 128:(i + 1) * 128], PB(b, 2).rearrange("p (c t) -> p c t", c=8), PK(b, 2), xt.pg())
            else:
                dma("sp", xt.ap, xs_d[:, :, t0:t0 + T], [("xd", ti)], xt.pg(), "xt")

        def store_tile(l, ti):
            t0 = ti * T
            if l == L - 1:
                for i in range(4):
                    b = nb2()
                    for c in range(8):
                        tr(PB(b, 2)[:, c * 128:(c + 1) * 128], xt.ap[:, c, i * 128:(i + 1) * 128], identf, xt.pg() + cst.pg(), PK(b, 2))
                    cp(xin.ap, PB(b, 2), PK(b, 2), xin.pg())
                    dma("sp", out_d[t0 + i * 128:t0 + (i + 1) * 128, :], xin.ap, xin.pg(), [("od", ti, i)], "xout")
            else:
                dma("sp", xs_d[:, :, t0:t0 + T], xt.ap, xt.pg(), [("xd", ti)], "xst")

        for l in range(L):
            ms(sst.ap, 0.0, sst.pg())
            ms(sstb.ap, 0.0, sstb.pg())
            ms(rst.ap, 0.0, rst.pg())
            ms(rstb.ap, 0.0, rstb.pg())
            ms(hist.ap, 0.0, hist.pg())
            ms(kTa.ap, 0.0, kTa.pg())
            act(a_b, pvc(l, 164, 16), AF.Exp, pv.pg(), smal.pg())
            ts1(a_b, a_b, -1.0, ALU.mult, smal.pg(), smal.pg())
            act(esink, pvc(l, 196, 16), AF.Exp, pv.pg(), smal.pg())
            for ti in range(NT):
                load_tile(l, ti)
                if stop >= 1:
                    ffn(l, w1i_d, w1o_d, 0, 8)
                if stop >= 2:
                    pre_norm(l, 16)
                    ssd_tile(l, ti)
                    if dbg and l == 0:
                        dma("pool", dbg_d[0, :, :, ti * T:(ti + 1) * T], yT.ap, yT.pg(), [("dbg", 0, ti)], "dbg")
                if stop >= 3:
                    merge(l, 0)
                if stop >= 4:
                    ret_tile(l, ti)
                    if dbg and l == 0:
                        dma("pool", dbg_d[1, :, :, ti * T:(ti + 1) * T], yT.ap, yT.pg(), [("dbg", 1, ti)], "dbg")
                    merge(l, 1)
                if stop > 4.1:
                    att_tile(l, ti)
                    if dbg and l == 0:
                        dma("pool", dbg_d[2, :, :, ti * T:(ti + 1) * T], yT.ap, yT.pg(), [("dbg", 2, ti)], "dbg")
                    if stop >= 5:
                        merge(l, 2)
                if stop >= 6:
                    out_proj(l)
                if stop >= 7:
                    ffn(l, w2i_d, w2o_d, 32, 40)
                store_tile(l, ti)
        S.emit()
    return nc


def _t5_bucket(dist):
    is_small = dist < 16
    d = np.maximum(dist, 1).astype(np.float32)
    large = 16 + (np.log(d / 16) / math.log(128 / 16) * 16).astype(np.int32)
    large = np.minimum(large, 31)
    return np.where(is_small, dist, large)


def _consts(ntok):
    idx = np.arange(128)
    cst = np.zeros((128, NCST), np.float32)
    cst[:, 0:128] = np.eye(128)
    cst[:, 128:256] = (idx[:, None] <= idx[None, :])
    cst[:, 256:384] = 1.0
    cst[:, 384:512] = (idx[:, None] > idx[None, :])
    cst[:, 512:640] = (idx[None, :] >= idx[:, None])
    R = np.zeros((128, 128), np.float32)
    for dp in range(64):
        R[dp + 64, dp] = -1.0
        R[dp, dp + 64] = 1.0
    cst[:, 640:768] = R
    lg = np.array(LOGG, np.float64)
    diff = idx[None, :] - idx[:, None]
    dm = np.where(diff[None] >= 0, np.exp(np.maximum(diff, 0)[None] * lg[:, None, None]), 0.0)
    cst[:, 768:1280] = np.transpose(dm, (1, 0, 2)).reshape(128, 512)
    xi = np.exp((idx + 1.0)[None, :] * lg[:, None])
    cst[:, 1280:1792] = np.broadcast_to(xi.reshape(1, 512), (128, 512))
    cst[:, 1792:1796] = np.exp((127.0 - idx)[:, None] * lg[None, :])
    pos = np.arange(ntok, dtype=np.float32)
    inv = (1.0 / (np.float32(10000.0) ** (np.arange(0, 128, 2, dtype=np.float32) / np.float32(128)))).astype(np.float32)
    ang = pos[:, None] * inv[None, :]
    cos, sin = np.cos(ang).astype(np.float32).T, np.sin(ang).astype(np.float32).T
    cs = np.stack([np.concatenate([cos, cos], 0), np.concatenate([sin, sin], 0)], 0).astype(np.float32)
    return cst, np.ascontiguousarray(cs)


def _bias_table(rel_bias):
    qi = np.arange(128)[:, None]
    sj = np.arange(256)[None, :]
    dist = qi + 128 - sj
    mask = (dist >= 0) & (dist < 128)
    bb = rel_bias[_t5_bucket(np.maximum(dist, 0))]
    bb = np.where(mask[:, :, None], bb, np.float32(-1e30)).astype(np.float32)
    t = bb.reshape(128, 2, 128, 16)
    t = np.transpose(t, (2, 1, 3, 0))
    return np.ascontiguousarray(t.reshape(128, 2 * 16 * 128))


def _pack_params(L, p):
    fm = lambda v, n: v.reshape(n, 128).T
    pv = np.zeros((128, L, NV), np.float32)
    for l in range(L):
        for j, nm in enumerate(["ffn1_pre_g", "ffn1_post_g", "mix_pre_g", "mix_post_g", "ffn2_pre_g", "ffn2_post_g",
                                "ssd_norm_g", "ret_gn_g"]):
            pv[:, l, j * 8:(j + 1) * 8] = fm(p[nm][l], 8)
        cw = p["conv_w"][l]
        pv[:, l, 64:112] = np.transpose(cw.reshape(4, 12, 128), (2, 1, 0)).reshape(128, 48)
        pv[:, l, 112:124] = fm(p["conv_b"][l], 12)
        pv[:, l, 124:148] = fm(p["b_gate"][l], 24)
        pv[:, l, 148:164] = p["dt_bias"][l][None, :]
        pv[:, l, 164:180] = p["a_log"][l][None, :]
        pv[:, l, 180:196] = p["d_skip"][l][None, :]
        pv[:, l, 196:212] = p["attn_sinks"][l][None, :]
    return np.ascontiguousarray(pv.reshape(128, L * NV))


_NC_CACHE = {}


def run(inputs, L=DEPTH, ntok=SEQ, ncores=8, dbg=False, stop=99):
    p = {k: np.asarray(v, dtype=np.float32) for k, v in inputs.items()}
    key = (L, ntok, dbg, stop)
    if key not in _NC_CACHE:
        _NC_CACHE[key] = build(L, ntok, dbg, stop)
    nc = _NC_CACHE[key]
    cst, cs = _consts(ntok)
    shared = {
        "w1i": np.ascontiguousarray(p["w_ffn1_in"][:L]), "w1o": np.ascontiguousarray(p["w_ffn1_out"][:L]),
        "w2i": np.ascontiguousarray(p["w_ffn2_in"][:L]), "w2o": np.ascontiguousarray(p["w_ffn2_out"][:L]),
        "win": np.ascontiguousarray(p["w_in"][:L]), "wbr": np.ascontiguousarray(p["w_branch"][:L]),
        "wo": np.ascontiguousarray(p["w_out"][:L]),
        "pv": _pack_params(L, p), "cst": cst, "cs": cs, "biasT": _bias_table(p["rel_bias"]),
    }
    in_maps = []
    for b in range(ncores):
        m = dict(shared)
        m["x"] = np.ascontiguousarray(p["x"][b, :ntok])
        in_maps.append(m)
    res = run_bass_kernel_spmd(nc, in_maps, core_ids=list(range(ncores)))
    return res


def kernel(**inputs):
    res = run(inputs)
    return np.stack([np.asarray(r["out"], dtype=np.float32) for r in res.results], axis=0)
```
